# Optimizing a Trainium2 kernel written in Bass

```python
import math
import jax, jax.numpy as jnp
from jax import lax
import numpy as np

D_MODEL = 1024
BATCH = 16
SEQ = 2048
DEPTH = 4

MIX_WIDTH = D_MODEL
DN_HEADS = 8
DN_HEAD_DIM = 64
DN_WIDTH = DN_HEADS * DN_HEAD_DIM
DN_CONV = 4
CHUNK = 64
SC_WIDTH = MIX_WIDTH - DN_WIDTH
SC_GROUPS = 8
SC_GROUP_DIM = SC_WIDTH // SC_GROUPS
SC_CONV = 3
D_FF = ((8 * D_MODEL // 3 + 127) // 128) * 128
FFN_CONV = 3
EPS = 1e-6
IN_COLS = 4 * DN_WIDTH + 2 * DN_HEADS + 3 * SC_WIDTH
IN_SPLITS = (3 * DN_WIDTH,
             4 * DN_WIDTH,
             4 * DN_WIDTH + DN_HEADS,
             4 * DN_WIDTH + 2 * DN_HEADS,
             4 * DN_WIDTH + 2 * DN_HEADS + SC_WIDTH,
             4 * DN_WIDTH + 2 * DN_HEADS + 2 * SC_WIDTH)

kernel_name = "hybrid_deltanet_shortconv_convffn"


def rms_norm(x, gain):
    xf = x.astype(jnp.float32)
    y = xf * lax.rsqrt(jnp.mean(xf * xf, axis=-1, keepdims=True) + EPS)
    return (y * gain.astype(jnp.float32)).astype(x.dtype)


def l2_norm(x):
    return x * lax.rsqrt(jnp.sum(x * x, axis=-1, keepdims=True) + EPS)


def causal_dwconv(x, w):
    K, C = w.shape
    return lax.conv_general_dilated(
        x, w[:, None, :].astype(x.dtype), window_strides=(1,), padding=[(K - 1, 0)],
        dimension_numbers=("NWC", "WIO", "NWC"), feature_group_count=C)


def gated_delta_rule(q, k, v, g, beta):
    Bsz, T, H, Dk = q.shape
    Dv = v.shape[-1]
    N = T // CHUNK

    def chunks(t):
        t = jnp.moveaxis(t, 2, 1)
        return t.reshape((Bsz, H, N, CHUNK) + t.shape[3:])

    q, k, v, g, beta = (chunks(t) for t in (q, k, v, g, beta))
    g = jnp.cumsum(g, axis=-1)
    causal = jnp.tril(jnp.ones((CHUNK, CHUNK), bool))
    strict = jnp.tril(jnp.ones((CHUNK, CHUNK), bool), -1)
    decay = jnp.exp(jnp.where(causal, g[..., :, None] - g[..., None, :], -jnp.inf))

    k_beta = k * beta[..., None]
    v_beta = v * beta[..., None]
    a_mat = jnp.where(strict, jnp.einsum("bhncd,bhnsd->bhncs", k_beta, k) * decay, 0.0)
    tri = a_mat + jnp.eye(CHUNK, dtype=a_mat.dtype)
    u = lax.linalg.triangular_solve(tri, v_beta, left_side=True, lower=True, unit_diagonal=True)
    w = lax.linalg.triangular_solve(tri, k_beta * jnp.exp(g)[..., None],
                                    left_side=True, lower=True, unit_diagonal=True)

    qk = jnp.where(causal, jnp.einsum("bhncd,bhnsd->bhncs", q, k) * decay, 0.0)
    q_dec = q * jnp.exp(g)[..., None]
    g_last = g[..., -1]
    k_dec = k * jnp.exp(g_last[..., None] - g)[..., None]

    def step(S, xs):
        qd_c, qk_c, u_c, w_c, kd_c, gl_c = xs
        v_new = u_c - jnp.einsum("bhck,bhkv->bhcv", w_c, S)
        o_c = jnp.einsum("bhck,bhkv->bhcv", qd_c, S) + jnp.einsum("bhcs,bhsv->bhcv", qk_c, v_new)
        S = S * jnp.exp(gl_c)[..., None, None] + jnp.einsum("bhck,bhcv->bhkv", kd_c, v_new)
        return S, o_c

    xs = tuple(jnp.moveaxis(t, 2, 0) for t in (q_dec, qk, u, w, k_dec, g_last))
    S0 = jnp.zeros((Bsz, H, Dk, Dv), jnp.float32)
    _, o = lax.scan(step, S0, xs)
    o = jnp.moveaxis(o, 0, 2).reshape(Bsz, H, T, Dv)
    return jnp.moveaxis(o, 1, 2)


def hybrid_mixer(h, w_in, conv_qkv, a_log, dt_bias, head_norm, conv_sc, sc_norm, w_out):
    Bsz, T, _ = h.shape
    f32 = jnp.float32
    proj = jnp.einsum("btd,de->bte", h, w_in)
    qkv, z, b, a, sc_b, sc_c, sc_x = jnp.split(proj, IN_SPLITS, axis=-1)

    qkv = jax.nn.silu(causal_dwconv(qkv, conv_qkv))
    q, k, v = jnp.split(qkv, 3, axis=-1)
    heads = lambda t: t.reshape(Bsz, T, DN_HEADS, DN_HEAD_DIM).astype(f32)
    q = l2_norm(heads(q)) * (DN_HEAD_DIM ** -0.5)
    k = l2_norm(heads(k))
    v = heads(v)
    beta = jax.nn.sigmoid(b.astype(f32))
    g = -jnp.exp(a_log.astype(f32)) * jax.nn.softplus(a.astype(f32) + dt_bias.astype(f32))
    o = gated_delta_rule(q, k, v, g, beta)
    o = rms_norm(o, head_norm) * jax.nn.silu(heads(z))
    o_dn = o.reshape(Bsz, T, DN_WIDTH).astype(h.dtype)

    y = sc_b * causal_dwconv(sc_c * sc_x, conv_sc)
    y = rms_norm(y.reshape(Bsz, T, SC_GROUPS, SC_GROUP_DIM), sc_norm).reshape(Bsz, T, SC_WIDTH)

    mixed = jnp.concatenate([o_dn, y.astype(h.dtype)], axis=-1)
    return jnp.einsum("bte,ed->btd", mixed, w_out)


def channel_mixer(h, w_up, conv_ffn, w_down):
    up = jnp.einsum("btd,df->btf", h, w_up)
    val, gate = jnp.split(up, 2, axis=-1)
    val = causal_dwconv(val, conv_ffn)
    return jnp.einsum("btf,fd->btd", jax.nn.silu(val) * gate, w_down)


def setup_inputs(seed: int = 0) -> dict:
    key = jax.random.key(seed)
    ks = jax.random.split(key, 16)
    f32 = jnp.float32
    nrm = lambda k, shape, scale: scale * jax.random.normal(k, shape, f32)
    gain = lambda k, shape: 1.0 + 0.02 * jax.random.normal(k, shape, f32)
    x = jax.random.normal(ks[0], (BATCH, SEQ, D_MODEL), f32)
    attn_norm = gain(ks[1], (DEPTH, D_MODEL))
    w_in = nrm(ks[2], (DEPTH, D_MODEL, IN_COLS), D_MODEL ** -0.5)
    conv_qkv = nrm(ks[3], (DEPTH, DN_CONV, 3 * DN_WIDTH), DN_CONV ** -0.5)
    a_log = jnp.log(jax.random.uniform(ks[4], (DEPTH, DN_HEADS), f32, 1.0, 16.0))
    dt = jnp.exp(jax.random.uniform(ks[5], (DEPTH, DN_HEADS), f32, math.log(1e-3), math.log(1e-1)))
    dt_bias = dt + jnp.log(-jnp.expm1(-dt))
    head_norm = gain(ks[6], (DEPTH, DN_HEAD_DIM))
    conv_sc = nrm(ks[7], (DEPTH, SC_CONV, SC_WIDTH), SC_CONV ** -0.5)
    sc_norm = gain(ks[8], (DEPTH, SC_GROUPS, SC_GROUP_DIM))
    w_out = nrm(ks[9], (DEPTH, MIX_WIDTH, D_MODEL), MIX_WIDTH ** -0.5)
    ffn_norm = gain(ks[10], (DEPTH, D_MODEL))
    w_up = nrm(ks[11], (DEPTH, D_MODEL, 2 * D_FF), D_MODEL ** -0.5)
    conv_ffn = nrm(ks[12], (DEPTH, FFN_CONV, D_FF), FFN_CONV ** -0.5)
    w_down = nrm(ks[13], (DEPTH, D_FF, D_MODEL), D_FF ** -0.5)
    final_norm = gain(ks[14], (D_MODEL,))
    return {"x": x, "attn_norm": attn_norm, "w_in": w_in, "conv_qkv": conv_qkv,
            "a_log": a_log, "dt_bias": dt_bias, "head_norm": head_norm, "conv_sc": conv_sc,
            "sc_norm": sc_norm, "w_out": w_out, "ffn_norm": ffn_norm, "w_up": w_up,
            "conv_ffn": conv_ffn, "w_down": w_down, "final_norm": final_norm}


def reference(x, attn_norm, w_in, conv_qkv, a_log, dt_bias, head_norm, conv_sc, sc_norm,
              w_out, ffn_norm, w_up, conv_ffn, w_down, final_norm):
    for l in range(DEPTH):
        x = x + hybrid_mixer(rms_norm(x, attn_norm[l]), w_in[l], conv_qkv[l], a_log[l], dt_bias[l],
                             head_norm[l], conv_sc[l], sc_norm[l], w_out[l])
        x = x + channel_mixer(rms_norm(x, ffn_norm[l]), w_up[l], conv_ffn[l], w_down[l])
    return rms_norm(x, final_norm)
```

```python
import numpy as np
from contextlib import ExitStack
import concourse.bass as bass
import concourse.mybir as mybir
from concourse.bass_utils import run_bass_kernel_spmd

F32 = mybir.dt.float32
BF16 = mybir.dt.bfloat16
ALU = mybir.AluOpType
AF = mybir.ActivationFunctionType

D = 1024
KD = 8
T = 2048
TT = 1024
NT = T // TT
NCH = TT // 64
DFF = 2816
NF = 22
EPS = 1e-6
NEG = -30000.0
CQ, CK, CV, CZ, CB, CA, CSB, CSC, CSX = 0, 512, 1024, 1536, 2048, 2056, 2064, 2576, 3088
PL = 163


def layer_jobspec():
    jl = []
    for m in range(4):
        jl.append([("w_in", CSC + m * 128, 128, KD), ("w_in", CSX + m * 128, 128, KD), ("w_in", CSB + m * 128, 128, KD)])
    jl.append([("w_in", CZ, 512, KD)])
    jl.append([("w_in", CB, 16, KD)])
    for c0_ in (CQ, CK, CV):
        jl.append([("w_in", c0_, 512, KD)])
    for dg in range(2):
        jl.append([("w_out", dg * 512, 512, KD)])
    for g in range(NF // 2):
        jl.append([("w_up", g * 256, 256, KD), ("w_up", DFF + g * 256, 256, KD)])
    for d in range(KD):
        jl.append([("w_down", d * 128, 128, NF)])
    return jl


def layer_stream_cols():
    return sum(K_ * n for job in layer_jobspec() for (_, _, n, K_) in job)


class Sched:
    def __init__(self, nc, es):
        self.nc = nc
        self.es = es
        self.eng = {"pe": nc.tensor, "act": nc.scalar, "dve": nc.vector, "pool": nc.gpsimd, "sp": nc.sync}
        self.sem = {n: es.enter_context(nc.semaphore("s_" + n)) for n in self.eng}
        self.cnt = {n: 0 for n in self.eng}
        self.known = {n: {} for n in self.eng}
        self.lastw = {}
        self.readers = {}
        self.dsem = {}
        self.dcnt = {}
        self.pending = {n: False for n in self.eng}
        self.nins = 0

    def _deps(self, reads, writes):
        toks = []
        for k in reads:
            t = self.lastw.get(k)
            if t is not None:
                toks.append(t)
        for k in writes:
            t = self.lastw.get(k)
            if t is not None:
                toks.append(t)
            toks.extend(self.readers.get(k, ()))
        return toks

    def _wait(self, en, toks):
        need = {}
        kn = self.known[en]
        for (s, v) in toks:
            if kn.get(s, 0) >= v:
                continue
            if need.get(s, 0) < v:
                need[s] = v
        for s, v in need.items():
            self.eng[en].wait_ge(s, v)
            kn[s] = v
            self.nins += 1

    def _record(self, tok, reads, writes):
        for k in writes:
            self.lastw[k] = tok
            self.readers[k] = []
        for k in reads:
            self.readers.setdefault(k, []).append(tok)

    def op(self, en, fn, reads=(), writes=(), inc=True):
        xr = [k for k in reads if k.startswith("pb")]
        if xr:
            reads = [k for k in reads if not k.startswith("pb")]
            writes = list(writes) + xr
        self._wait(en, self._deps(reads, writes))
        ins = fn(self.eng[en])
        s = self.sem[en]
        self.nins += 1
        if inc:
            self.cnt[en] += 1
            ins.then_inc(s, 1)
            tok = (s, self.cnt[en])
            self.pending[en] = False
        else:
            tok = (s, self.cnt[en] + 1)
            self.pending[en] = True
        if en == "pe":
            self.known[en][s] = tok[1]
        self._record(tok, reads, writes)
        return ins

    def dma(self, en, out, in_, slot, reads=(), writes=()):
        if slot not in self.dsem:
            self.dsem[slot] = self.es.enter_context(self.nc.semaphore("d_" + slot))
            self.dcnt[slot] = 0
        self._wait(en, self._deps(reads, writes))
        self.dcnt[slot] += 16
        ins = self.eng[en].dma_start(out=out, in_=in_)
        ins.then_inc(self.dsem[slot], 16)
        self.nins += 1
        tok = (self.dsem[slot], self.dcnt[slot])
        self._record(tok, reads, writes)
        return ins

    def barrier(self):
        for en in self.eng:
            assert not self.pending[en], en
        toks = [(self.sem[o], self.cnt[o]) for o in self.eng if self.cnt[o] > 0]
        toks += [(self.dsem[k], self.dcnt[k]) for k in self.dsem]
        for en in ("pe", "act", "dve", "pool", "sp"):
            self._wait(en, toks)

    def drain_all(self, en):
        toks = [(self.sem[o], self.cnt[o]) for o in self.eng if self.cnt[o] > 0]
        toks += [(self.dsem[k], self.dcnt[k]) for k in self.dsem]
        self._wait(en, toks)


DBG = False


def build(NSEQ, DEPTH):
    nc = bass.Bass("TRN2", target_bir_lowering=False)
    x_d = nc.dram_tensor("x", [NSEQ * T, D], F32, kind="ExternalInput").ap()
    TOTL = layer_stream_cols()
    wst_d = nc.dram_tensor("wst", [DEPTH, 128, TOTL], F32, kind="ExternalInput").ap()
    NPRM = PL * DEPTH + 8
    prm_d = nc.dram_tensor("prm", [128, NPRM], F32, kind="ExternalInput").ap()
    NCST = 128 * 4 + 64 * 4 + 128 + 512
    cst_d = nc.dram_tensor("cst", [128, NCST], F32, kind="ExternalInput").ap()
    out_d = nc.dram_tensor("out", [NSEQ * T, D], F32, kind="ExternalOutput").ap()
    dbg_d = nc.dram_tensor("dbg", [16, 128, 512], F32, kind="ExternalOutput").ap() if DBG else None

    with ExitStack() as es:
        S = Sched(nc, es)

        def sb(name, shape, dt):
            return es.enter_context(nc.sbuf_tensor(name, shape, dt))

        def ps(name, shape, dt):
            return es.enter_context(nc.psum_tensor(name, shape, dt))

        xT = sb("xT", [128, KD, TT], F32)
        arena = sb("arena", [128, 28 * 1024], BF16)
        arenaB = sb("arenaB", [128, 12 * 1024], F32)
        rstd = sb("rstd", [128, TT], F32)
        kqbd_t = sb("kqbd_t", [128, 1024], BF16)
        ctmp = rstd
        HTK = ["hT%d" % k_ for k_ in range(KD)]
        NW = 4
        wbuf = [sb("wbuf%d" % i, [128, 4096], BF16) for i in range(NW)]
        prm = sb("prm_sb", [128, NPRM], F32)
        cst = sb("cst_sb", [128, NCST], F32)
        identb = sb("identb", [128, 128], BF16)
        onesb = sb("onesb", [128, 128], BF16)
        blkb = sb("blkb", [128, 128], BF16)
        Sbd = [sb("Sbd%d" % l, [128, 512], F32) for l in range(DEPTH)]
        Sb = [sb("Sb%d" % l, [128, 512], BF16) for l in range(DEPTH)]
        hal_qkv = sb("hal_qkv", [128, DEPTH * 12 * 3], F32)
        hal_sc = sb("hal_sc", [128, DEPTH * 4 * 2], F32)
        hal_ff = sb("hal_ff", [128, DEPTH * NF * 2], F32)
        tk = {n: sb("tk_" + n, [64, 128], F32) for n in
              ("bet", "nbet", "xg", "g", "gc", "gam", "bg", "ekd", "nega")}
        egl = sb("egl", [128, 128], F32)

        def av(off, n):
            return arena[:, off:off + n]
        qT = av(0, 4096).rearrange("p (m t) -> p m t", m=4)
        kT = av(4096, 4096).rearrange("p (m t) -> p m t", m=4)
        vT = av(8192, 4096).rearrange("p (m t) -> p m t", m=4)
        yT = av(12288, 4096).rearrange("p (m t) -> p m t", m=4)
        oT = av(16384, 4096).rearrange("p (m t) -> p m t", m=4)
        szT = av(20480, 8192).bitcast(F32).rearrange("p (m t) -> p m t", m=4)
        actT = av(0, NF * TT).rearrange("p (f t) -> p f t", f=NF)
        stage = [av(22528 + i * 2048, 2048).bitcast(F32) for i in range(2)]
        hT = arenaB[:, 0:4096].bitcast(BF16).rearrange("p (k t) -> p k t", k=KD)
        def bv(off, n, dt=F32, parts=64):
            a = arenaB[0:parts, off:off + n]
            return a.bitcast(dt) if dt is not F32 else a
        o_ = [0]
        def balloc(n_f32, dt=F32, parts=64):
            a = bv(o_[0], n_f32, dt, parts)
            o_[0] += n_f32
            return a
        decL = balloc(512); decQ = balloc(512)
        R2L = balloc(256, BF16); R2Q = balloc(256, BF16)
        N0 = balloc(256, BF16)
        Pb = [balloc(256, BF16) for _ in range(2)]
        PTT = [[balloc(512, BF16) for _ in range(2)] for _ in range(2)]
        qkTb2 = [balloc(256, BF16) for _ in range(2)]
        kd2 = [balloc(256, BF16) for _ in range(2)]
        vb2 = [balloc(512) for _ in range(2)]
        t_x = balloc(512)
        r_b = balloc(256, BF16)
        vnew = balloc(256, BF16)
        t1 = balloc(512, F32, 128)
        o1 = balloc(512); o_tok = balloc(512); osq = balloc(512)
        on_b = balloc(256, BF16)
        egm2 = [balloc(512, F32, 128) for _ in range(2)]
        ssr = balloc(8); rr = balloc(8)
        kqbd = kqbd_t[:, :]
        assert o_[0] <= 11 * 1024, o_[0]
        CHS = []
        off_ = 4096
        for s_ in range(3):
            cb_ = arenaB[:, off_:off_ + TT + 4]; off_ += TT + 4
            ac_ = arenaB[:, off_:off_ + TT]; off_ += TT
            sq_ = arenaB[:, off_:off_ + TT // 2].bitcast(BF16); off_ += TT // 2
            CHS.append(dict(cb=cb_, ac=ac_, sq=sq_, n="%d" % s_))
        assert off_ <= 12 * 1024
        chn = [0]
        def next_set():
            c_ = CHS[chn[0] % 3]
            chn[0] += 1
            return c_
        bkn = [0]
        def next_banks():
            b_ = ((0, 1), (2, 3), (4, 5))[bkn[0] % 3]
            bkn[0] += 1
            return b_
        pend = []
        def flushA(keep=0):
            for it in pend[:max(0, len(pend) - keep)]:
                if not it[1]:
                    next(it[0])
                    it[1] = True
        def flushB(keep=0):
            while len(pend) > keep:
                it = pend.pop(0)
                if not it[1]:
                    next(it[0])
                for _ in it[0]:
                    pass
        def flush(keep=0):
            flushA(keep)
            flushB(keep)

        pb = [ps("pb%d" % i, [128, 512], F32) for i in range(7)] + [ps("pb7", [128, 1024], BF16)]
        pk = ["pb%d" % i for i in range(8)]

        S.dma("sp", prm[:], prm_d, "prm", writes=["prm"])
        S.dma("sp", cst[:], cst_d, "cst", writes=["cst"])
        ident = cst[:, 0:128]
        ones_f = cst[:, 128:256]
        blk_f = cst[:, 256:384]
        sellast = cst[0:64, 384:512]
        c0 = 512
        tri = cst[0:64, c0:c0 + 64]
        maskLs = cst[0:64, c0 + 64:c0 + 128]
        maskUs = cst[0:64, c0 + 128:c0 + 192]
        maskUi = cst[0:64, c0 + 192:c0 + 256]
        c1 = c0 + 256
        ones64 = cst[0:64, c1:c1 + 64]
        i64 = cst[0:64, 0:64]
        c2 = c1 + 128
        maskrep = cst[:, c2:c2 + 512]
        S.op("act", lambda e: e.copy(out=identb[:], in_=ident), reads=["cst"], writes=["identb"])
        S.op("act", lambda e: e.copy(out=onesb[:], in_=ones_f), reads=["cst"], writes=["onesb"])
        S.op("act", lambda e: e.copy(out=blkb[:], in_=blk_f), reads=["cst"], writes=["blkb"])
        S.op("pool", lambda e: e.memset(kqbd, 0.0), writes=["kqbd"])
        cb16 = sb("cb16", [64, 128 + 1024], BF16)
        trigt_f = sb("trigt_f", [64, 64], F32)
        tri_b = cb16[:, 0:64]
        trigt_b = cb16[:, 64:128]
        mLs_b = cb16[:, 128:640]
        mUi_b = cb16[:, 640:1152]
        S.op("dve", lambda e: e.tensor_scalar(out=trigt_f[:, :], in0=maskLs, scalar1=1.0 / 30000.0, scalar2=1.0,
                                              op0=ALU.mult, op1=ALU.add), reads=["cst"], writes=["cb16"])
        S.op("act", lambda e: e.copy(out=tri_b, in_=tri), reads=["cst"], writes=["cb16"])
        S.op("act", lambda e: e.copy(out=trigt_b, in_=trigt_f[:, :]), reads=["cb16"], writes=["cb16"])
        S.op("act", lambda e: e.copy(out=mLs_b.rearrange("p (h x) -> p h x", h=8),
                                     in_=maskLs.unsqueeze(1).to_broadcast([64, 8, 64])), reads=["cst"], writes=["cb16"])
        S.op("act", lambda e: e.copy(out=mUi_b.rearrange("p (h x) -> p h x", h=8),
                                     in_=maskUi.unsqueeze(1).to_broadcast([64, 8, 64])), reads=["cst"], writes=["cb16"])

        def P(l, off, n=1):
            return prm[:, l * PL + off: l * PL + off + n]

        wjobs = []
        wstate = {"use": 0, "iss": 0}
        LOOK = 2
        def wviews(j):
            i = j % NW
            views = []
            off = 0
            for (K_, n_) in wjobs[j][2]:
                views.append(wbuf[i][:, off:off + K_ * n_].rearrange("p (k n) -> p k n", k=K_))
                off += K_ * n_
            assert off <= 4096
            return i, views, off
        def wissue(j):
            i, views, sz = wviews(j)
            l_, off_, _ = wjobs[j]
            S.dma("pool", wbuf[i][:, 0:sz], wst_d[l_][:, off_:off_ + sz], "w%d" % i, writes=["w%d" % i])
        def wload(shapes):
            j = wstate["use"]
            assert list(shapes) == list(wjobs[j][2]), (shapes, wjobs[j])
            while wstate["iss"] < len(wjobs) and wstate["iss"] <= j + LOOK:
                wissue(wstate["iss"])
                wstate["iss"] += 1
            wstate["use"] += 1
            i, views, _ = wviews(j)
            return i, views

        def proj(banks, lhs_fn, rhs, K_, wkey, rkeys):
            for half in range(2):
                b = banks[half]
                for k in range(K_):
                    S.op("pe", lambda e, k=k, half=half, b=b: e.matmul(
                        pb[b][:, :], lhsT=lhs_fn(k), rhs=rhs(k, half), start=(k == 0), stop=(k == K_ - 1)),
                        reads=[wkey] + rkeys, writes=[pk[b]], inc=(k == K_ - 1))

        def hT_rhs(k, half):
            return hT[:, k, half * 512:(half + 1) * 512]

        def rsqrt_from(psbank, half, out_ap, scale, key_out):
            S.op("act", lambda e: e.activation(out=out_ap, in_=pb[psbank][:, :], func=AF.Ln, bias=EPS, scale=scale),
                 reads=[pk[psbank]], writes=[key_out])
            S.op("act", lambda e: e.activation(out=out_ap, in_=out_ap, func=AF.Exp, scale=-0.5),
                 reads=[key_out], writes=[key_out])

        def rmsnorm_to_hT(gcol_fn):
            for k in range(KD):
                S.op("act", lambda e, k=k: e.activation(out=hT[:, k, :], in_=xT[:, k, :], func=AF.Square),
                     reads=["xT"], writes=["hT%d" % k])
            bk = next_banks()
            for half in range(2):
                for k in range(KD):
                    S.op("pe", lambda e, k=k, half=half: e.matmul(
                        pb[bk[half]][:, :], lhsT=onesb[:], rhs=hT[:, k, half * 512:(half + 1) * 512],
                        start=(k == 0), stop=(k == KD - 1)),
                        reads=["onesb", "hT%d" % k], writes=[pk[bk[half]]], inc=(k == KD - 1))
                rsqrt_from(bk[half], half, rstd[:, half * 512:(half + 1) * 512], 1.0 / D, "rstd%d" % half)
            for k in range(KD):
                S.op("dve", lambda e, k=k: e.scalar_tensor_tensor(
                    out=hT[:, k, :], in0=xT[:, k, :], scalar=gcol_fn(k), in1=rstd[:, :],
                    op0=ALU.mult, op1=ALU.mult), reads=["xT", "rstd0", "rstd1", "prm"], writes=["hT%d" % k])

        def conv(C_, K_, wcol_fn):
            n = C_["n"]
            sp = 896 if K_ == 4 else 768
            wp = TT - sp
            rk = ["cb" + n + "h0", "cb" + n + "h1", "cb" + n + "hal", "prm"]
            hk = ["ac" + n + "h0", "ac" + n + "h1"]
            S.op("dve", lambda e: e.tensor_scalar(out=C_["ac"][:, 0:sp], in0=C_["cb"][:, 0:sp],
                                                  scalar1=wcol_fn(0), scalar2=None, op0=ALU.mult),
                 reads=rk, writes=["ac" + n + "c0"] + hk)
            for j in range(1, K_):
                S.op("dve", lambda e, j=j: e.scalar_tensor_tensor(
                    out=C_["ac"][:, 0:sp], in0=C_["cb"][:, j:j + sp], scalar=wcol_fn(j),
                    in1=C_["ac"][:, 0:sp], op0=ALU.mult, op1=ALU.add), reads=rk, writes=["ac" + n + "c0"])
            tmp = C_["sq"].bitcast(F32)[:, 0:wp]
            S.op("pool", lambda e: e.tensor_tensor(out=C_["ac"][:, sp:TT], in0=C_["cb"][:, sp:TT],
                                                   in1=wcol_fn(0).to_broadcast([128, wp]), op=ALU.mult),
                 reads=rk, writes=["ac" + n + "c1"] + hk)
            for j in range(1, K_):
                S.op("pool", lambda e, j=j: e.tensor_tensor(out=tmp, in0=C_["cb"][:, sp + j:TT + j],
                                                            in1=wcol_fn(j).to_broadcast([128, wp]), op=ALU.mult),
                     reads=rk, writes=["sq" + n])
                S.op("pool", lambda e: e.tensor_tensor(out=C_["ac"][:, sp:TT], in0=C_["ac"][:, sp:TT], in1=tmp,
                                                       op=ALU.add), reads=["sq" + n], writes=["ac" + n + "c1"])

        def ackeys(C_):
            return ["ac" + C_["n"] + "h0", "ac" + C_["n"] + "h1", "ac" + C_["n"] + "c0", "ac" + C_["n"] + "c1"]

        def cbkeys(C_):
            return ["cb" + C_["n"] + "h0", "cb" + C_["n"] + "h1", "cb" + C_["n"] + "hal"]

        def halo_in(C_, hal_ap, hkey, K_, first):
            n = C_["n"]
            if first:
                S.op("pool", lambda e: e.memset(C_["cb"][:, 0:K_ - 1], 0.0), writes=["cb" + n + "hal"])
            else:
                S.op("pool", lambda e: e.tensor_copy(out=C_["cb"][:, 0:K_ - 1], in_=hal_ap), reads=[hkey],
                     writes=["cb" + n + "hal"])

        def halo_out(C_, hal_ap, hkey, K_):
            n = C_["n"]
            S.op("pool", lambda e: e.tensor_copy(out=hal_ap, in_=C_["cb"][:, TT:TT + K_ - 1]), reads=["cb" + n + "h1"],
                 writes=[hkey])

        for _s in range(NSEQ):
            for _t in range(NT):
                for l in range(DEPTH):
                    off_ = 0
                    for job in layer_jobspec():
                        shp = [(K_, n) for (_, _, n, K_) in job]
                        wjobs.append((l, off_, shp))
                        off_ += sum(K_ * n for (K_, n) in shp)

        for sq_i in range(NSEQ):
            for ti in range(NT):
                first = (ti == 0)
                row0 = sq_i * T + ti * TT
                S.barrier()
                for tb in range(8):
                    st = stage[tb % 2]
                    S.dma("sp", st, x_d[row0 + tb * 128: row0 + (tb + 1) * 128, :], "st%d" % (tb % 2),
                          writes=["stage%d" % (tb % 2)])
                    for dq in range(2):
                        b = 5 + dq
                        for dd in range(4):
                            d = dq * 4 + dd
                            S.op("pe", lambda e, d=d, dd=dd, b=b, st=st: e.transpose(
                                pb[b][:, dd * 128:(dd + 1) * 128], st[:, d * 128:(d + 1) * 128], ident),
                                reads=["stage%d" % (tb % 2), "cst"], writes=[pk[b]], inc=(dd == 3))
                        S.op("act", lambda e, dq=dq, b=b, tb=tb: e.copy(
                            out=xT[:, dq * 4:(dq + 1) * 4, tb * 128:(tb + 1) * 128],
                            in_=pb[b][:, :].rearrange("p (d t) -> p d t", d=4)),
                            reads=[pk[b]], writes=["xT"])
                if first:
                    for l in range(DEPTH):
                        S.op("pool", lambda e, l=l: e.memset(Sbd[l][:], 0.0), writes=["Sbd%d" % l])
                        S.op("pool", lambda e, l=l: e.memset(Sb[l][:], 0.0), writes=["Sb%d" % l])

                for l in range(DEPTH):
                    S.barrier()
                    rmsnorm_to_hT(lambda k: P(l, 0 + k))
                    for m in range(4):
                        wi, (wC, wX, wB) = wload([(KD, 128)] * 3)
                        wkey = "w%d" % wi
                        C_ = next_set(); n = C_["n"]
                        bC = next_banks()
                        proj(bC, lambda k: wC[:, k, :], hT_rhs, KD, wkey, HTK)
                        for half in range(2):
                            S.op("act", lambda e, half=half: e.copy(out=C_["ac"][:, half * 512:(half + 1) * 512],
                                                                     in_=pb[bC[half]][:, :]),
                                 reads=[pk[bC[half]]], writes=["ac" + n + "h%d" % half])
                        flushA(keep=1)
                        bX = next_banks()
                        proj(bX, lambda k: wX[:, k, :], hT_rhs, KD, wkey, HTK)
                        hal = hal_sc[:, (l * 4 + m) * 2:(l * 4 + m) * 2 + 2]
                        hkey = "hsc%d_%d" % (l, m)
                        halo_in(C_, hal, hkey, 3, first)
                        for half in range(2):
                            S.op("dve", lambda e, half=half: e.tensor_tensor(
                                out=C_["cb"][:, 2 + half * 512:2 + (half + 1) * 512], in0=pb[bX[half]][:, :],
                                in1=C_["ac"][:, half * 512:(half + 1) * 512], op=ALU.mult),
                                reads=[pk[bX[half]], "ac" + n + "h%d" % half], writes=["cb" + n + "h%d" % half])
                        conv(C_, 3, lambda j: P(l, 64 + m * 3 + j))
                        halo_out(C_, hal, hkey, 3)
                        bB = next_banks()
                        proj(bB, lambda k: wB[:, k, :], hT_rhs, KD, wkey, HTK)
                        for half in range(2):
                            S.op("dve", lambda e, half=half: e.tensor_tensor(
                                out=C_["cb"][:, half * 512:(half + 1) * 512], in0=pb[bB[half]][:, :],
                                in1=C_["ac"][:, half * 512:(half + 1) * 512], op=ALU.mult),
                                reads=[pk[bB[half]]] + ackeys(C_) + cbkeys(C_), writes=cbkeys(C_))
                        S.op("pool", lambda e: e.tensor_tensor(out=C_["sq"][:, :], in0=C_["cb"][:, 0:TT], in1=C_["cb"][:, 0:TT],
                                                               op=ALU.mult), reads=cbkeys(C_), writes=["sq" + n])
                        flush(keep=1)
                        def st2(C_=C_, n=n, m=m):
                            bS = next_banks()
                            for half in range(2):
                                S.op("pe", lambda e, half=half: e.matmul(pb[bS[half]][:, :], lhsT=blkb[:],
                                                                          rhs=C_["sq"][:, half * 512:(half + 1) * 512],
                                                                          start=True, stop=True),
                                     reads=["blkb", "sq" + n], writes=[pk[bS[half]]])
                                rsqrt_from(bS[half], half, C_["ac"][:, half * 512:(half + 1) * 512], 1.0 / 64, "ac" + n + "h%d" % half)
                            yield
                            S.op("dve", lambda e: e.scalar_tensor_tensor(
                                out=yT[:, m, :], in0=C_["cb"][:, 0:TT], scalar=P(l, 142 + m), in1=C_["ac"][:, :],
                                op0=ALU.mult, op1=ALU.mult), reads=cbkeys(C_) + ackeys(C_) + ["prm"], writes=["yT"])
                        pend.append([st2(), False])
                    wi, (wZ,) = wload([(KD, 512)])
                    for m in range(4):
                        bZ = next_banks()
                        proj(bZ, lambda k, m=m: wZ[:, k, m * 128:(m + 1) * 128], hT_rhs, KD, "w%d" % wi, HTK)
                        for half in range(2):
                            S.op("act", lambda e, half=half, m=m: e.activation(
                                out=szT[:, m, half * 512:(half + 1) * 512], in_=pb[bZ[half]][:, :], func=AF.Silu),
                                reads=[pk[bZ[half]]], writes=["szT"])
                        flush()
                    wi, (wBA,) = wload([(KD, 16)])
                    for i in range(NCH):
                        for k in range(KD):
                            S.op("pe", lambda e, i=i, k=k: e.matmul(
                                pb[4][0:64, i * 16:(i + 1) * 16], lhsT=hT[:, k, i * 64:(i + 1) * 64], rhs=wBA[:, k, :],
                                start=(k == 0), stop=(k == KD - 1)),
                                reads=["w%d" % wi] + HTK, writes=[pk[4]], inc=(k == KD - 1))
                    bav = pb[4][0:64, 0:256].rearrange("p (i c) -> p i c", c=16)
                    def tv(n):
                        return tk[n][:, :].rearrange("p (i h) -> p i h", h=8)
                    S.op("act", lambda e: e.activation(out=tv("bet"), in_=bav[:, :, 0:8], func=AF.Sigmoid),
                         reads=[pk[4]], writes=["bet"])
                    S.op("dve", lambda e: e.tensor_scalar(out=tk["nbet"][:, :], in0=tk["bet"][:, :], scalar1=-1.0, scalar2=None,
                                                          op0=ALU.mult), reads=["bet"], writes=["nbet"])
                    S.op("dve", lambda e: e.tensor_tensor(
                        out=tv("xg"), in0=bav[:, :, 8:16],
                        in1=prm[0:64, l * PL + 155:l * PL + 163].unsqueeze(1).to_broadcast([64, NCH, 8]), op=ALU.add),
                        reads=[pk[4], "prm"], writes=["xg"])
                    S.op("act", lambda e: e.activation(out=tk["xg"][:, :], in_=tk["xg"][:, :], func=AF.Exp),
                         reads=["xg"], writes=["xg"])
                    S.op("act", lambda e: e.activation(out=tk["xg"][:, :], in_=tk["xg"][:, :], func=AF.Ln, bias=1.0),
                         reads=["xg"], writes=["xg"])
                    S.op("act", lambda e: e.activation(out=tk["nega"][:, 0:8], in_=prm[0:64, l * PL + 147:l * PL + 155],
                                                       func=AF.Exp), reads=["prm"], writes=["nega"])
                    S.op("dve", lambda e: e.scalar_tensor_tensor(
                        out=tv("g"), in0=tv("xg"), scalar=-1.0,
                        in1=tk["nega"][:, 0:8].unsqueeze(1).to_broadcast([64, NCH, 8]), op0=ALU.mult, op1=ALU.mult),
                        reads=["xg", "nega"], writes=["g"])
                    S.op("pe", lambda e: e.matmul(pb[5][0:64, 0:128], lhsT=tri, rhs=tk["g"][:, :], start=True, stop=True),
                         reads=["cst", "g"], writes=[pk[5]])
                    S.op("act", lambda e: e.copy(out=tk["gc"][:, :], in_=pb[5][0:64, 0:128]), reads=[pk[5]], writes=["gc"])
                    S.op("pe", lambda e: e.matmul(pb[4][:, 0:128], lhsT=sellast, rhs=tk["gc"][:, :], start=True, stop=True),
                         reads=["cst", "gc"], writes=[pk[4]])
                    S.op("act", lambda e: e.activation(out=egl[:, :], in_=pb[4][:, 0:128], func=AF.Exp),
                         reads=[pk[4]], writes=["egl"])
                    S.op("dve", lambda e: e.tensor_tensor(out=tk["ekd"][:, :], in0=pb[4][0:64, 0:128], in1=tk["gc"][:, :],
                                                          op=ALU.subtract), reads=[pk[4], "gc"], writes=["ekd"])
                    S.op("act", lambda e: e.activation(out=tk["ekd"][:, :], in_=tk["ekd"][:, :], func=AF.Exp),
                         reads=["ekd"], writes=["ekd"])
                    S.op("act", lambda e: e.activation(out=tk["gam"][:, :], in_=tk["gc"][:, :], func=AF.Exp),
                         reads=["gc"], writes=["gam"])
                    S.op("dve", lambda e: e.tensor_tensor(out=tk["bg"][:, :], in0=tk["bet"][:, :], in1=tk["gam"][:, :],
                                                          op=ALU.mult), reads=["bet", "gam"], writes=["bg"])
                    for ty, (c0_, dst) in enumerate(((CQ, qT), (CK, kT), (CV, vT))):
                        wi, (wQ,) = wload([(KD, 512)])
                        for m in range(4):
                            ch = ty * 4 + m
                            C_ = next_set(); n = C_["n"]
                            bQ = next_banks()
                            proj(bQ, lambda k, m=m: wQ[:, k, m * 128:(m + 1) * 128], hT_rhs, KD, "w%d" % wi, HTK)
                            hal = hal_qkv[:, (l * 12 + ch) * 3:(l * 12 + ch) * 3 + 3]
                            hkey = "hqkv%d_%d" % (l, ch)
                            halo_in(C_, hal, hkey, 4, first)
                            for half, en in ((0, "act"), (1, "act")):
                                if en == "act":
                                    S.op("act", lambda e, half=half: e.copy(
                                        out=C_["cb"][:, 3 + half * 512:3 + (half + 1) * 512], in_=pb[bQ[half]][:, :]),
                                        reads=[pk[bQ[half]]], writes=["cb" + n + "h%d" % half])
                                else:
                                    S.op("dve", lambda e, half=half: e.tensor_copy(
                                        out=C_["cb"][:, 3 + half * 512:3 + (half + 1) * 512], in_=pb[bQ[half]][:, :]),
                                        reads=[pk[bQ[half]]], writes=["cb" + n + "h%d" % half])
                            flushA(keep=1 if ty < 2 else 0)
                            conv(C_, 4, lambda j, ch=ch: P(l, 16 + ch * 4 + j))
                            halo_out(C_, hal, hkey, 4)
                            if ty == 2:
                                S.op("act", lambda e, m=m: e.activation(out=vT[:, m, :], in_=C_["ac"][:, :], func=AF.Silu),
                                     reads=ackeys(C_), writes=["vT"])
                                flush()
                            else:
                                S.op("act", lambda e: e.activation(out=C_["ac"][:, :], in_=C_["ac"][:, :], func=AF.Silu),
                                     reads=ackeys(C_), writes=ackeys(C_))
                                S.op("pool", lambda e: e.tensor_tensor(out=C_["sq"][:, :], in0=C_["ac"][:, :], in1=C_["ac"][:, :],
                                                                       op=ALU.mult), reads=ackeys(C_), writes=["sq" + n])
                                flush(keep=1)
                                def st2(C_=C_, n=n, m=m, ty=ty):
                                    bS = next_banks()
                                    for half in range(2):
                                        S.op("pe", lambda e, half=half: e.matmul(pb[bS[half]][:, :], lhsT=blkb[:],
                                                                                  rhs=C_["sq"][:, half * 512:(half + 1) * 512],
                                                                                  start=True, stop=True),
                                             reads=["blkb", "sq" + n], writes=[pk[bS[half]]])
                                        rsqrt_from(bS[half], half, C_["cb"][:, half * 512:(half + 1) * 512], 1.0,
                                                   "cb" + n + "h%d" % half)
                                    yield
                                    if ty == 0:
                                        S.op("dve", lambda e: e.scalar_tensor_tensor(
                                            out=qT[:, m, :], in0=C_["ac"][:, :], scalar=0.125, in1=C_["cb"][:, 0:TT],
                                            op0=ALU.mult, op1=ALU.mult), reads=ackeys(C_) + cbkeys(C_), writes=["qT"])
                                    else:
                                        S.op("dve", lambda e: e.tensor_tensor(
                                            out=kT[:, m, :], in0=C_["ac"][:, :], in1=C_["cb"][:, 0:TT], op=ALU.mult),
                                            reads=ackeys(C_) + cbkeys(C_), writes=["kT"])
                                pend.append([st2(), False])
                    flush()
                    S.barrier()
                    SL = Sbd[l]; SBl = Sb[l]; skey = "Sbd%d" % l; sbkey = "Sb%d" % l
                    def h3(ap):
                        return ap.rearrange("p (h x) -> p h x", h=8)
                    def tkb(n, i, parts=64):
                        return tk[n][0:parts, i * 8:(i + 1) * 8].unsqueeze(2).to_broadcast([parts, 8, 64])
                    def cb(ap):
                        return ap.unsqueeze(1).to_broadcast([64, 8, 64])
                    i64b = identb[0:64, 0:64]
                    pT = pb[7][:, :]

                    def prep(i):
                        pp = str(i % 2)
                        qkTb = qkTb2[i % 2]; kd = kd2[i % 2]; vb = vb2[i % 2]; egm = egm2[i % 2]
                        tok = slice(i * 64, (i + 1) * 64)
                        kq5 = kqbd.rearrange("p (w m hh x) -> p w m hh x", w=2, m=4, hh=2)
                        for w_, src, skey_ in ((0, kT, "kT"), (1, qT, "qT")):
                            for hh in range(2):
                                S.op("pool", lambda e, w_=w_, src=src, hh=hh: e.tensor_copy(
                                    out=kq5[hh * 64:(hh + 1) * 64, w_, :, hh, :], in_=src[hh * 64:(hh + 1) * 64, :, tok]),
                                    reads=[skey_], writes=["kqbd"])
                        kq3 = kqbd.rearrange("p (w m n) -> p w m n", w=2, m=4)
                        for w_ in range(2):
                            for m in range(4):
                                S.op("pe", lambda e, w_=w_, m=m: e.matmul(
                                    pb[w_][0:64, m * 128:(m + 1) * 128], lhsT=kT[:, m, tok], rhs=kq3[:, w_, m, :],
                                    start=True, stop=True), reads=["kT", "kqbd"], writes=[pk[w_]], inc=(m == 3))
                        for w_, src, skey_ in ((0, kT, "kT"), (1, vT, "vT")):
                            for m in range(4):
                                S.op("pe", lambda e, w_=w_, m=m, src=src: e.transpose(
                                    pT[0:64, w_ * 512 + m * 128: w_ * 512 + (m + 1) * 128], src[:, m, tok], identb[:]),
                                    reads=[skey_, "identb"], writes=[pk[7]], inc=(w_ == 1 and m == 3))
                        yield
                        S.op("pool", lambda e: e.tensor_tensor(out=h3(R2L), in0=cb(trigt_f[:, :]), in1=tkb("g", i), op=ALU.mult),
                             reads=["cb16", "g"], writes=["R2L"])
                        S.op("pool", lambda e: e.tensor_tensor(out=h3(R2Q), in0=cb(tri), in1=tkb("g", i), op=ALU.mult),
                             reads=["cst", "g"], writes=["R2Q"])
                        S.op("dve", lambda e: e.tensor_tensor(out=h3(kd), in0=h3(pT[0:64, 0:512]), in1=tkb("ekd", i),
                                                              op=ALU.mult), reads=[pk[7], "ekd"], writes=["kd" + pp])
                        S.op("dve", lambda e: e.tensor_tensor(out=h3(vb), in0=h3(pT[0:64, 512:1024]), in1=tkb("bet", i),
                                                              op=ALU.mult), reads=[pk[7], "bet"], writes=["vb" + pp])
                        yield
                        S.op("pe", lambda e: e.matmul(pb[3][0:64, :], lhsT=tri_b, rhs=R2L, start=True, stop=False),
                             reads=["cb16", "R2L"], writes=[pk[3]], inc=False)
                        S.op("pe", lambda e: e.matmul(pb[3][0:64, :], lhsT=i64b, rhs=mLs_b, start=False, stop=True),
                             reads=["cb16", "identb"], writes=[pk[3]])
                        S.op("pe", lambda e: e.matmul(pb[2][0:64, :], lhsT=trigt_b, rhs=R2Q, start=True, stop=False),
                             reads=["cb16", "R2Q"], writes=[pk[2]], inc=False)
                        S.op("pe", lambda e: e.matmul(pb[2][0:64, :], lhsT=i64b, rhs=mUi_b, start=False, stop=True),
                             reads=["cb16", "identb"], writes=[pk[2]])
                        S.op("act", lambda e: e.activation(out=decL, in_=pb[3][0:64, :], func=AF.Exp), reads=[pk[3]], writes=["decL"])
                        S.op("act", lambda e: e.activation(out=decQ, in_=pb[2][0:64, :], func=AF.Exp), reads=[pk[2]], writes=["decQ"])
                        yield
                        S.op("pool", lambda e: e.tensor_tensor(out=h3(decL), in0=h3(decL), in1=tkb("nbet", i), op=ALU.mult),
                             reads=["decL", "nbet"], writes=["decL"])
                        S.op("dve", lambda e: e.tensor_tensor(out=qkTb, in0=pb[1][0:64, :], in1=decQ, op=ALU.mult),
                             reads=[pk[1], "decQ"], writes=["qkT" + pp])
                        S.op("dve", lambda e: e.tensor_tensor(out=N0, in0=pb[0][0:64, :], in1=decL, op=ALU.mult),
                             reads=[pk[0], "decL"], writes=["N0"])
                        yield
                        def ptt(lv):
                            return PTT[i % 2][lv].rearrange("p (h w x) -> p h w x", h=8, w=2)
                        def pttk(lv):
                            return "PTT%s_%d" % (pp, lv)
                        def PTK(lv):
                            return [pttk(lv) + x_ for x_ in ("a0", "a1", "b0", "b1")]
                        for h in range(8):
                            hb = slice(h * 64, (h + 1) * 64)
                            S.op("pe", lambda e, hb=hb: e.transpose(pT[0:64, hb], N0[:, hb], i64b),
                                 reads=["N0", "identb"], writes=[pk[7]], inc=(h == 7))
                        S.op("act", lambda e: e.copy(out=ptt(0)[:, :, 0, :], in_=h3(pT[0:64, 0:512])), reads=[pk[7]],
                             writes=[pttk(0) + "a0", pttk(0) + "a1"])
                        S.op("pool", lambda e: e.tensor_copy(out=ptt(0)[:, :, 1, :], in_=cb(i64)), reads=["cst"],
                             writes=[pttk(0) + "b0", pttk(0) + "b1"])
                        yield
                        Pc, pkc = N0, "N0"
                        for j in range(1, 6):
                            cur, nxt = (j - 1) % 2, j % 2
                            Pn = Pb[j % 2]; pkn = "P%d" % (j % 2)
                            for h in range(8):
                                hb = slice(h * 64, (h + 1) * 64)
                                S.op("pe", lambda e, hb=hb, h=h, Pc=Pc: e.matmul(pb[6][0:64, hb], lhsT=ptt(cur)[:, h, 0, :], rhs=Pc[:, hb],
                                                                                start=True, stop=True),
                                     reads=[pkc] + PTK(cur), writes=[pk[6]], inc=(h == 7))
                            for h in range(8):
                                hb = slice(h * 64, (h + 1) * 64)
                                bk_ = 2 + h // 4
                                S.op("pe", lambda e, hb=hb, h=h, Pc=Pc, bk_=bk_: e.matmul(
                                    pb[bk_][0:64, (h % 4) * 128:(h % 4 + 1) * 128], lhsT=Pc[:, hb],
                                    rhs=ptt(cur)[:, h, :, :].rearrange("p w x -> p (w x)"), start=True, stop=True),
                                    reads=[pkc] + PTK(cur), writes=[pk[bk_]], inc=(h % 4 == 3))
                            S.op("act", lambda e, Pn=Pn: e.copy(out=Pn, in_=pb[6][0:64, :]), reads=[pk[6]], writes=[pkn])
                            for hf in range(2):
                                S.op("act", lambda e, hf=hf: e.copy(out=PTT[i % 2][nxt][:, hf * 512:(hf + 1) * 512], in_=pb[2 + hf][0:64, :]),
                                     reads=[pk[2 + hf]], writes=[pttk(nxt) + "a%d" % hf, pttk(nxt) + "b%d" % hf])
                                S.op("pool", lambda e, hf=hf: e.tensor_tensor(
                                    out=ptt(nxt)[:, hf * 4:(hf + 1) * 4, 1, :], in0=ptt(nxt)[:, hf * 4:(hf + 1) * 4, 1, :],
                                    in1=ptt(cur)[:, hf * 4:(hf + 1) * 4, 1, :], op=ALU.add),
                                    reads=[pttk(nxt) + "b%d" % hf] + PTK(cur), writes=[pttk(nxt) + "b%d" % hf])
                            Pc, pkc = Pn, pkn
                            yield
                        for h in range(8):
                            hb = slice(h * 64, (h + 1) * 64)
                            S.op("pe", lambda e, hb=hb, h=h, Pc=Pc: e.matmul(pb[6][0:64, hb], lhsT=Pc[:, hb], rhs=ptt(1)[:, h, 1, :],
                                                                            start=True, stop=True),
                                 reads=[pkc] + PTK(1), writes=[pk[6]], inc=(h == 7))
                        S.op("dve", lambda e: e.tensor_tensor(out=ptt(1)[:, :, 1, :], in0=h3(pb[6][0:64, :]), in1=ptt(1)[:, :, 1, :],
                                                              op=ALU.add), reads=[pk[6]] + PTK(1), writes=[pttk(1) + "b0", pttk(1) + "b1"])
                        yield
                        S.op("pool", lambda e: e.tensor_tensor(
                            out=egm.rearrange("p (h x) -> p h x", h=8),
                            in0=maskrep.rearrange("p (h x) -> p h x", h=8),
                            in1=egl[:, i * 8:(i + 1) * 8].unsqueeze(2).to_broadcast([128, 8, 64]), op=ALU.mult),
                            reads=["cst", "egl"], writes=["egm" + pp])
                        yield

                    def rec(i):
                        pp = str(i % 2)
                        qkTb = qkTb2[i % 2]; kd = kd2[i % 2]; vb = vb2[i % 2]; egm = egm2[i % 2]
                        tok = slice(i * 64, (i + 1) * 64)
                        for m in range(4):
                            mb = slice(m * 128, (m + 1) * 128)
                            S.op("pe", lambda e, m=m, mb=mb: e.matmul(pb[5][0:64, mb], lhsT=kT[:, m, tok], rhs=SBl[:, mb],
                                                                      start=True, stop=True),
                                 reads=["kT", sbkey], writes=[pk[5]], inc=(m == 3))
                        for m in range(4):
                            mb = slice(m * 128, (m + 1) * 128)
                            S.op("pe", lambda e, m=m, mb=mb: e.matmul(pb[4][0:64, mb], lhsT=qT[:, m, tok], rhs=SBl[:, mb],
                                                                      start=True, stop=True),
                                 reads=["qT", sbkey], writes=[pk[4]], inc=(m == 3))
                        yield
                        S.op("dve", lambda e: e.tensor_tensor(out=h3(t_x), in0=h3(pb[5][0:64, :]), in1=tkb("bg", i), op=ALU.mult),
                             reads=[pk[5], "bg"], writes=["t_x"])
                        S.op("dve", lambda e: e.tensor_tensor(out=r_b, in0=vb, in1=t_x, op=ALU.subtract),
                             reads=["vb" + pp, "t_x"], writes=["r_b"])
                        S.op("dve", lambda e: e.tensor_tensor(out=h3(o1), in0=h3(pb[4][0:64, :]), in1=tkb("gam", i), op=ALU.mult),
                             reads=[pk[4], "gam"], writes=["o1"])
                        yield
                        for h in range(8):
                            hb = slice(h * 64, (h + 1) * 64)
                            S.op("pe", lambda e, hb=hb, h=h: e.matmul(
                                pb[5][0:64, hb], lhsT=PTT[i % 2][1].rearrange("p (h w x) -> p h w x", h=8, w=2)[:, h, 1, :],
                                rhs=r_b[:, hb], start=True, stop=True),
                                 reads=["PTT%s_1b0" % pp, "PTT%s_1b1" % pp, "r_b"], writes=[pk[5]], inc=(h == 7))
                        S.op("act", lambda e: e.copy(out=vnew, in_=pb[5][0:64, :]), reads=[pk[5]], writes=["vnew"])
                        yield
                        for m in range(4):
                            mb = slice(m * 128, (m + 1) * 128)
                            S.op("pe", lambda e, mb=mb: e.matmul(pb[4][:, mb], lhsT=kd[:, mb], rhs=vnew[:, mb],
                                                                 start=True, stop=True),
                                 reads=["kd" + pp, "vnew"], writes=[pk[4]], inc=(m == 3))
                        for h in range(8):
                            hb = slice(h * 64, (h + 1) * 64)
                            S.op("pe", lambda e, hb=hb: e.matmul(pb[5][0:64, hb], lhsT=qkTb[:, hb], rhs=vnew[:, hb],
                                                                 start=True, stop=True),
                                 reads=["qkT" + pp, "vnew"], writes=[pk[5]], inc=(h == 7))
                        yield
                        S.op("dve", lambda e: e.tensor_tensor(out=t1, in0=pb[4][:, :], in1=maskrep, op=ALU.mult),
                             reads=[pk[4], "cst"], writes=["t1"])
                        S.op("pool", lambda e: e.tensor_tensor(out=SL[:, :], in0=SL[:, :], in1=egm, op=ALU.mult),
                             reads=[skey, "egm" + pp], writes=[skey])
                        S.op("dve", lambda e: e.tensor_tensor(out=SL[:, :], in0=SL[:, :], in1=t1, op=ALU.add),
                             reads=[skey, "t1"], writes=[skey])
                        S.op("act", lambda e: e.copy(out=SBl[:, :], in_=SL[:, :]), reads=[skey], writes=[sbkey])
                        yield
                        S.op("dve", lambda e: e.tensor_tensor(out=o_tok, in0=o1, in1=pb[5][0:64, :], op=ALU.add),
                             reads=["o1", pk[5]], writes=["o_tok"])
                        S.op("act", lambda e: e.activation(out=osq, in_=o_tok, func=AF.Square), reads=["o_tok"], writes=["osq"])
                        S.op("dve", lambda e: e.tensor_reduce(out=ssr, in_=h3(osq), axis=mybir.AxisListType.X, op=ALU.add),
                             reads=["osq"], writes=["ssr"])
                        S.op("act", lambda e: e.activation(out=rr, in_=ssr, func=AF.Ln, bias=EPS, scale=1.0 / 64),
                             reads=["ssr"], writes=["rr"])
                        S.op("act", lambda e: e.activation(out=rr, in_=rr, func=AF.Exp, scale=-0.5), reads=["rr"], writes=["rr"])
                        S.op("dve", lambda e: e.tensor_tensor(out=h3(on_b), in0=h3(o_tok),
                                                              in1=rr.unsqueeze(2).to_broadcast([64, 8, 64]), op=ALU.mult),
                             reads=["o_tok", "rr"], writes=["on_b"])
                        yield
                        for m in range(4):
                            S.op("pe", lambda e, m=m: e.transpose(pT[:, 512 + m * 64:512 + (m + 1) * 64], on_b[:, m * 128:(m + 1) * 128],
                                                                  i64b),
                                 reads=["on_b", "identb"], writes=[pk[7]], inc=(m == 3))
                        S.op("dve", lambda e: e.scalar_tensor_tensor(
                            out=oT[:, :, tok], in0=pT[:, 512:768].rearrange("p (m c) -> p m c", m=4), scalar=P(l, 146),
                            in1=szT[:, :, tok], op0=ALU.mult, op1=ALU.mult),
                            reads=[pk[7], "szT", "prm"], writes=["oT"])
                        yield

                    for _ in prep(0):
                        pass
                    for i in range(NCH):
                        ga = rec(i)
                        gb = prep(i + 1) if i + 1 < NCH else iter(())
                        da = db = False
                        while not (da and db):
                            if not da:
                                try:
                                    next(ga)
                                except StopIteration:
                                    da = True
                            for _ in range(2):
                                if not db:
                                    try:
                                        next(gb)
                                    except StopIteration:
                                        db = True
                    for dg in range(2):
                        wi, (wO,) = wload([(KD, 512)])
                        for dd in range(4):
                            d = dg * 4 + dd
                            banks = next_banks()
                            proj(banks, lambda k, dd=dd: wO[:, k, dd * 128:(dd + 1) * 128],
                                 lambda k, half: (oT[:, k, half * 512:(half + 1) * 512] if k < 4
                                                  else yT[:, k - 4, half * 512:(half + 1) * 512]),
                                 KD, "w%d" % wi, ["oT", "yT"])
                            for half in range(2):
                                S.op("dve", lambda e, d=d, half=half, b=banks[half]: e.tensor_tensor(
                                    out=xT[:, d, half * 512:(half + 1) * 512], in0=pb[b][:, :],
                                    in1=xT[:, d, half * 512:(half + 1) * 512], op=ALU.add),
                                    reads=[pk[banks[half]], "xT"], writes=["xT"])
                    S.barrier()
                    rmsnorm_to_hT(lambda k: P(l, 8 + k))
                    for g in range(NF // 2):
                        wi, (wV, wG) = wload([(KD, 256)] * 2)
                        for jj in range(2):
                            f = g * 2 + jj
                            C_ = next_set(); n = C_["n"]
                            bV = next_banks()
                            proj(bV, lambda k, jj=jj: wV[:, k, jj * 128:(jj + 1) * 128], hT_rhs, KD, "w%d" % wi, HTK)
                            hal = hal_ff[:, (l * NF + f) * 2:(l * NF + f) * 2 + 2]
                            hkey = "hff%d_%d" % (l, f)
                            halo_in(C_, hal, hkey, 3, first)
                            for half in range(2):
                                S.op("act", lambda e, half=half: e.copy(out=C_["cb"][:, 2 + half * 512:2 + (half + 1) * 512],
                                                                         in_=pb[bV[half]][:, :]),
                                     reads=[pk[bV[half]]], writes=["cb" + n + "h%d" % half])
                            conv(C_, 3, lambda j, f=f: P(l, 76 + f * 3 + j))
                            halo_out(C_, hal, hkey, 3)
                            S.op("act", lambda e: e.activation(out=C_["ac"][:, :], in_=C_["ac"][:, :], func=AF.Silu),
                                 reads=ackeys(C_), writes=ackeys(C_))
                            bG = next_banks()
                            proj(bG, lambda k, jj=jj: wG[:, k, jj * 128:(jj + 1) * 128], hT_rhs, KD, "w%d" % wi, HTK)
                            for half in range(2):
                                S.op("dve", lambda e, f=f, half=half: e.tensor_tensor(
                                    out=actT[:, f, half * 512:(half + 1) * 512], in0=pb[bG[half]][:, :],
                                    in1=C_["ac"][:, half * 512:(half + 1) * 512], op=ALU.mult),
                                    reads=[pk[bG[half]]] + ackeys(C_), writes=["actT"])
                    for d in range(KD):
                        wi, (wD,) = wload([(NF, 128)])
                        banks = next_banks()
                        proj(banks, lambda f: wD[:, f, :], lambda f, half: actT[:, f, half * 512:(half + 1) * 512],
                             NF, "w%d" % wi, ["actT"])
                        for half in range(2):
                            S.op("dve", lambda e, d=d, half=half, b=banks[half]: e.tensor_tensor(
                                out=xT[:, d, half * 512:(half + 1) * 512], in0=pb[b][:, :],
                                in1=xT[:, d, half * 512:(half + 1) * 512], op=ALU.add),
                                reads=[pk[banks[half]], "xT"], writes=["xT"])
                S.barrier()
                rmsnorm_to_hT(lambda k: prm[:, PL * DEPTH + k:PL * DEPTH + k + 1])
                for k in range(KD):
                    S.op("dve", lambda e, k=k: e.scalar_tensor_tensor(
                        out=xT[:, k, :], in0=xT[:, k, :], scalar=prm[:, PL * DEPTH + k:PL * DEPTH + k + 1], in1=rstd[:, :],
                        op0=ALU.mult, op1=ALU.mult), reads=["xT", "rstd0", "rstd1", "prm"] + HTK, writes=["xT"])
                for tb in range(8):
                    st = stage[tb % 2]
                    for dq in range(2):
                        b = 5 + dq
                        for dd in range(4):
                            d = dq * 4 + dd
                            S.op("pe", lambda e, d=d, dd=dd, b=b: e.transpose(
                                pb[b][:, dd * 128:(dd + 1) * 128], xT[:, d, tb * 128:(tb + 1) * 128], ident),
                                reads=["xT", "cst"], writes=[pk[b]], inc=(dd == 3))
                        S.op("act", lambda e, dq=dq, b=b, st=st: e.copy(out=st[:, dq * 512:(dq + 1) * 512], in_=pb[b][:, :]),
                             reads=[pk[b]], writes=["stage%d" % (tb % 2)])
                    S.dma("sp", out_d[row0 + tb * 128: row0 + (tb + 1) * 128, :], st, "so%d" % (tb % 2),
                          reads=["stage%d" % (tb % 2)])
        S.drain_all("sp")
        build.nins = S.nins
    return nc


def make_consts():
    c = np.zeros((128, 128 * 4 + 64 * 4 + 128 + 512), np.float32)
    c[:, 0:128] = np.eye(128)
    c[:, 128:256] = 1.0
    c[0:64, 256:320] = 1.0
    c[64:128, 320:384] = 1.0
    c[63, 384:512] = 1.0
    p = np.arange(64)[:, None]
    f = np.arange(64)[None, :]
    c0 = 512
    c[0:64, c0:c0 + 64] = (p <= f)
    c[0:64, c0 + 64:c0 + 128] = np.where(p > f, 0.0, NEG)
    c[0:64, c0 + 128:c0 + 192] = np.where(f > p, 0.0, NEG)
    c[0:64, c0 + 192:c0 + 256] = np.where(f >= p, 0.0, NEG)
    c1 = c0 + 256
    c[0:64, c1:c1 + 64] = 1.0
    c2 = c1 + 128
    for h in range(8):
        hh = h % 2
        c[hh * 64:(hh + 1) * 64, c2 + h * 64:c2 + (h + 1) * 64] = 1.0
    return c


def make_params(depth, attn_norm, conv_qkv, a_log, dt_bias, head_norm, conv_sc, sc_norm, ffn_norm, conv_ffn, final_norm):
    pr = np.zeros((128, PL * depth + 8), np.float32)
    for l in range(depth):
        o = l * PL
        pr[:, o + 0:o + 8] = attn_norm[l].reshape(8, 128).T
        pr[:, o + 8:o + 16] = ffn_norm[l].reshape(8, 128).T
        pr[:, o + 16:o + 64] = conv_qkv[l].reshape(4, 12, 128).transpose(2, 1, 0).reshape(128, 48)
        pr[:, o + 64:o + 76] = conv_sc[l].reshape(3, 4, 128).transpose(2, 1, 0).reshape(128, 12)
        pr[:, o + 76:o + 142] = conv_ffn[l].reshape(3, NF, 128).transpose(2, 1, 0).reshape(128, 66)
        pr[:, o + 142:o + 146] = sc_norm[l].reshape(4, 128).T
        pr[:, o + 146] = np.tile(head_norm[l], 2)
        pr[:, o + 147:o + 155] = a_log[l][None, :]
        pr[:, o + 155:o + 163] = dt_bias[l][None, :]
    pr[:, PL * depth:PL * depth + 8] = final_norm.reshape(8, 128).T
    return pr


def make_wstream(depth, W):
    tot = layer_stream_cols()
    out = np.empty((depth, 128, tot), np.float32)
    for l in range(depth):
        off = 0
        for job in layer_jobspec():
            for (name, c0, n, K_) in job:
                blk = W[name][l][:, c0:c0 + n].reshape(K_, 128, n).transpose(1, 0, 2).reshape(128, K_ * n)
                out[l, :, off:off + K_ * n] = blk
                off += K_ * n
        assert off == tot
    return out


_cache = {}


def run(x, attn_norm, w_in, conv_qkv, a_log, dt_bias, head_norm, conv_sc, sc_norm, w_out, ffn_norm, w_up,
        conv_ffn, w_down, final_norm, n_cores=8):
    x = np.asarray(x, np.float32)
    B = x.shape[0]
    depth = w_in.shape[0]
    nseq = B // n_cores
    key = (nseq, depth)
    if key not in _cache:
        _cache[key] = build(nseq, depth)
    nc = _cache[key]
    prm = make_params(depth, *(np.asarray(a, np.float32) for a in (attn_norm, conv_qkv, a_log, dt_bias, head_norm,
                                                                     conv_sc, sc_norm, ffn_norm, conv_ffn, final_norm)))
    cst = make_consts()
    shared = {"wst": make_wstream(depth, {"w_in": np.asarray(w_in, np.float32), "w_out": np.asarray(w_out, np.float32),
                                          "w_up": np.asarray(w_up, np.float32), "w_down": np.asarray(w_down, np.float32)}),
              "prm": prm, "cst": cst}
    in_maps = []
    for c in range(n_cores):
        m = dict(shared)
        m["x"] = np.ascontiguousarray(x[c * nseq:(c + 1) * nseq].reshape(nseq * T, D))
        in_maps.append(m)
    res = run_bass_kernel_spmd(nc, in_maps, core_ids=list(range(n_cores)))
    out = np.concatenate([np.asarray(r["out"]).reshape(nseq, T, D) for r in res.results], axis=0)
    if DBG:
        np.save("_dbg.npy", np.asarray(res.results[0]["dbg"]))
    return out.astype(np.float32)


def kernel(x, attn_norm, w_in, conv_qkv, a_log, dt_bias, head_norm, conv_sc, sc_norm, w_out, ffn_norm, w_up,
           conv_ffn, w_down, final_norm):
    return run(x, attn_norm, w_in, conv_qkv, a_log, dt_bias, head_norm, conv_sc, sc_norm, w_out, ffn_norm, w_up,
               conv_ffn, w_down, final_norm, n_cores=8)
```

```python
import numpy as np
from contextlib import ExitStack
import concourse.bass as bass
import concourse.mybir as mybir
from concourse.bass_utils import run_bass_kernel_spmd

F32 = mybir.dt.float32
BF16 = mybir.dt.bfloat16
ALU = mybir.AluOpType
AF = mybir.ActivationFunctionType

D = 1024
KD = 8
T = 2048
TT = 1024
NT = T // TT
NCH = TT // 64
DFF = 2816
NF = 22
EPS = 1e-6
NEG = -30000.0
CQ, CK, CV, CZ, CB, CA, CSB, CSC, CSX = 0, 512, 1024, 1536, 2048, 2056, 2064, 2576, 3088
PL = 163


def layer_jobspec():
    jl = []
    for m in range(4):
        jl.append([("w_in", CSC + m * 128, 128, KD), ("w_in", CSX + m * 128, 128, KD), ("w_in", CSB + m * 128, 128, KD)])
    jl.append([("w_in", CZ, 512, KD)])
    jl.append([("w_in", CB, 16, KD)])
    for c0_ in (CQ, CK, CV):
        jl.append([("w_in", c0_, 512, KD)])
    for dg in range(2):
        jl.append([("w_out", dg * 512, 512, KD)])
    for g in range(NF // 2):
        jl.append([("w_up", g * 256, 256, KD), ("w_up", DFF + g * 256, 256, KD)])
    for d in range(KD):
        jl.append([("w_down", d * 128, 128, NF)])
    return jl


def layer_stream_cols():
    return sum(K_ * n for job in layer_jobspec() for (_, _, n, K_) in job)


class Sched:
    def __init__(self, nc, es):
        self.nc = nc
        self.es = es
        self.eng = {"pe": nc.tensor, "act": nc.scalar, "dve": nc.vector, "pool": nc.gpsimd, "sp": nc.sync}
        self.sem = {n: es.enter_context(nc.semaphore("s_" + n)) for n in self.eng}
        self.cnt = {n: 0 for n in self.eng}
        self.known = {n: {} for n in self.eng}
        self.lastw = {}
        self.readers = {}
        self.dsem = {}
        self.dcnt = {}
        self.pending = {n: False for n in self.eng}
        self.nins = 0

    def _deps(self, reads, writes):
        toks = []
        for k in reads:
            t = self.lastw.get(k)
            if t is not None:
                toks.append(t)
        for k in writes:
            t = self.lastw.get(k)
            if t is not None:
                toks.append(t)
            toks.extend(self.readers.get(k, ()))
        return toks

    def _wait(self, en, toks):
        need = {}
        kn = self.known[en]
        for (s, v) in toks:
            if kn.get(s, 0) >= v:
                continue
            if need.get(s, 0) < v:
                need[s] = v
        for s, v in need.items():
            self.eng[en].wait_ge(s, v)
            kn[s] = v
            self.nins += 1

    def _record(self, tok, reads, writes):
        for k in writes:
            self.lastw[k] = tok
            self.readers[k] = []
        for k in reads:
            self.readers.setdefault(k, []).append(tok)

    def op(self, en, fn, reads=(), writes=(), inc=True):
        toks = self._deps(reads, writes)
        for k in reads:
            if k.startswith("pb"):
                toks.extend(t for t in self.readers.get(k, ()) if t[0] is not self.sem[en])
        self._wait(en, toks)
        ins = fn(self.eng[en])
        s = self.sem[en]
        self.nins += 1
        if inc:
            self.cnt[en] += 1
            ins.then_inc(s, 1)
            tok = (s, self.cnt[en])
            self.pending[en] = False
        else:
            tok = (s, self.cnt[en] + 1)
            self.pending[en] = True
        if en == "pe":
            self.known[en][s] = tok[1]
        self._record(tok, reads, writes)
        return ins

    def dma(self, en, out, in_, slot, reads=(), writes=()):
        if slot not in self.dsem:
            self.dsem[slot] = self.es.enter_context(self.nc.semaphore("d_" + slot))
            self.dcnt[slot] = 0
        self._wait(en, self._deps(reads, writes))
        self.dcnt[slot] += 16
        ins = self.eng[en].dma_start(out=out, in_=in_)
        ins.then_inc(self.dsem[slot], 16)
        self.nins += 1
        tok = (self.dsem[slot], self.dcnt[slot])
        self._record(tok, reads, writes)
        return ins

    def barrier(self):
        for en in self.eng:
            assert not self.pending[en], en
        toks = [(self.sem[o], self.cnt[o]) for o in self.eng if self.cnt[o] > 0]
        toks += [(self.dsem[k], self.dcnt[k]) for k in self.dsem]
        for en in ("pe", "act", "dve", "pool", "sp"):
            self._wait(en, toks)

    def drain_all(self, en):
        toks = [(self.sem[o], self.cnt[o]) for o in self.eng if self.cnt[o] > 0]
        toks += [(self.dsem[k], self.dcnt[k]) for k in self.dsem]
        self._wait(en, toks)


DBG = False


def build(NSEQ, DEPTH):
    nc = bass.Bass("TRN2", target_bir_lowering=False)
    x_d = nc.dram_tensor("x", [NSEQ * T, D], F32, kind="ExternalInput").ap()
    TOTL = layer_stream_cols()
    wst_d = nc.dram_tensor("wst", [DEPTH, 128, TOTL], F32, kind="ExternalInput").ap()
    NPRM = PL * DEPTH + 8
    prm_d = nc.dram_tensor("prm", [128, NPRM], F32, kind="ExternalInput").ap()
    NCST = 128 * 4 + 64 * 4 + 128 + 512
    cst_d = nc.dram_tensor("cst", [128, NCST], F32, kind="ExternalInput").ap()
    out_d = nc.dram_tensor("out", [NSEQ * T, D], F32, kind="ExternalOutput").ap()
    dbg_d = nc.dram_tensor("dbg", [16, 128, 512], F32, kind="ExternalOutput").ap() if DBG else None

    with ExitStack() as es:
        S = Sched(nc, es)

        def sb(name, shape, dt):
            return es.enter_context(nc.sbuf_tensor(name, shape, dt))

        def ps(name, shape, dt):
            return es.enter_context(nc.psum_tensor(name, shape, dt))

        xT = sb("xT", [128, KD, TT], F32)
        arena = sb("arena", [128, 28 * 1024], BF16)
        arenaB = sb("arenaB", [128, 12 * 1024], F32)
        rstd = sb("rstd", [128, TT], F32)
        kqbd_t = sb("kqbd_t", [128, 1024], BF16)
        ctmp = rstd
        HTK = ["hT%d" % k_ for k_ in range(KD)]
        NW = 4
        wbuf = [sb("wbuf%d" % i, [128, 4096], BF16) for i in range(NW)]
        prm = sb("prm_sb", [128, NPRM], F32)
        cst = sb("cst_sb", [128, NCST], F32)
        identb = sb("identb", [128, 128], BF16)
        onesb = sb("onesb", [128, 128], BF16)
        blkb = sb("blkb", [128, 128], BF16)
        Sbd = [sb("Sbd%d" % l, [128, 512], F32) for l in range(DEPTH)]
        Sb = [sb("Sb%d" % l, [128, 512], BF16) for l in range(DEPTH)]
        hal_qkv = sb("hal_qkv", [128, DEPTH * 12 * 3], F32)
        hal_sc = sb("hal_sc", [128, DEPTH * 4 * 2], F32)
        hal_ff = sb("hal_ff", [128, DEPTH * NF * 2], F32)
        tk = {n: sb("tk_" + n, [64, 128], F32) for n in
              ("bet", "nbet", "xg", "g", "gc", "gam", "bg", "ekd", "nega")}
        egl = sb("egl", [128, 128], F32)

        def av(off, n):
            return arena[:, off:off + n]
        qT = av(0, 4096).rearrange("p (m t) -> p m t", m=4)
        kT = av(4096, 4096).rearrange("p (m t) -> p m t", m=4)
        vT = av(8192, 4096).rearrange("p (m t) -> p m t", m=4)
        yT = av(12288, 4096).rearrange("p (m t) -> p m t", m=4)
        oT = av(16384, 4096).rearrange("p (m t) -> p m t", m=4)
        szT = av(20480, 8192).bitcast(F32).rearrange("p (m t) -> p m t", m=4)
        actT = av(0, NF * TT).rearrange("p (f t) -> p f t", f=NF)
        stage = [av(22528 + i * 2048, 2048).bitcast(F32) for i in range(2)]
        hT = arenaB[:, 0:4096].bitcast(BF16).rearrange("p (k t) -> p k t", k=KD)
        def bv(off, n, dt=F32, parts=64):
            a = arenaB[0:parts, off:off + n]
            return a.bitcast(dt) if dt is not F32 else a
        o_ = [0]
        def balloc(n_f32, dt=F32, parts=64):
            a = bv(o_[0], n_f32, dt, parts)
            o_[0] += n_f32
            return a
        decL = balloc(512); decQ = balloc(512)
        R2L = balloc(256, BF16); R2Q = balloc(256, BF16)
        N0 = balloc(256, BF16)
        Pb = [balloc(256, BF16) for _ in range(2)]
        PTT = [[balloc(512, BF16) for _ in range(2)] for _ in range(2)]
        qkTb2 = [balloc(256, BF16) for _ in range(2)]
        kd2 = [balloc(256, BF16) for _ in range(2)]
        vb2 = [balloc(512) for _ in range(2)]
        t_x = balloc(512)
        r_b = balloc(256, BF16)
        vnew = balloc(256, BF16)
        t1 = balloc(512, F32, 128)
        o1 = balloc(512); o_tok = balloc(512); osq = balloc(512)
        on_b = balloc(256, BF16)
        egm2 = [balloc(512, F32, 128) for _ in range(2)]
        ssr = balloc(8); rr = balloc(8)
        kqbd = kqbd_t[:, :]
        assert o_[0] <= 11 * 1024, o_[0]
        CHS = []
        off_ = 4096
        for s_ in range(3):
            cb_ = arenaB[:, off_:off_ + TT + 4]; off_ += TT + 4
            ac_ = arenaB[:, off_:off_ + TT]; off_ += TT
            sq_ = arenaB[:, off_:off_ + TT // 2].bitcast(BF16); off_ += TT // 2
            CHS.append(dict(cb=cb_, ac=ac_, sq=sq_, n="%d" % s_))
        assert off_ <= 12 * 1024
        chn = [0]
        def next_set():
            c_ = CHS[chn[0] % 3]
            chn[0] += 1
            return c_
        bkn = [0]
        def next_banks():
            b_ = ((0, 1), (2, 3), (4, 5))[bkn[0] % 3]
            bkn[0] += 1
            return b_
        pend = []
        def flushA(keep=0):
            for it in pend[:max(0, len(pend) - keep)]:
                if not it[1]:
                    next(it[0])
                    it[1] = True
        def flushB(keep=0):
            while len(pend) > keep:
                it = pend.pop(0)
                if not it[1]:
                    next(it[0])
                for _ in it[0]:
                    pass
        def flush(keep=0):
            flushA(keep)
            flushB(keep)

        pb = [ps("pb%d" % i, [128, 512], F32) for i in range(7)] + [ps("pb7", [128, 1024], BF16)]
        pk = ["pb%d" % i for i in range(8)]

        S.dma("sp", prm[:], prm_d, "prm", writes=["prm"])
        S.dma("sp", cst[:], cst_d, "cst", writes=["cst"])
        ident = cst[:, 0:128]
        ones_f = cst[:, 128:256]
        blk_f = cst[:, 256:384]
        sellast = cst[0:64, 384:512]
        c0 = 512
        tri = cst[0:64, c0:c0 + 64]
        maskLs = cst[0:64, c0 + 64:c0 + 128]
        maskUs = cst[0:64, c0 + 128:c0 + 192]
        maskUi = cst[0:64, c0 + 192:c0 + 256]
        c1 = c0 + 256
        ones64 = cst[0:64, c1:c1 + 64]
        i64 = cst[0:64, 0:64]
        c2 = c1 + 128
        maskrep = cst[:, c2:c2 + 512]
        S.op("act", lambda e: e.copy(out=identb[:], in_=ident), reads=["cst"], writes=["identb"])
        S.op("act", lambda e: e.copy(out=onesb[:], in_=ones_f), reads=["cst"], writes=["onesb"])
        S.op("act", lambda e: e.copy(out=blkb[:], in_=blk_f), reads=["cst"], writes=["blkb"])
        S.op("pool", lambda e: e.memset(kqbd, 0.0), writes=["kqbd"])
        cb16 = sb("cb16", [64, 128 + 1024], BF16)
        trigt_f = sb("trigt_f", [64, 64], F32)
        tri_b = cb16[:, 0:64]
        trigt_b = cb16[:, 64:128]
        mLs_b = cb16[:, 128:640]
        mUi_b = cb16[:, 640:1152]
        S.op("dve", lambda e: e.tensor_scalar(out=trigt_f[:, :], in0=maskLs, scalar1=1.0 / 30000.0, scalar2=1.0,
                                              op0=ALU.mult, op1=ALU.add), reads=["cst"], writes=["cb16"])
        S.op("act", lambda e: e.copy(out=tri_b, in_=tri), reads=["cst"], writes=["cb16"])
        S.op("act", lambda e: e.copy(out=trigt_b, in_=trigt_f[:, :]), reads=["cb16"], writes=["cb16"])
        S.op("act", lambda e: e.copy(out=mLs_b.rearrange("p (h x) -> p h x", h=8),
                                     in_=maskLs.unsqueeze(1).to_broadcast([64, 8, 64])), reads=["cst"], writes=["cb16"])
        S.op("act", lambda e: e.copy(out=mUi_b.rearrange("p (h x) -> p h x", h=8),
                                     in_=maskUi.unsqueeze(1).to_broadcast([64, 8, 64])), reads=["cst"], writes=["cb16"])

        def P(l, off, n=1):
            return prm[:, l * PL + off: l * PL + off + n]

        wjobs = []
        wstate = {"use": 0, "iss": 0}
        LOOK = 2
        def wviews(j):
            i = j % NW
            views = []
            off = 0
            for (K_, n_) in wjobs[j][2]:
                views.append(wbuf[i][:, off:off + K_ * n_].rearrange("p (k n) -> p k n", k=K_))
                off += K_ * n_
            assert off <= 4096
            return i, views, off
        def wissue(j):
            i, views, sz = wviews(j)
            l_, off_, _ = wjobs[j]
            S.dma("pool", wbuf[i][:, 0:sz], wst_d[l_][:, off_:off_ + sz], "w%d" % i, writes=["w%d" % i])
        def wload(shapes):
            j = wstate["use"]
            assert list(shapes) == list(wjobs[j][2]), (shapes, wjobs[j])
            while wstate["iss"] < len(wjobs) and wstate["iss"] <= j + LOOK:
                wissue(wstate["iss"])
                wstate["iss"] += 1
            wstate["use"] += 1
            i, views, _ = wviews(j)
            return i, views

        def proj(banks, lhs_fn, rhs, K_, wkey, rkeys):
            for half in range(2):
                b = banks[half]
                for k in range(K_):
                    S.op("pe", lambda e, k=k, half=half, b=b: e.matmul(
                        pb[b][:, :], lhsT=lhs_fn(k), rhs=rhs(k, half), start=(k == 0), stop=(k == K_ - 1)),
                        reads=[wkey] + rkeys, writes=[pk[b]], inc=(k == K_ - 1))

        def hT_rhs(k, half):
            return hT[:, k, half * 512:(half + 1) * 512]

        def rsqrt_from(psbank, half, out_ap, scale, key_out):
            S.op("act", lambda e: e.activation(out=out_ap, in_=pb[psbank][:, :], func=AF.Ln, bias=EPS, scale=scale),
                 reads=[pk[psbank]], writes=[key_out])
            S.op("act", lambda e: e.activation(out=out_ap, in_=out_ap, func=AF.Exp, scale=-0.5),
                 reads=[key_out], writes=[key_out])

        def rmsnorm_to_hT(gcol_fn):
            for k in range(KD):
                S.op("act", lambda e, k=k: e.activation(out=hT[:, k, :], in_=xT[:, k, :], func=AF.Square),
                     reads=["xT"], writes=["hT%d" % k])
            bk = next_banks()
            for half in range(2):
                for k in range(KD):
                    S.op("pe", lambda e, k=k, half=half: e.matmul(
                        pb[bk[half]][:, :], lhsT=onesb[:], rhs=hT[:, k, half * 512:(half + 1) * 512],
                        start=(k == 0), stop=(k == KD - 1)),
                        reads=["onesb", "hT%d" % k], writes=[pk[bk[half]]], inc=(k == KD - 1))
                rsqrt_from(bk[half], half, rstd[:, half * 512:(half + 1) * 512], 1.0 / D, "rstd%d" % half)
            for k in range(KD):
                S.op("dve", lambda e, k=k: e.scalar_tensor_tensor(
                    out=hT[:, k, :], in0=xT[:, k, :], scalar=gcol_fn(k), in1=rstd[:, :],
                    op0=ALU.mult, op1=ALU.mult), reads=["xT", "rstd0", "rstd1", "prm"], writes=["hT%d" % k])

        def conv(C_, K_, wcol_fn):
            n = C_["n"]
            sp = 896 if K_ == 4 else 768
            wp = TT - sp
            rk = ["cb" + n + "h0", "cb" + n + "h1", "cb" + n + "hal", "prm"]
            hk = ["ac" + n + "h0", "ac" + n + "h1"]
            S.op("dve", lambda e: e.tensor_scalar(out=C_["ac"][:, 0:sp], in0=C_["cb"][:, 0:sp],
                                                  scalar1=wcol_fn(0), scalar2=None, op0=ALU.mult),
                 reads=rk, writes=["ac" + n + "c0"] + hk)
            for j in range(1, K_):
                S.op("dve", lambda e, j=j: e.scalar_tensor_tensor(
                    out=C_["ac"][:, 0:sp], in0=C_["cb"][:, j:j + sp], scalar=wcol_fn(j),
                    in1=C_["ac"][:, 0:sp], op0=ALU.mult, op1=ALU.add), reads=rk, writes=["ac" + n + "c0"])
            tmp = C_["sq"].bitcast(F32)[:, 0:wp]
            S.op("pool", lambda e: e.tensor_tensor(out=C_["ac"][:, sp:TT], in0=C_["cb"][:, sp:TT],
                                                   in1=wcol_fn(0).to_broadcast([128, wp]), op=ALU.mult),
                 reads=rk, writes=["ac" + n + "c1"] + hk)
            for j in range(1, K_):
                S.op("pool", lambda e, j=j: e.tensor_tensor(out=tmp, in0=C_["cb"][:, sp + j:TT + j],
                                                            in1=wcol_fn(j).to_broadcast([128, wp]), op=ALU.mult),
                     reads=rk, writes=["sq" + n])
                S.op("pool", lambda e: e.tensor_tensor(out=C_["ac"][:, sp:TT], in0=C_["ac"][:, sp:TT], in1=tmp,
                                                       op=ALU.add), reads=["sq" + n], writes=["ac" + n + "c1"])

        def ackeys(C_):
            return ["ac" + C_["n"] + "h0", "ac" + C_["n"] + "h1", "ac" + C_["n"] + "c0", "ac" + C_["n"] + "c1"]

        def cbkeys(C_):
            return ["cb" + C_["n"] + "h0", "cb" + C_["n"] + "h1", "cb" + C_["n"] + "hal"]

        def halo_in(C_, hal_ap, hkey, K_, first):
            n = C_["n"]
            if first:
                S.op("pool", lambda e: e.memset(C_["cb"][:, 0:K_ - 1], 0.0), writes=["cb" + n + "hal"])
            else:
                S.op("pool", lambda e: e.tensor_copy(out=C_["cb"][:, 0:K_ - 1], in_=hal_ap), reads=[hkey],
                     writes=["cb" + n + "hal"])

        def halo_out(C_, hal_ap, hkey, K_):
            n = C_["n"]
            S.op("pool", lambda e: e.tensor_copy(out=hal_ap, in_=C_["cb"][:, TT:TT + K_ - 1]), reads=["cb" + n + "h1"],
                 writes=[hkey])

        for _s in range(NSEQ):
            for _t in range(NT):
                for l in range(DEPTH):
                    off_ = 0
                    for job in layer_jobspec():
                        shp = [(K_, n) for (_, _, n, K_) in job]
                        wjobs.append((l, off_, shp))
                        off_ += sum(K_ * n for (K_, n) in shp)

        for sq_i in range(NSEQ):
            for ti in range(NT):
                first = (ti == 0)
                row0 = sq_i * T + ti * TT
                S.barrier()
                for tb in range(8):
                    st = stage[tb % 2]
                    S.dma("sp", st, x_d[row0 + tb * 128: row0 + (tb + 1) * 128, :], "st%d" % (tb % 2),
                          writes=["stage%d" % (tb % 2)])
                    for dq in range(2):
                        b = 5 + dq
                        for dd in range(4):
                            d = dq * 4 + dd
                            S.op("pe", lambda e, d=d, dd=dd, b=b, st=st: e.transpose(
                                pb[b][:, dd * 128:(dd + 1) * 128], st[:, d * 128:(d + 1) * 128], ident),
                                reads=["stage%d" % (tb % 2), "cst"], writes=[pk[b]], inc=(dd == 3))
                        S.op("act", lambda e, dq=dq, b=b, tb=tb: e.copy(
                            out=xT[:, dq * 4:(dq + 1) * 4, tb * 128:(tb + 1) * 128],
                            in_=pb[b][:, :].rearrange("p (d t) -> p d t", d=4)),
                            reads=[pk[b]], writes=["xT"])
                if first:
                    for l in range(DEPTH):
                        S.op("pool", lambda e, l=l: e.memset(Sbd[l][:], 0.0), writes=["Sbd%d" % l])
                        S.op("pool", lambda e, l=l: e.memset(Sb[l][:], 0.0), writes=["Sb%d" % l])

                for l in range(DEPTH):
                    S.barrier()
                    rmsnorm_to_hT(lambda k: P(l, 0 + k))
                    for m in range(4):
                        wi, (wC, wX, wB) = wload([(KD, 128)] * 3)
                        wkey = "w%d" % wi
                        C_ = next_set(); n = C_["n"]
                        bC = next_banks()
                        proj(bC, lambda k: wC[:, k, :], hT_rhs, KD, wkey, HTK)
                        for half in range(2):
                            S.op("act", lambda e, half=half: e.copy(out=C_["ac"][:, half * 512:(half + 1) * 512],
                                                                     in_=pb[bC[half]][:, :]),
                                 reads=[pk[bC[half]]], writes=["ac" + n + "h%d" % half])
                        flushA(keep=1)
                        bX = next_banks()
                        proj(bX, lambda k: wX[:, k, :], hT_rhs, KD, wkey, HTK)
                        hal = hal_sc[:, (l * 4 + m) * 2:(l * 4 + m) * 2 + 2]
                        hkey = "hsc%d_%d" % (l, m)
                        halo_in(C_, hal, hkey, 3, first)
                        for half in range(2):
                            S.op("dve", lambda e, half=half: e.tensor_tensor(
                                out=C_["cb"][:, 2 + half * 512:2 + (half + 1) * 512], in0=pb[bX[half]][:, :],
                                in1=C_["ac"][:, half * 512:(half + 1) * 512], op=ALU.mult),
                                reads=[pk[bX[half]], "ac" + n + "h%d" % half], writes=["cb" + n + "h%d" % half])
                        conv(C_, 3, lambda j: P(l, 64 + m * 3 + j))
                        halo_out(C_, hal, hkey, 3)
                        bB = next_banks()
                        proj(bB, lambda k: wB[:, k, :], hT_rhs, KD, wkey, HTK)
                        for half in range(2):
                            S.op("dve", lambda e, half=half: e.tensor_tensor(
                                out=C_["cb"][:, half * 512:(half + 1) * 512], in0=pb[bB[half]][:, :],
                                in1=C_["ac"][:, half * 512:(half + 1) * 512], op=ALU.mult),
                                reads=[pk[bB[half]]] + ackeys(C_) + cbkeys(C_), writes=cbkeys(C_))
                        S.op("pool", lambda e: e.tensor_tensor(out=C_["sq"][:, :], in0=C_["cb"][:, 0:TT], in1=C_["cb"][:, 0:TT],
                                                               op=ALU.mult), reads=cbkeys(C_), writes=["sq" + n])
                        flush(keep=1)
                        def st2(C_=C_, n=n, m=m):
                            bS = next_banks()
                            for half in range(2):
                                S.op("pe", lambda e, half=half: e.matmul(pb[bS[half]][:, :], lhsT=blkb[:],
                                                                          rhs=C_["sq"][:, half * 512:(half + 1) * 512],
                                                                          start=True, stop=True),
                                     reads=["blkb", "sq" + n], writes=[pk[bS[half]]])
                                rsqrt_from(bS[half], half, C_["ac"][:, half * 512:(half + 1) * 512], 1.0 / 64, "ac" + n + "h%d" % half)
                            yield
                            S.op("dve", lambda e: e.scalar_tensor_tensor(
                                out=yT[:, m, :], in0=C_["cb"][:, 0:TT], scalar=P(l, 142 + m), in1=C_["ac"][:, :],
                                op0=ALU.mult, op1=ALU.mult), reads=cbkeys(C_) + ackeys(C_) + ["prm"], writes=["yT"])
                        pend.append([st2(), False])
                    wi, (wZ,) = wload([(KD, 512)])
                    for m in range(4):
                        bZ = next_banks()
                        proj(bZ, lambda k, m=m: wZ[:, k, m * 128:(m + 1) * 128], hT_rhs, KD, "w%d" % wi, HTK)
                        for half in range(2):
                            S.op("act", lambda e, half=half, m=m: e.activation(
                                out=szT[:, m, half * 512:(half + 1) * 512], in_=pb[bZ[half]][:, :], func=AF.Silu),
                                reads=[pk[bZ[half]]], writes=["szT"])
                        flush()
                    wi, (wBA,) = wload([(KD, 16)])
                    for i in range(NCH):
                        for k in range(KD):
                            S.op("pe", lambda e, i=i, k=k: e.matmul(
                                pb[4][0:64, i * 16:(i + 1) * 16], lhsT=hT[:, k, i * 64:(i + 1) * 64], rhs=wBA[:, k, :],
                                start=(k == 0), stop=(k == KD - 1)),
                                reads=["w%d" % wi] + HTK, writes=[pk[4]], inc=(k == KD - 1))
                    bav = pb[4][0:64, 0:256].rearrange("p (i c) -> p i c", c=16)
                    def tv(n):
                        return tk[n][:, :].rearrange("p (i h) -> p i h", h=8)
                    S.op("act", lambda e: e.activation(out=tv("bet"), in_=bav[:, :, 0:8], func=AF.Sigmoid),
                         reads=[pk[4]], writes=["bet"])
                    S.op("dve", lambda e: e.tensor_scalar(out=tk["nbet"][:, :], in0=tk["bet"][:, :], scalar1=-1.0, scalar2=None,
                                                          op0=ALU.mult), reads=["bet"], writes=["nbet"])
                    S.op("dve", lambda e: e.tensor_tensor(
                        out=tv("xg"), in0=bav[:, :, 8:16],
                        in1=prm[0:64, l * PL + 155:l * PL + 163].unsqueeze(1).to_broadcast([64, NCH, 8]), op=ALU.add),
                        reads=[pk[4], "prm"], writes=["xg"])
                    S.op("act", lambda e: e.activation(out=tk["xg"][:, :], in_=tk["xg"][:, :], func=AF.Exp),
                         reads=["xg"], writes=["xg"])
                    S.op("act", lambda e: e.activation(out=tk["xg"][:, :], in_=tk["xg"][:, :], func=AF.Ln, bias=1.0),
                         reads=["xg"], writes=["xg"])
                    S.op("act", lambda e: e.activation(out=tk["nega"][:, 0:8], in_=prm[0:64, l * PL + 147:l * PL + 155],
                                                       func=AF.Exp), reads=["prm"], writes=["nega"])
                    S.op("dve", lambda e: e.scalar_tensor_tensor(
                        out=tv("g"), in0=tv("xg"), scalar=-1.0,
                        in1=tk["nega"][:, 0:8].unsqueeze(1).to_broadcast([64, NCH, 8]), op0=ALU.mult, op1=ALU.mult),
                        reads=["xg", "nega"], writes=["g"])
                    S.op("pe", lambda e: e.matmul(pb[5][0:64, 0:128], lhsT=tri, rhs=tk["g"][:, :], start=True, stop=True),
                         reads=["cst", "g"], writes=[pk[5]])
                    S.op("act", lambda e: e.copy(out=tk["gc"][:, :], in_=pb[5][0:64, 0:128]), reads=[pk[5]], writes=["gc"])
                    S.op("pe", lambda e: e.matmul(pb[4][:, 0:128], lhsT=sellast, rhs=tk["gc"][:, :], start=True, stop=True),
                         reads=["cst", "gc"], writes=[pk[4]])
                    S.op("act", lambda e: e.activation(out=egl[:, :], in_=pb[4][:, 0:128], func=AF.Exp),
                         reads=[pk[4]], writes=["egl"])
                    S.op("dve", lambda e: e.tensor_tensor(out=tk["ekd"][:, :], in0=pb[4][0:64, 0:128], in1=tk["gc"][:, :],
                                                          op=ALU.subtract), reads=[pk[4], "gc"], writes=["ekd"])
                    S.op("act", lambda e: e.activation(out=tk["ekd"][:, :], in_=tk["ekd"][:, :], func=AF.Exp),
                         reads=["ekd"], writes=["ekd"])
                    S.op("act", lambda e: e.activation(out=tk["gam"][:, :], in_=tk["gc"][:, :], func=AF.Exp),
                         reads=["gc"], writes=["gam"])
                    S.op("dve", lambda e: e.tensor_tensor(out=tk["bg"][:, :], in0=tk["bet"][:, :], in1=tk["gam"][:, :],
                                                          op=ALU.mult), reads=["bet", "gam"], writes=["bg"])
                    for ty, (c0_, dst) in enumerate(((CQ, qT), (CK, kT), (CV, vT))):
                        wi, (wQ,) = wload([(KD, 512)])
                        for m in range(4):
                            ch = ty * 4 + m
                            C_ = next_set(); n = C_["n"]
                            bQ = next_banks()
                            proj(bQ, lambda k, m=m: wQ[:, k, m * 128:(m + 1) * 128], hT_rhs, KD, "w%d" % wi, HTK)
                            hal = hal_qkv[:, (l * 12 + ch) * 3:(l * 12 + ch) * 3 + 3]
                            hkey = "hqkv%d_%d" % (l, ch)
                            halo_in(C_, hal, hkey, 4, first)
                            for half, en in ((0, "act"), (1, "act")):
                                if en == "act":
                                    S.op("act", lambda e, half=half: e.copy(
                                        out=C_["cb"][:, 3 + half * 512:3 + (half + 1) * 512], in_=pb[bQ[half]][:, :]),
                                        reads=[pk[bQ[half]]], writes=["cb" + n + "h%d" % half])
                                else:
                                    S.op("dve", lambda e, half=half: e.tensor_copy(
                                        out=C_["cb"][:, 3 + half * 512:3 + (half + 1) * 512], in_=pb[bQ[half]][:, :]),
                                        reads=[pk[bQ[half]]], writes=["cb" + n + "h%d" % half])
                            flushA(keep=1 if ty < 2 else 0)
                            conv(C_, 4, lambda j, ch=ch: P(l, 16 + ch * 4 + j))
                            halo_out(C_, hal, hkey, 4)
                            if ty == 2:
                                S.op("act", lambda e, m=m: e.activation(out=vT[:, m, :], in_=C_["ac"][:, :], func=AF.Silu),
                                     reads=ackeys(C_), writes=["vT"])
                                flush()
                            else:
                                S.op("act", lambda e: e.activation(out=C_["ac"][:, :], in_=C_["ac"][:, :], func=AF.Silu),
                                     reads=ackeys(C_), writes=ackeys(C_))
                                S.op("pool", lambda e: e.tensor_tensor(out=C_["sq"][:, :], in0=C_["ac"][:, :], in1=C_["ac"][:, :],
                                                                       op=ALU.mult), reads=ackeys(C_), writes=["sq" + n])
                                flush(keep=1)
                                def st2(C_=C_, n=n, m=m, ty=ty):
                                    bS = next_banks()
                                    for half in range(2):
                                        S.op("pe", lambda e, half=half: e.matmul(pb[bS[half]][:, :], lhsT=blkb[:],
                                                                                  rhs=C_["sq"][:, half * 512:(half + 1) * 512],
                                                                                  start=True, stop=True),
                                             reads=["blkb", "sq" + n], writes=[pk[bS[half]]])
                                        rsqrt_from(bS[half], half, C_["cb"][:, half * 512:(half + 1) * 512], 1.0,
                                                   "cb" + n + "h%d" % half)
                                    yield
                                    if ty == 0:
                                        S.op("dve", lambda e: e.scalar_tensor_tensor(
                                            out=qT[:, m, :], in0=C_["ac"][:, :], scalar=0.125, in1=C_["cb"][:, 0:TT],
                                            op0=ALU.mult, op1=ALU.mult), reads=ackeys(C_) + cbkeys(C_), writes=["qT"])
                                    else:
                                        S.op("dve", lambda e: e.tensor_tensor(
                                            out=kT[:, m, :], in0=C_["ac"][:, :], in1=C_["cb"][:, 0:TT], op=ALU.mult),
                                            reads=ackeys(C_) + cbkeys(C_), writes=["kT"])
                                pend.append([st2(), False])
                    flush()
                    S.barrier()
                    SL = Sbd[l]; SBl = Sb[l]; skey = "Sbd%d" % l; sbkey = "Sb%d" % l
                    def h3(ap):
                        return ap.rearrange("p (h x) -> p h x", h=8)
                    def tkb(n, i, parts=64):
                        return tk[n][0:parts, i * 8:(i + 1) * 8].unsqueeze(2).to_broadcast([parts, 8, 64])
                    def cb(ap):
                        return ap.unsqueeze(1).to_broadcast([64, 8, 64])
                    i64b = identb[0:64, 0:64]
                    pT = pb[7][:, :]

                    def prep(i):
                        pp = str(i % 2)
                        qkTb = qkTb2[i % 2]; kd = kd2[i % 2]; vb = vb2[i % 2]; egm = egm2[i % 2]
                        tok = slice(i * 64, (i + 1) * 64)
                        kq5 = kqbd.rearrange("p (w m hh x) -> p w m hh x", w=2, m=4, hh=2)
                        for w_, src, skey_ in ((0, kT, "kT"), (1, qT, "qT")):
                            for hh in range(2):
                                S.op("pool", lambda e, w_=w_, src=src, hh=hh: e.tensor_copy(
                                    out=kq5[hh * 64:(hh + 1) * 64, w_, :, hh, :], in_=src[hh * 64:(hh + 1) * 64, :, tok]),
                                    reads=[skey_], writes=["kqbd"])
                        kq3 = kqbd.rearrange("p (w m n) -> p w m n", w=2, m=4)
                        for w_ in range(2):
                            for m in range(4):
                                S.op("pe", lambda e, w_=w_, m=m: e.matmul(
                                    pb[w_][0:64, m * 128:(m + 1) * 128], lhsT=kT[:, m, tok], rhs=kq3[:, w_, m, :],
                                    start=True, stop=True), reads=["kT", "kqbd"], writes=[pk[w_]], inc=(m == 3))
                        for w_, src, skey_ in ((0, kT, "kT"), (1, vT, "vT")):
                            for m in range(4):
                                S.op("pe", lambda e, w_=w_, m=m, src=src: e.transpose(
                                    pT[0:64, w_ * 512 + m * 128: w_ * 512 + (m + 1) * 128], src[:, m, tok], identb[:]),
                                    reads=[skey_, "identb"], writes=[pk[7]], inc=(w_ == 1 and m == 3))
                        yield
                        S.op("pool", lambda e: e.tensor_tensor(out=h3(R2L), in0=cb(trigt_f[:, :]), in1=tkb("g", i), op=ALU.mult),
                             reads=["cb16", "g"], writes=["R2L"])
                        S.op("pool", lambda e: e.tensor_tensor(out=h3(R2Q), in0=cb(tri), in1=tkb("g", i), op=ALU.mult),
                             reads=["cst", "g"], writes=["R2Q"])
                        S.op("dve", lambda e: e.tensor_tensor(out=h3(kd), in0=h3(pT[0:64, 0:512]), in1=tkb("ekd", i),
                                                              op=ALU.mult), reads=[pk[7], "ekd"], writes=["kd" + pp])
                        S.op("dve", lambda e: e.tensor_tensor(out=h3(vb), in0=h3(pT[0:64, 512:1024]), in1=tkb("bet", i),
                                                              op=ALU.mult), reads=[pk[7], "bet"], writes=["vb" + pp])
                        yield
                        S.op("pe", lambda e: e.matmul(pb[3][0:64, :], lhsT=tri_b, rhs=R2L, start=True, stop=False),
                             reads=["cb16", "R2L"], writes=[pk[3]], inc=False)
                        S.op("pe", lambda e: e.matmul(pb[3][0:64, :], lhsT=i64b, rhs=mLs_b, start=False, stop=True),
                             reads=["cb16", "identb"], writes=[pk[3]])
                        S.op("pe", lambda e: e.matmul(pb[2][0:64, :], lhsT=trigt_b, rhs=R2Q, start=True, stop=False),
                             reads=["cb16", "R2Q"], writes=[pk[2]], inc=False)
                        S.op("pe", lambda e: e.matmul(pb[2][0:64, :], lhsT=i64b, rhs=mUi_b, start=False, stop=True),
                             reads=["cb16", "identb"], writes=[pk[2]])
                        S.op("act", lambda e: e.activation(out=decL, in_=pb[3][0:64, :], func=AF.Exp), reads=[pk[3]], writes=["decL"])
                        S.op("act", lambda e: e.activation(out=decQ, in_=pb[2][0:64, :], func=AF.Exp), reads=[pk[2]], writes=["decQ"])
                        yield
                        S.op("pool", lambda e: e.tensor_tensor(out=h3(decL), in0=h3(decL), in1=tkb("nbet", i), op=ALU.mult),
                             reads=["decL", "nbet"], writes=["decL"])
                        S.op("dve", lambda e: e.tensor_tensor(out=qkTb, in0=pb[1][0:64, :], in1=decQ, op=ALU.mult),
                             reads=[pk[1], "decQ"], writes=["qkT" + pp])
                        S.op("dve", lambda e: e.tensor_tensor(out=N0, in0=pb[0][0:64, :], in1=decL, op=ALU.mult),
                             reads=[pk[0], "decL"], writes=["N0"])
                        yield
                        def ptt(lv):
                            return PTT[i % 2][lv].rearrange("p (h w x) -> p h w x", h=8, w=2)
                        def pttk(lv):
                            return "PTT%s_%d" % (pp, lv)
                        def PTK(lv):
                            return [pttk(lv) + x_ for x_ in ("a0", "a1", "b0", "b1")]
                        for h in range(8):
                            hb = slice(h * 64, (h + 1) * 64)
                            S.op("pe", lambda e, hb=hb: e.transpose(pT[0:64, hb], N0[:, hb], i64b),
                                 reads=["N0", "identb"], writes=[pk[7]], inc=(h == 7))
                        S.op("act", lambda e: e.copy(out=ptt(0)[:, :, 0, :], in_=h3(pT[0:64, 0:512])), reads=[pk[7]],
                             writes=[pttk(0) + "a0", pttk(0) + "a1"])
                        S.op("pool", lambda e: e.tensor_copy(out=ptt(0)[:, :, 1, :], in_=cb(i64)), reads=["cst"],
                             writes=[pttk(0) + "b0", pttk(0) + "b1"])
                        yield
                        Pc, pkc = N0, "N0"
                        for j in range(1, 6):
                            cur, nxt = (j - 1) % 2, j % 2
                            Pn = Pb[j % 2]; pkn = "P%d" % (j % 2)
                            for h in range(8):
                                hb = slice(h * 64, (h + 1) * 64)
                                S.op("pe", lambda e, hb=hb, h=h, Pc=Pc: e.matmul(pb[6][0:64, hb], lhsT=ptt(cur)[:, h, 0, :], rhs=Pc[:, hb],
                                                                                start=True, stop=True),
                                     reads=[pkc] + PTK(cur), writes=[pk[6]], inc=(h == 7))
                            for h in range(8):
                                hb = slice(h * 64, (h + 1) * 64)
                                bk_ = 2 + h // 4
                                S.op("pe", lambda e, hb=hb, h=h, Pc=Pc, bk_=bk_: e.matmul(
                                    pb[bk_][0:64, (h % 4) * 128:(h % 4 + 1) * 128], lhsT=Pc[:, hb],
                                    rhs=ptt(cur)[:, h, :, :].rearrange("p w x -> p (w x)"), start=True, stop=True),
                                    reads=[pkc] + PTK(cur), writes=[pk[bk_]], inc=(h % 4 == 3))
                            S.op("act", lambda e, Pn=Pn: e.copy(out=Pn, in_=pb[6][0:64, :]), reads=[pk[6]], writes=[pkn])
                            for hf in range(2):
                                S.op("act", lambda e, hf=hf: e.copy(out=PTT[i % 2][nxt][:, hf * 512:(hf + 1) * 512], in_=pb[2 + hf][0:64, :]),
                                     reads=[pk[2 + hf]], writes=[pttk(nxt) + "a%d" % hf, pttk(nxt) + "b%d" % hf])
                                S.op("dve", lambda e, hf=hf: e.tensor_tensor(
                                    out=ptt(nxt)[:, hf * 4:(hf + 1) * 4, 1, :], in0=ptt(nxt)[:, hf * 4:(hf + 1) * 4, 1, :],
                                    in1=ptt(cur)[:, hf * 4:(hf + 1) * 4, 1, :], op=ALU.add),
                                    reads=[pttk(nxt) + "b%d" % hf] + PTK(cur), writes=[pttk(nxt) + "b%d" % hf])
                            Pc, pkc = Pn, pkn
                            yield
                        for h in range(8):
                            hb = slice(h * 64, (h + 1) * 64)
                            S.op("pe", lambda e, hb=hb, h=h, Pc=Pc: e.matmul(pb[6][0:64, hb], lhsT=Pc[:, hb], rhs=ptt(1)[:, h, 1, :],
                                                                            start=True, stop=True),
                                 reads=[pkc] + PTK(1), writes=[pk[6]], inc=(h == 7))
                        S.op("dve", lambda e: e.tensor_tensor(out=ptt(1)[:, :, 1, :], in0=h3(pb[6][0:64, :]), in1=ptt(1)[:, :, 1, :],
                                                              op=ALU.add), reads=[pk[6]] + PTK(1), writes=[pttk(1) + "b0", pttk(1) + "b1"])
                        yield
                        S.op("pool", lambda e: e.tensor_tensor(
                            out=egm.rearrange("p (h x) -> p h x", h=8),
                            in0=maskrep.rearrange("p (h x) -> p h x", h=8),
                            in1=egl[:, i * 8:(i + 1) * 8].unsqueeze(2).to_broadcast([128, 8, 64]), op=ALU.mult),
                            reads=["cst", "egl"], writes=["egm" + pp])
                        yield

                    def rec(i):
                        pp = str(i % 2)
                        qkTb = qkTb2[i % 2]; kd = kd2[i % 2]; vb = vb2[i % 2]; egm = egm2[i % 2]
                        tok = slice(i * 64, (i + 1) * 64)
                        for m in range(4):
                            mb = slice(m * 128, (m + 1) * 128)
                            S.op("pe", lambda e, m=m, mb=mb: e.matmul(pb[5][0:64, mb], lhsT=kT[:, m, tok], rhs=SBl[:, mb],
                                                                      start=True, stop=True),
                                 reads=["kT", sbkey], writes=[pk[5]], inc=(m == 3))
                        for m in range(4):
                            mb = slice(m * 128, (m + 1) * 128)
                            S.op("pe", lambda e, m=m, mb=mb: e.matmul(pb[4][0:64, mb], lhsT=qT[:, m, tok], rhs=SBl[:, mb],
                                                                      start=True, stop=True),
                                 reads=["qT", sbkey], writes=[pk[4]], inc=(m == 3))
                        yield
                        S.op("dve", lambda e: e.tensor_tensor(out=h3(t_x), in0=h3(pb[5][0:64, :]), in1=tkb("bg", i), op=ALU.mult),
                             reads=[pk[5], "bg"], writes=["t_x"])
                        S.op("dve", lambda e: e.tensor_tensor(out=r_b, in0=vb, in1=t_x, op=ALU.subtract),
                             reads=["vb" + pp, "t_x"], writes=["r_b"])
                        S.op("dve", lambda e: e.tensor_tensor(out=h3(o1), in0=h3(pb[4][0:64, :]), in1=tkb("gam", i), op=ALU.mult),
                             reads=[pk[4], "gam"], writes=["o1"])
                        yield
                        for h in range(8):
                            hb = slice(h * 64, (h + 1) * 64)
                            S.op("pe", lambda e, hb=hb, h=h: e.matmul(
                                pb[5][0:64, hb], lhsT=PTT[i % 2][1].rearrange("p (h w x) -> p h w x", h=8, w=2)[:, h, 1, :],
                                rhs=r_b[:, hb], start=True, stop=True),
                                 reads=["PTT%s_1b0" % pp, "PTT%s_1b1" % pp, "r_b"], writes=[pk[5]], inc=(h == 7))
                        S.op("act", lambda e: e.copy(out=vnew, in_=pb[5][0:64, :]), reads=[pk[5]], writes=["vnew"])
                        yield
                        for m in range(4):
                            mb = slice(m * 128, (m + 1) * 128)
                            S.op("pe", lambda e, mb=mb: e.matmul(pb[4][:, mb], lhsT=kd[:, mb], rhs=vnew[:, mb],
                                                                 start=True, stop=True),
                                 reads=["kd" + pp, "vnew"], writes=[pk[4]], inc=(m == 3))
                        for h in range(8):
                            hb = slice(h * 64, (h + 1) * 64)
                            S.op("pe", lambda e, hb=hb: e.matmul(pb[5][0:64, hb], lhsT=qkTb[:, hb], rhs=vnew[:, hb],
                                                                 start=True, stop=True),
                                 reads=["qkT" + pp, "vnew"], writes=[pk[5]], inc=(h == 7))
                        yield
                        S.op("dve", lambda e: e.tensor_tensor(out=t1, in0=pb[4][:, :], in1=maskrep, op=ALU.mult),
                             reads=[pk[4], "cst"], writes=["t1"])
                        S.op("pool", lambda e: e.tensor_tensor(out=SL[:, :], in0=SL[:, :], in1=egm, op=ALU.mult),
                             reads=[skey, "egm" + pp], writes=[skey])
                        S.op("dve", lambda e: e.tensor_tensor(out=SL[:, :], in0=SL[:, :], in1=t1, op=ALU.add),
                             reads=[skey, "t1"], writes=[skey])
                        S.op("act", lambda e: e.copy(out=SBl[:, :], in_=SL[:, :]), reads=[skey], writes=[sbkey])
                        yield
                        S.op("dve", lambda e: e.tensor_tensor(out=o_tok, in0=o1, in1=pb[5][0:64, :], op=ALU.add),
                             reads=["o1", pk[5]], writes=["o_tok"])
                        S.op("act", lambda e: e.activation(out=osq, in_=o_tok, func=AF.Square), reads=["o_tok"], writes=["osq"])
                        S.op("dve", lambda e: e.tensor_reduce(out=ssr, in_=h3(osq), axis=mybir.AxisListType.X, op=ALU.add),
                             reads=["osq"], writes=["ssr"])
                        S.op("act", lambda e: e.activation(out=rr, in_=ssr, func=AF.Ln, bias=EPS, scale=1.0 / 64),
                             reads=["ssr"], writes=["rr"])
                        S.op("act", lambda e: e.activation(out=rr, in_=rr, func=AF.Exp, scale=-0.5), reads=["rr"], writes=["rr"])
                        S.op("dve", lambda e: e.tensor_tensor(out=h3(on_b), in0=h3(o_tok),
                                                              in1=rr.unsqueeze(2).to_broadcast([64, 8, 64]), op=ALU.mult),
                             reads=["o_tok", "rr"], writes=["on_b"])
                        yield
                        for m in range(4):
                            S.op("pe", lambda e, m=m: e.transpose(pT[:, 512 + m * 64:512 + (m + 1) * 64], on_b[:, m * 128:(m + 1) * 128],
                                                                  i64b),
                                 reads=["on_b", "identb"], writes=[pk[7]], inc=(m == 3))
                        S.op("dve", lambda e: e.scalar_tensor_tensor(
                            out=oT[:, :, tok], in0=pT[:, 512:768].rearrange("p (m c) -> p m c", m=4), scalar=P(l, 146),
                            in1=szT[:, :, tok], op0=ALU.mult, op1=ALU.mult),
                            reads=[pk[7], "szT", "prm"], writes=["oT"])
                        yield

                    for _ in prep(0):
                        pass
                    for i in range(NCH):
                        ga = rec(i)
                        gb = prep(i + 1) if i + 1 < NCH else iter(())
                        da = db = False
                        while not (da and db):
                            if not da:
                                try:
                                    next(ga)
                                except StopIteration:
                                    da = True
                            for _ in range(2):
                                if not db:
                                    try:
                                        next(gb)
                                    except StopIteration:
                                        db = True
                    for dg in range(2):
                        wi, (wO,) = wload([(KD, 512)])
                        for dd in range(4):
                            d = dg * 4 + dd
                            banks = next_banks()
                            proj(banks, lambda k, dd=dd: wO[:, k, dd * 128:(dd + 1) * 128],
                                 lambda k, half: (oT[:, k, half * 512:(half + 1) * 512] if k < 4
                                                  else yT[:, k - 4, half * 512:(half + 1) * 512]),
                                 KD, "w%d" % wi, ["oT", "yT"])
                            for half in range(2):
                                S.op("dve", lambda e, d=d, half=half, b=banks[half]: e.tensor_tensor(
                                    out=xT[:, d, half * 512:(half + 1) * 512], in0=pb[b][:, :],
                                    in1=xT[:, d, half * 512:(half + 1) * 512], op=ALU.add),
                                    reads=[pk[banks[half]], "xT"], writes=["xT"])
                    S.barrier()
                    rmsnorm_to_hT(lambda k: P(l, 8 + k))
                    for g in range(NF // 2):
                        wi, (wV, wG) = wload([(KD, 256)] * 2)
                        for jj in range(2):
                            f = g * 2 + jj
                            C_ = next_set(); n = C_["n"]
                            bV = next_banks()
                            proj(bV, lambda k, jj=jj: wV[:, k, jj * 128:(jj + 1) * 128], hT_rhs, KD, "w%d" % wi, HTK)
                            hal = hal_ff[:, (l * NF + f) * 2:(l * NF + f) * 2 + 2]
                            hkey = "hff%d_%d" % (l, f)
                            halo_in(C_, hal, hkey, 3, first)
                            for half in range(2):
                                S.op("act", lambda e, half=half: e.copy(out=C_["cb"][:, 2 + half * 512:2 + (half + 1) * 512],
                                                                         in_=pb[bV[half]][:, :]),
                                     reads=[pk[bV[half]]], writes=["cb" + n + "h%d" % half])
                            conv(C_, 3, lambda j, f=f: P(l, 76 + f * 3 + j))
                            halo_out(C_, hal, hkey, 3)
                            S.op("act", lambda e: e.activation(out=C_["ac"][:, :], in_=C_["ac"][:, :], func=AF.Silu),
                                 reads=ackeys(C_), writes=ackeys(C_))
                            bG = next_banks()
                            proj(bG, lambda k, jj=jj: wG[:, k, jj * 128:(jj + 1) * 128], hT_rhs, KD, "w%d" % wi, HTK)
                            for half in range(2):
                                S.op("dve", lambda e, f=f, half=half: e.tensor_tensor(
                                    out=actT[:, f, half * 512:(half + 1) * 512], in0=pb[bG[half]][:, :],
                                    in1=C_["ac"][:, half * 512:(half + 1) * 512], op=ALU.mult),
                                    reads=[pk[bG[half]]] + ackeys(C_), writes=["actT"])
                    for d in range(KD):
                        wi, (wD,) = wload([(NF, 128)])
                        banks = next_banks()
                        proj(banks, lambda f: wD[:, f, :], lambda f, half: actT[:, f, half * 512:(half + 1) * 512],
                             NF, "w%d" % wi, ["actT"])
                        for half in range(2):
                            S.op("dve", lambda e, d=d, half=half, b=banks[half]: e.tensor_tensor(
                                out=xT[:, d, half * 512:(half + 1) * 512], in0=pb[b][:, :],
                                in1=xT[:, d, half * 512:(half + 1) * 512], op=ALU.add),
                                reads=[pk[banks[half]], "xT"], writes=["xT"])
                S.barrier()
                rmsnorm_to_hT(lambda k: prm[:, PL * DEPTH + k:PL * DEPTH + k + 1])
                for k in range(KD):
                    S.op("dve", lambda e, k=k: e.scalar_tensor_tensor(
                        out=xT[:, k, :], in0=xT[:, k, :], scalar=prm[:, PL * DEPTH + k:PL * DEPTH + k + 1], in1=rstd[:, :],
                        op0=ALU.mult, op1=ALU.mult), reads=["xT", "rstd0", "rstd1", "prm"] + HTK, writes=["xT"])
                for tb in range(8):
                    st = stage[tb % 2]
                    for dq in range(2):
                        b = 5 + dq
                        for dd in range(4):
                            d = dq * 4 + dd
                            S.op("pe", lambda e, d=d, dd=dd, b=b: e.transpose(
                                pb[b][:, dd * 128:(dd + 1) * 128], xT[:, d, tb * 128:(tb + 1) * 128], ident),
                                reads=["xT", "cst"], writes=[pk[b]], inc=(dd == 3))
                        S.op("act", lambda e, dq=dq, b=b, st=st: e.copy(out=st[:, dq * 512:(dq + 1) * 512], in_=pb[b][:, :]),
                             reads=[pk[b]], writes=["stage%d" % (tb % 2)])
                    S.dma("sp", out_d[row0 + tb * 128: row0 + (tb + 1) * 128, :], st, "so%d" % (tb % 2),
                          reads=["stage%d" % (tb % 2)])
        S.drain_all("sp")
        build.nins = S.nins
    return nc


def make_consts():
    c = np.zeros((128, 128 * 4 + 64 * 4 + 128 + 512), np.float32)
    c[:, 0:128] = np.eye(128)
    c[:, 128:256] = 1.0
    c[0:64, 256:320] = 1.0
    c[64:128, 320:384] = 1.0
    c[63, 384:512] = 1.0
    p = np.arange(64)[:, None]
    f = np.arange(64)[None, :]
    c0 = 512
    c[0:64, c0:c0 + 64] = (p <= f)
    c[0:64, c0 + 64:c0 + 128] = np.where(p > f, 0.0, NEG)
    c[0:64, c0 + 128:c0 + 192] = np.where(f > p, 0.0, NEG)
    c[0:64, c0 + 192:c0 + 256] = np.where(f >= p, 0.0, NEG)
    c1 = c0 + 256
    c[0:64, c1:c1 + 64] = 1.0
    c2 = c1 + 128
    for h in range(8):
        hh = h % 2
        c[hh * 64:(hh + 1) * 64, c2 + h * 64:c2 + (h + 1) * 64] = 1.0
    return c


def make_params(depth, attn_norm, conv_qkv, a_log, dt_bias, head_norm, conv_sc, sc_norm, ffn_norm, conv_ffn, final_norm):
    pr = np.zeros((128, PL * depth + 8), np.float32)
    for l in range(depth):
        o = l * PL
        pr[:, o + 0:o + 8] = attn_norm[l].reshape(8, 128).T
        pr[:, o + 8:o + 16] = ffn_norm[l].reshape(8, 128).T
        pr[:, o + 16:o + 64] = conv_qkv[l].reshape(4, 12, 128).transpose(2, 1, 0).reshape(128, 48)
        pr[:, o + 64:o + 76] = conv_sc[l].reshape(3, 4, 128).transpose(2, 1, 0).reshape(128, 12)
        pr[:, o + 76:o + 142] = conv_ffn[l].reshape(3, NF, 128).transpose(2, 1, 0).reshape(128, 66)
        pr[:, o + 142:o + 146] = sc_norm[l].reshape(4, 128).T
        pr[:, o + 146] = np.tile(head_norm[l], 2)
        pr[:, o + 147:o + 155] = a_log[l][None, :]
        pr[:, o + 155:o + 163] = dt_bias[l][None, :]
    pr[:, PL * depth:PL * depth + 8] = final_norm.reshape(8, 128).T
    return pr


def make_wstream(depth, W):
    tot = layer_stream_cols()
    out = np.empty((depth, 128, tot), np.float32)
    for l in range(depth):
        off = 0
        for job in layer_jobspec():
            for (name, c0, n, K_) in job:
                blk = W[name][l][:, c0:c0 + n].reshape(K_, 128, n).transpose(1, 0, 2).reshape(128, K_ * n)
                out[l, :, off:off + K_ * n] = blk
                off += K_ * n
        assert off == tot
    return out


_cache = {}


def run(x, attn_norm, w_in, conv_qkv, a_log, dt_bias, head_norm, conv_sc, sc_norm, w_out, ffn_norm, w_up,
        conv_ffn, w_down, final_norm, n_cores=8):
    x = np.asarray(x, np.float32)
    B = x.shape[0]
    depth = w_in.shape[0]
    nseq = B // n_cores
    key = (nseq, depth)
    if key not in _cache:
        _cache[key] = build(nseq, depth)
    nc = _cache[key]
    prm = make_params(depth, *(np.asarray(a, np.float32) for a in (attn_norm, conv_qkv, a_log, dt_bias, head_norm,
                                                                     conv_sc, sc_norm, ffn_norm, conv_ffn, final_norm)))
    cst = make_consts()
    shared = {"wst": make_wstream(depth, {"w_in": np.asarray(w_in, np.float32), "w_out": np.asarray(w_out, np.float32),
                                          "w_up": np.asarray(w_up, np.float32), "w_down": np.asarray(w_down, np.float32)}),
              "prm": prm, "cst": cst}
    in_maps = []
    for c in range(n_cores):
        m = dict(shared)
        m["x"] = np.ascontiguousarray(x[c * nseq:(c + 1) * nseq].reshape(nseq * T, D))
        in_maps.append(m)
    res = run_bass_kernel_spmd(nc, in_maps, core_ids=list(range(n_cores)))
    out = np.concatenate([np.asarray(r["out"]).reshape(nseq, T, D) for r in res.results], axis=0)
    if DBG:
        np.save("_dbg.npy", np.asarray(res.results[0]["dbg"]))
    return out.astype(np.float32)


def kernel(x, attn_norm, w_in, conv_qkv, a_log, dt_bias, head_norm, conv_sc, sc_norm, w_out, ffn_norm, w_up,
           conv_ffn, w_down, final_norm):
    return run(x, attn_norm, w_in, conv_qkv, a_log, dt_bias, head_norm, conv_sc, sc_norm, w_out, ffn_norm, w_up,
               conv_ffn, w_down, final_norm, n_cores=8)
```

```python
import numpy as np
from contextlib import ExitStack
import concourse.bass as bass
import concourse.mybir as mybir
from concourse.bass_utils import run_bass_kernel_spmd

F32 = mybir.dt.float32
BF16 = mybir.dt.bfloat16
ALU = mybir.AluOpType
AF = mybir.ActivationFunctionType

D = 1024
KD = 8
T = 2048
TT = 1024
NT = T // TT
NCH = TT // 64
DFF = 2816
NF = 22
EPS = 1e-6
NEG = -30000.0
CQ, CK, CV, CZ, CB, CA, CSB, CSC, CSX = 0, 512, 1024, 1536, 2048, 2056, 2064, 2576, 3088
PL = 163


def layer_jobspec():
    jl = []
    for m in range(4):
        jl.append([("w_in", CSC + m * 128, 128, KD), ("w_in", CSX + m * 128, 128, KD), ("w_in", CSB + m * 128, 128, KD)])
    jl.append([("w_in", CZ, 512, KD)])
    jl.append([("w_in", CB, 16, KD)])
    for c0_ in (CQ, CK, CV):
        jl.append([("w_in", c0_, 512, KD)])
    for dg in range(2):
        jl.append([("w_out", dg * 512, 512, KD)])
    for g in range(NF // 2):
        jl.append([("w_up", g * 256, 256, KD), ("w_up", DFF + g * 256, 256, KD)])
    for d in range(KD):
        jl.append([("w_down", d * 128, 128, NF)])
    return jl


def layer_stream_cols():
    return sum(K_ * n for job in layer_jobspec() for (_, _, n, K_) in job)


class Sched:
    def __init__(self, nc, es):
        self.nc = nc
        self.es = es
        self.eng = {"pe": nc.tensor, "act": nc.scalar, "dve": nc.vector, "pool": nc.gpsimd, "sp": nc.sync}
        self.sem = {n: es.enter_context(nc.semaphore("s_" + n)) for n in self.eng}
        self.cnt = {n: 0 for n in self.eng}
        self.known = {n: {} for n in self.eng}
        self.lastw = {}
        self.readers = {}
        self.dsem = {}
        self.dcnt = {}
        self.pending = {n: False for n in self.eng}
        self.nins = 0

    def _deps(self, reads, writes):
        toks = []
        for k in reads:
            t = self.lastw.get(k)
            if t is not None:
                toks.append(t)
        for k in writes:
            t = self.lastw.get(k)
            if t is not None:
                toks.append(t)
            toks.extend(self.readers.get(k, ()))
        return toks

    def _wait(self, en, toks):
        need = {}
        kn = self.known[en]
        for (s, v) in toks:
            if kn.get(s, 0) >= v:
                continue
            if need.get(s, 0) < v:
                need[s] = v
        for s, v in need.items():
            self.eng[en].wait_ge(s, v)
            kn[s] = v
            self.nins += 1

    def _record(self, tok, reads, writes):
        for k in writes:
            self.lastw[k] = tok
            self.readers[k] = []
        for k in reads:
            self.readers.setdefault(k, []).append(tok)

    def op(self, en, fn, reads=(), writes=(), inc=True):
        toks = self._deps(reads, writes)
        for k in reads:
            if k.startswith("pb"):
                toks.extend(t for t in self.readers.get(k, ()) if t[0] is not self.sem[en])
        self._wait(en, toks)
        ins = fn(self.eng[en])
        s = self.sem[en]
        self.nins += 1
        if inc:
            self.cnt[en] += 1
            ins.then_inc(s, 1)
            tok = (s, self.cnt[en])
            self.pending[en] = False
        else:
            tok = (s, self.cnt[en] + 1)
            self.pending[en] = True
        if en == "pe":
            self.known[en][s] = tok[1]
        self._record(tok, reads, writes)
        return ins

    def dma(self, en, out, in_, slot, reads=(), writes=()):
        if slot not in self.dsem:
            self.dsem[slot] = self.es.enter_context(self.nc.semaphore("d_" + slot))
            self.dcnt[slot] = 0
        self._wait(en, self._deps(reads, writes))
        self.dcnt[slot] += 16
        ins = self.eng[en].dma_start(out=out, in_=in_)
        ins.then_inc(self.dsem[slot], 16)
        self.nins += 1
        tok = (self.dsem[slot], self.dcnt[slot])
        self._record(tok, reads, writes)
        return ins

    def barrier(self):
        for en in self.eng:
            assert not self.pending[en], en
        toks = [(self.sem[o], self.cnt[o]) for o in self.eng if self.cnt[o] > 0]
        toks += [(self.dsem[k], self.dcnt[k]) for k in self.dsem]
        for en in ("pe", "act", "dve", "pool", "sp"):
            self._wait(en, toks)

    def drain_all(self, en):
        toks = [(self.sem[o], self.cnt[o]) for o in self.eng if self.cnt[o] > 0]
        toks += [(self.dsem[k], self.dcnt[k]) for k in self.dsem]
        self._wait(en, toks)


DBG = False


def build(NSEQ, DEPTH):
    nc = bass.Bass("TRN2", target_bir_lowering=False)
    x_d = nc.dram_tensor("x", [NSEQ * T, D], F32, kind="ExternalInput").ap()
    TOTL = layer_stream_cols()
    wst_d = nc.dram_tensor("wst", [DEPTH, 128, TOTL], F32, kind="ExternalInput").ap()
    NPRM = PL * DEPTH + 8
    prm_d = nc.dram_tensor("prm", [128, NPRM], F32, kind="ExternalInput").ap()
    NCST = 128 * 4 + 64 * 4 + 128 + 512
    cst_d = nc.dram_tensor("cst", [128, NCST], F32, kind="ExternalInput").ap()
    out_d = nc.dram_tensor("out", [NSEQ * T, D], F32, kind="ExternalOutput").ap()
    dbg_d = nc.dram_tensor("dbg", [16, 128, 512], F32, kind="ExternalOutput").ap() if DBG else None

    with ExitStack() as es:
        S = Sched(nc, es)

        def sb(name, shape, dt):
            return es.enter_context(nc.sbuf_tensor(name, shape, dt))

        def ps(name, shape, dt):
            return es.enter_context(nc.psum_tensor(name, shape, dt))

        xT = sb("xT", [128, KD, TT], F32)
        arena = sb("arena", [128, 28 * 1024], BF16)
        arenaB = sb("arenaB", [128, 12 * 1024], F32)
        rstd = sb("rstd", [128, TT], F32)
        kqbd_t = sb("kqbd_t", [128, 1024], BF16)
        ctmp = rstd
        HTK = ["hT%d" % k_ for k_ in range(KD)]
        NW = 4
        wbuf = [sb("wbuf%d" % i, [128, 4096], BF16) for i in range(NW)]
        prm = sb("prm_sb", [128, NPRM], F32)
        cst = sb("cst_sb", [128, NCST], F32)
        identb = sb("identb", [128, 128], BF16)
        onesb = sb("onesb", [128, 128], BF16)
        blkb = sb("blkb", [128, 128], BF16)
        Sbd = [sb("Sbd%d" % l, [128, 512], F32) for l in range(DEPTH)]
        Sb = [sb("Sb%d" % l, [128, 512], BF16) for l in range(DEPTH)]
        hal_qkv = sb("hal_qkv", [128, DEPTH * 12 * 3], F32)
        hal_sc = sb("hal_sc", [128, DEPTH * 4 * 2], F32)
        hal_ff = sb("hal_ff", [128, DEPTH * NF * 2], F32)
        tk = {n: sb("tk_" + n, [64, 128], F32) for n in
              ("bet", "nbet", "xg", "g", "gc", "gam", "bg", "ekd", "nega")}
        egl = sb("egl", [128, 128], F32)

        def av(off, n):
            return arena[:, off:off + n]
        qT = av(0, 4096).rearrange("p (m t) -> p m t", m=4)
        kT = av(4096, 4096).rearrange("p (m t) -> p m t", m=4)
        vT = av(8192, 4096).rearrange("p (m t) -> p m t", m=4)
        yT = av(12288, 4096).rearrange("p (m t) -> p m t", m=4)
        oT = av(16384, 4096).rearrange("p (m t) -> p m t", m=4)
        szT = av(20480, 8192).bitcast(F32).rearrange("p (m t) -> p m t", m=4)
        actT = av(0, NF * TT).rearrange("p (f t) -> p f t", f=NF)
        stage = [av(22528 + i * 2048, 2048).bitcast(F32) for i in range(2)]
        hT = arenaB[:, 0:4096].bitcast(BF16).rearrange("p (k t) -> p k t", k=KD)
        def bv(off, n, dt=F32, parts=64):
            a = arenaB[0:parts, off:off + n]
            return a.bitcast(dt) if dt is not F32 else a
        o_ = [0]
        def balloc(n_f32, dt=F32, parts=64):
            a = bv(o_[0], n_f32, dt, parts)
            o_[0] += n_f32
            return a
        decL = balloc(512); decQ = balloc(512)
        R2L = balloc(256, BF16); R2Q = balloc(256, BF16)
        N0 = balloc(256, BF16); N0T = balloc(256, BF16)
        Pb = [balloc(256, BF16) for _ in range(2)]
        PTb = [balloc(256, BF16) for _ in range(2)]
        TTm2 = [balloc(256, BF16) for _ in range(2)]
        qkTb2 = [balloc(256, BF16) for _ in range(2)]
        kd2 = [balloc(256, BF16) for _ in range(2)]
        vb2 = [balloc(512) for _ in range(2)]
        t_x = balloc(512)
        r_b = balloc(256, BF16)
        vnew = balloc(256, BF16)
        t1 = balloc(512, F32, 128)
        o1 = balloc(512); o_tok = balloc(512); osq = balloc(512)
        on_b = balloc(256, BF16)
        egm2 = [balloc(512, F32, 128) for _ in range(2)]
        ssr = balloc(8); rr = balloc(8)
        kqbd = kqbd_t[:, :]
        assert o_[0] <= 11 * 1024, o_[0]
        CHS = []
        off_ = 4096
        for s_ in range(3):
            cb_ = arenaB[:, off_:off_ + TT + 4]; off_ += TT + 4
            ac_ = arenaB[:, off_:off_ + TT]; off_ += TT
            sq_ = arenaB[:, off_:off_ + TT // 2].bitcast(BF16); off_ += TT // 2
            CHS.append(dict(cb=cb_, ac=ac_, sq=sq_, n="%d" % s_))
        assert off_ <= 12 * 1024
        chn = [0]
        def next_set():
            c_ = CHS[chn[0] % 3]
            chn[0] += 1
            return c_
        bkn = [0]
        def next_banks():
            b_ = ((0, 1), (2, 3), (4, 5))[bkn[0] % 3]
            bkn[0] += 1
            return b_
        pend = []
        def flushA(keep=0):
            for it in pend[:max(0, len(pend) - keep)]:
                if not it[1]:
                    next(it[0])
                    it[1] = True
        def flushB(keep=0):
            while len(pend) > keep:
                it = pend.pop(0)
                if not it[1]:
                    next(it[0])
                for _ in it[0]:
                    pass
        def flush(keep=0):
            flushA(keep)
            flushB(keep)

        pb = [ps("pb%d" % i, [128, 512], F32) for i in range(7)] + [ps("pb7", [128, 1024], BF16)]
        pk = ["pb%d" % i for i in range(8)]

        S.dma("sp", prm[:], prm_d, "prm", writes=["prm"])
        S.dma("sp", cst[:], cst_d, "cst", writes=["cst"])
        ident = cst[:, 0:128]
        ones_f = cst[:, 128:256]
        blk_f = cst[:, 256:384]
        sellast = cst[0:64, 384:512]
        c0 = 512
        tri = cst[0:64, c0:c0 + 64]
        maskLs = cst[0:64, c0 + 64:c0 + 128]
        maskUs = cst[0:64, c0 + 128:c0 + 192]
        maskUi = cst[0:64, c0 + 192:c0 + 256]
        c1 = c0 + 256
        ones64 = cst[0:64, c1:c1 + 64]
        i64 = cst[0:64, 0:64]
        c2 = c1 + 128
        maskrep = cst[:, c2:c2 + 512]
        S.op("act", lambda e: e.copy(out=identb[:], in_=ident), reads=["cst"], writes=["identb"])
        S.op("act", lambda e: e.copy(out=onesb[:], in_=ones_f), reads=["cst"], writes=["onesb"])
        S.op("act", lambda e: e.copy(out=blkb[:], in_=blk_f), reads=["cst"], writes=["blkb"])
        S.op("pool", lambda e: e.memset(kqbd, 0.0), writes=["kqbd"])
        cb16 = sb("cb16", [64, 128 + 1024], BF16)
        trigt_f = sb("trigt_f", [64, 64], F32)
        tri_b = cb16[:, 0:64]
        trigt_b = cb16[:, 64:128]
        mLs_b = cb16[:, 128:640]
        mUi_b = cb16[:, 640:1152]
        S.op("dve", lambda e: e.tensor_scalar(out=trigt_f[:, :], in0=maskLs, scalar1=1.0 / 30000.0, scalar2=1.0,
                                              op0=ALU.mult, op1=ALU.add), reads=["cst"], writes=["cb16"])
        S.op("act", lambda e: e.copy(out=tri_b, in_=tri), reads=["cst"], writes=["cb16"])
        S.op("act", lambda e: e.copy(out=trigt_b, in_=trigt_f[:, :]), reads=["cb16"], writes=["cb16"])
        S.op("act", lambda e: e.copy(out=mLs_b.rearrange("p (h x) -> p h x", h=8),
                                     in_=maskLs.unsqueeze(1).to_broadcast([64, 8, 64])), reads=["cst"], writes=["cb16"])
        S.op("act", lambda e: e.copy(out=mUi_b.rearrange("p (h x) -> p h x", h=8),
                                     in_=maskUi.unsqueeze(1).to_broadcast([64, 8, 64])), reads=["cst"], writes=["cb16"])

        def P(l, off, n=1):
            return prm[:, l * PL + off: l * PL + off + n]

        wjobs = []
        wstate = {"use": 0, "iss": 0}
        LOOK = 2
        def wviews(j):
            i = j % NW
            views = []
            off = 0
            for (K_, n_) in wjobs[j][2]:
                views.append(wbuf[i][:, off:off + K_ * n_].rearrange("p (k n) -> p k n", k=K_))
                off += K_ * n_
            assert off <= 4096
            return i, views, off
        def wissue(j):
            i, views, sz = wviews(j)
            l_, off_, _ = wjobs[j]
            S.dma("pool", wbuf[i][:, 0:sz], wst_d[l_][:, off_:off_ + sz], "w%d" % i, writes=["w%d" % i])
        def wload(shapes):
            j = wstate["use"]
            assert list(shapes) == list(wjobs[j][2]), (shapes, wjobs[j])
            while wstate["iss"] < len(wjobs) and wstate["iss"] <= j + LOOK:
                wissue(wstate["iss"])
                wstate["iss"] += 1
            wstate["use"] += 1
            i, views, _ = wviews(j)
            return i, views

        def proj(banks, lhs_fn, rhs, K_, wkey, rkeys):
            for half in range(2):
                b = banks[half]
                for k in range(K_):
                    S.op("pe", lambda e, k=k, half=half, b=b: e.matmul(
                        pb[b][:, :], lhsT=lhs_fn(k), rhs=rhs(k, half), start=(k == 0), stop=(k == K_ - 1)),
                        reads=[wkey] + rkeys, writes=[pk[b]], inc=(k == K_ - 1))

        def hT_rhs(k, half):
            return hT[:, k, half * 512:(half + 1) * 512]

        def rsqrt_from(psbank, half, out_ap, scale, key_out):
            S.op("act", lambda e: e.activation(out=out_ap, in_=pb[psbank][:, :], func=AF.Ln, bias=EPS, scale=scale),
                 reads=[pk[psbank]], writes=[key_out])
            S.op("act", lambda e: e.activation(out=out_ap, in_=out_ap, func=AF.Exp, scale=-0.5),
                 reads=[key_out], writes=[key_out])

        def rmsnorm_to_hT(gcol_fn):
            for k in range(KD):
                S.op("act", lambda e, k=k: e.activation(out=hT[:, k, :], in_=xT[:, k, :], func=AF.Square),
                     reads=["xT"], writes=["hT%d" % k])
            bk = next_banks()
            for half in range(2):
                for k in range(KD):
                    S.op("pe", lambda e, k=k, half=half: e.matmul(
                        pb[bk[half]][:, :], lhsT=onesb[:], rhs=hT[:, k, half * 512:(half + 1) * 512],
                        start=(k == 0), stop=(k == KD - 1)),
                        reads=["onesb", "hT%d" % k], writes=[pk[bk[half]]], inc=(k == KD - 1))
                rsqrt_from(bk[half], half, rstd[:, half * 512:(half + 1) * 512], 1.0 / D, "rstd%d" % half)
            for k in range(KD):
                S.op("dve", lambda e, k=k: e.scalar_tensor_tensor(
                    out=hT[:, k, :], in0=xT[:, k, :], scalar=gcol_fn(k), in1=rstd[:, :],
                    op0=ALU.mult, op1=ALU.mult), reads=["xT", "rstd0", "rstd1", "prm"], writes=["hT%d" % k])

        def conv(C_, K_, wcol_fn):
            n = C_["n"]
            sp = 896 if K_ == 4 else 768
            wp = TT - sp
            rk = ["cb" + n + "h0", "cb" + n + "h1", "cb" + n + "hal", "prm"]
            hk = ["ac" + n + "h0", "ac" + n + "h1"]
            S.op("dve", lambda e: e.tensor_scalar(out=C_["ac"][:, 0:sp], in0=C_["cb"][:, 0:sp],
                                                  scalar1=wcol_fn(0), scalar2=None, op0=ALU.mult),
                 reads=rk, writes=["ac" + n + "c0"] + hk)
            for j in range(1, K_):
                S.op("dve", lambda e, j=j: e.scalar_tensor_tensor(
                    out=C_["ac"][:, 0:sp], in0=C_["cb"][:, j:j + sp], scalar=wcol_fn(j),
                    in1=C_["ac"][:, 0:sp], op0=ALU.mult, op1=ALU.add), reads=rk, writes=["ac" + n + "c0"])
            tmp = C_["sq"].bitcast(F32)[:, 0:wp]
            S.op("pool", lambda e: e.tensor_tensor(out=C_["ac"][:, sp:TT], in0=C_["cb"][:, sp:TT],
                                                   in1=wcol_fn(0).to_broadcast([128, wp]), op=ALU.mult),
                 reads=rk, writes=["ac" + n + "c1"] + hk)
            for j in range(1, K_):
                S.op("pool", lambda e, j=j: e.tensor_tensor(out=tmp, in0=C_["cb"][:, sp + j:TT + j],
                                                            in1=wcol_fn(j).to_broadcast([128, wp]), op=ALU.mult),
                     reads=rk, writes=["sq" + n])
                S.op("pool", lambda e: e.tensor_tensor(out=C_["ac"][:, sp:TT], in0=C_["ac"][:, sp:TT], in1=tmp,
                                                       op=ALU.add), reads=["sq" + n], writes=["ac" + n + "c1"])

        def ackeys(C_):
            return ["ac" + C_["n"] + "h0", "ac" + C_["n"] + "h1", "ac" + C_["n"] + "c0", "ac" + C_["n"] + "c1"]

        def cbkeys(C_):
            return ["cb" + C_["n"] + "h0", "cb" + C_["n"] + "h1", "cb" + C_["n"] + "hal"]

        def halo_in(C_, hal_ap, hkey, K_, first):
            n = C_["n"]
            if first:
                S.op("pool", lambda e: e.memset(C_["cb"][:, 0:K_ - 1], 0.0), writes=["cb" + n + "hal"])
            else:
                S.op("pool", lambda e: e.tensor_copy(out=C_["cb"][:, 0:K_ - 1], in_=hal_ap), reads=[hkey],
                     writes=["cb" + n + "hal"])

        def halo_out(C_, hal_ap, hkey, K_):
            n = C_["n"]
            S.op("pool", lambda e: e.tensor_copy(out=hal_ap, in_=C_["cb"][:, TT:TT + K_ - 1]), reads=["cb" + n + "h1"],
                 writes=[hkey])

        for _s in range(NSEQ):
            for _t in range(NT):
                for l in range(DEPTH):
                    off_ = 0
                    for job in layer_jobspec():
                        shp = [(K_, n) for (_, _, n, K_) in job]
                        wjobs.append((l, off_, shp))
                        off_ += sum(K_ * n for (K_, n) in shp)

        for sq_i in range(NSEQ):
            for ti in range(NT):
                first = (ti == 0)
                row0 = sq_i * T + ti * TT
                S.barrier()
                for tb in range(8):
                    st = stage[tb % 2]
                    S.dma("sp", st, x_d[row0 + tb * 128: row0 + (tb + 1) * 128, :], "st%d" % (tb % 2),
                          writes=["stage%d" % (tb % 2)])
                    for dq in range(2):
                        b = 5 + dq
                        for dd in range(4):
                            d = dq * 4 + dd
                            S.op("pe", lambda e, d=d, dd=dd, b=b, st=st: e.transpose(
                                pb[b][:, dd * 128:(dd + 1) * 128], st[:, d * 128:(d + 1) * 128], ident),
                                reads=["stage%d" % (tb % 2), "cst"], writes=[pk[b]], inc=(dd == 3))
                        S.op("act", lambda e, dq=dq, b=b, tb=tb: e.copy(
                            out=xT[:, dq * 4:(dq + 1) * 4, tb * 128:(tb + 1) * 128],
                            in_=pb[b][:, :].rearrange("p (d t) -> p d t", d=4)),
                            reads=[pk[b]], writes=["xT"])
                if first:
                    for l in range(DEPTH):
                        S.op("pool", lambda e, l=l: e.memset(Sbd[l][:], 0.0), writes=["Sbd%d" % l])
                        S.op("pool", lambda e, l=l: e.memset(Sb[l][:], 0.0), writes=["Sb%d" % l])

                for l in range(DEPTH):
                    S.barrier()
                    rmsnorm_to_hT(lambda k: P(l, 0 + k))
                    for m in range(4):
                        wi, (wC, wX, wB) = wload([(KD, 128)] * 3)
                        wkey = "w%d" % wi
                        C_ = next_set(); n = C_["n"]
                        bC = next_banks()
                        proj(bC, lambda k: wC[:, k, :], hT_rhs, KD, wkey, HTK)
                        for half in range(2):
                            S.op("act", lambda e, half=half: e.copy(out=C_["ac"][:, half * 512:(half + 1) * 512],
                                                                     in_=pb[bC[half]][:, :]),
                                 reads=[pk[bC[half]]], writes=["ac" + n + "h%d" % half])
                        flushA(keep=1)
                        bX = next_banks()
                        proj(bX, lambda k: wX[:, k, :], hT_rhs, KD, wkey, HTK)
                        hal = hal_sc[:, (l * 4 + m) * 2:(l * 4 + m) * 2 + 2]
                        hkey = "hsc%d_%d" % (l, m)
                        halo_in(C_, hal, hkey, 3, first)
                        for half in range(2):
                            S.op("dve", lambda e, half=half: e.tensor_tensor(
                                out=C_["cb"][:, 2 + half * 512:2 + (half + 1) * 512], in0=pb[bX[half]][:, :],
                                in1=C_["ac"][:, half * 512:(half + 1) * 512], op=ALU.mult),
                                reads=[pk[bX[half]], "ac" + n + "h%d" % half], writes=["cb" + n + "h%d" % half])
                        conv(C_, 3, lambda j: P(l, 64 + m * 3 + j))
                        halo_out(C_, hal, hkey, 3)
                        bB = next_banks()
                        proj(bB, lambda k: wB[:, k, :], hT_rhs, KD, wkey, HTK)
                        for half in range(2):
                            S.op("dve", lambda e, half=half: e.tensor_tensor(
                                out=C_["cb"][:, half * 512:(half + 1) * 512], in0=pb[bB[half]][:, :],
                                in1=C_["ac"][:, half * 512:(half + 1) * 512], op=ALU.mult),
                                reads=[pk[bB[half]]] + ackeys(C_) + cbkeys(C_), writes=cbkeys(C_))
                        S.op("pool", lambda e: e.tensor_tensor(out=C_["sq"][:, :], in0=C_["cb"][:, 0:TT], in1=C_["cb"][:, 0:TT],
                                                               op=ALU.mult), reads=cbkeys(C_), writes=["sq" + n])
                        flush(keep=1)
                        def st2(C_=C_, n=n, m=m):
                            bS = next_banks()
                            for half in range(2):
                                S.op("pe", lambda e, half=half: e.matmul(pb[bS[half]][:, :], lhsT=blkb[:],
                                                                          rhs=C_["sq"][:, half * 512:(half + 1) * 512],
                                                                          start=True, stop=True),
                                     reads=["blkb", "sq" + n], writes=[pk[bS[half]]])
                                rsqrt_from(bS[half], half, C_["ac"][:, half * 512:(half + 1) * 512], 1.0 / 64, "ac" + n + "h%d" % half)
                            yield
                            S.op("dve", lambda e: e.scalar_tensor_tensor(
                                out=yT[:, m, :], in0=C_["cb"][:, 0:TT], scalar=P(l, 142 + m), in1=C_["ac"][:, :],
                                op0=ALU.mult, op1=ALU.mult), reads=cbkeys(C_) + ackeys(C_) + ["prm"], writes=["yT"])
                        pend.append([st2(), False])
                    wi, (wZ,) = wload([(KD, 512)])
                    for m in range(4):
                        bZ = next_banks()
                        proj(bZ, lambda k, m=m: wZ[:, k, m * 128:(m + 1) * 128], hT_rhs, KD, "w%d" % wi, HTK)
                        for half in range(2):
                            S.op("act", lambda e, half=half, m=m: e.activation(
                                out=szT[:, m, half * 512:(half + 1) * 512], in_=pb[bZ[half]][:, :], func=AF.Silu),
                                reads=[pk[bZ[half]]], writes=["szT"])
                        flush()
                    wi, (wBA,) = wload([(KD, 16)])
                    for i in range(NCH):
                        for k in range(KD):
                            S.op("pe", lambda e, i=i, k=k: e.matmul(
                                pb[4][0:64, i * 16:(i + 1) * 16], lhsT=hT[:, k, i * 64:(i + 1) * 64], rhs=wBA[:, k, :],
                                start=(k == 0), stop=(k == KD - 1)),
                                reads=["w%d" % wi] + HTK, writes=[pk[4]], inc=(k == KD - 1))
                    bav = pb[4][0:64, 0:256].rearrange("p (i c) -> p i c", c=16)
                    def tv(n):
                        return tk[n][:, :].rearrange("p (i h) -> p i h", h=8)
                    S.op("act", lambda e: e.activation(out=tv("bet"), in_=bav[:, :, 0:8], func=AF.Sigmoid),
                         reads=[pk[4]], writes=["bet"])
                    S.op("dve", lambda e: e.tensor_scalar(out=tk["nbet"][:, :], in0=tk["bet"][:, :], scalar1=-1.0, scalar2=None,
                                                          op0=ALU.mult), reads=["bet"], writes=["nbet"])
                    S.op("dve", lambda e: e.tensor_tensor(
                        out=tv("xg"), in0=bav[:, :, 8:16],
                        in1=prm[0:64, l * PL + 155:l * PL + 163].unsqueeze(1).to_broadcast([64, NCH, 8]), op=ALU.add),
                        reads=[pk[4], "prm"], writes=["xg"])
                    S.op("act", lambda e: e.activation(out=tk["xg"][:, :], in_=tk["xg"][:, :], func=AF.Exp),
                         reads=["xg"], writes=["xg"])
                    S.op("act", lambda e: e.activation(out=tk["xg"][:, :], in_=tk["xg"][:, :], func=AF.Ln, bias=1.0),
                         reads=["xg"], writes=["xg"])
                    S.op("act", lambda e: e.activation(out=tk["nega"][:, 0:8], in_=prm[0:64, l * PL + 147:l * PL + 155],
                                                       func=AF.Exp), reads=["prm"], writes=["nega"])
                    S.op("dve", lambda e: e.scalar_tensor_tensor(
                        out=tv("g"), in0=tv("xg"), scalar=-1.0,
                        in1=tk["nega"][:, 0:8].unsqueeze(1).to_broadcast([64, NCH, 8]), op0=ALU.mult, op1=ALU.mult),
                        reads=["xg", "nega"], writes=["g"])
                    S.op("pe", lambda e: e.matmul(pb[5][0:64, 0:128], lhsT=tri, rhs=tk["g"][:, :], start=True, stop=True),
                         reads=["cst", "g"], writes=[pk[5]])
                    S.op("act", lambda e: e.copy(out=tk["gc"][:, :], in_=pb[5][0:64, 0:128]), reads=[pk[5]], writes=["gc"])
                    S.op("pe", lambda e: e.matmul(pb[4][:, 0:128], lhsT=sellast, rhs=tk["gc"][:, :], start=True, stop=True),
                         reads=["cst", "gc"], writes=[pk[4]])
                    S.op("act", lambda e: e.activation(out=egl[:, :], in_=pb[4][:, 0:128], func=AF.Exp),
                         reads=[pk[4]], writes=["egl"])
                    S.op("dve", lambda e: e.tensor_tensor(out=tk["ekd"][:, :], in0=pb[4][0:64, 0:128], in1=tk["gc"][:, :],
                                                          op=ALU.subtract), reads=[pk[4], "gc"], writes=["ekd"])
                    S.op("act", lambda e: e.activation(out=tk["ekd"][:, :], in_=tk["ekd"][:, :], func=AF.Exp),
                         reads=["ekd"], writes=["ekd"])
                    S.op("act", lambda e: e.activation(out=tk["gam"][:, :], in_=tk["gc"][:, :], func=AF.Exp),
                         reads=["gc"], writes=["gam"])
                    S.op("dve", lambda e: e.tensor_tensor(out=tk["bg"][:, :], in0=tk["bet"][:, :], in1=tk["gam"][:, :],
                                                          op=ALU.mult), reads=["bet", "gam"], writes=["bg"])
                    for ty, (c0_, dst) in enumerate(((CQ, qT), (CK, kT), (CV, vT))):
                        wi, (wQ,) = wload([(KD, 512)])
                        for m in range(4):
                            ch = ty * 4 + m
                            C_ = next_set(); n = C_["n"]
                            bQ = next_banks()
                            proj(bQ, lambda k, m=m: wQ[:, k, m * 128:(m + 1) * 128], hT_rhs, KD, "w%d" % wi, HTK)
                            hal = hal_qkv[:, (l * 12 + ch) * 3:(l * 12 + ch) * 3 + 3]
                            hkey = "hqkv%d_%d" % (l, ch)
                            halo_in(C_, hal, hkey, 4, first)
                            for half, en in ((0, "act"), (1, "act")):
                                if en == "act":
                                    S.op("act", lambda e, half=half: e.copy(
                                        out=C_["cb"][:, 3 + half * 512:3 + (half + 1) * 512], in_=pb[bQ[half]][:, :]),
                                        reads=[pk[bQ[half]]], writes=["cb" + n + "h%d" % half])
                                else:
                                    S.op("dve", lambda e, half=half: e.tensor_copy(
                                        out=C_["cb"][:, 3 + half * 512:3 + (half + 1) * 512], in_=pb[bQ[half]][:, :]),
                                        reads=[pk[bQ[half]]], writes=["cb" + n + "h%d" % half])
                            flushA(keep=1 if ty < 2 else 0)
                            conv(C_, 4, lambda j, ch=ch: P(l, 16 + ch * 4 + j))
                            halo_out(C_, hal, hkey, 4)
                            if ty == 2:
                                S.op("act", lambda e, m=m: e.activation(out=vT[:, m, :], in_=C_["ac"][:, :], func=AF.Silu),
                                     reads=ackeys(C_), writes=["vT"])
                                flush()
                            else:
                                S.op("act", lambda e: e.activation(out=C_["ac"][:, :], in_=C_["ac"][:, :], func=AF.Silu),
                                     reads=ackeys(C_), writes=ackeys(C_))
                                S.op("pool", lambda e: e.tensor_tensor(out=C_["sq"][:, :], in0=C_["ac"][:, :], in1=C_["ac"][:, :],
                                                                       op=ALU.mult), reads=ackeys(C_), writes=["sq" + n])
                                flush(keep=1)
                                def st2(C_=C_, n=n, m=m, ty=ty):
                                    bS = next_banks()
                                    for half in range(2):
                                        S.op("pe", lambda e, half=half: e.matmul(pb[bS[half]][:, :], lhsT=blkb[:],
                                                                                  rhs=C_["sq"][:, half * 512:(half + 1) * 512],
                                                                                  start=True, stop=True),
                                             reads=["blkb", "sq" + n], writes=[pk[bS[half]]])
                                        rsqrt_from(bS[half], half, C_["cb"][:, half * 512:(half + 1) * 512], 1.0,
                                                   "cb" + n + "h%d" % half)
                                    yield
                                    if ty == 0:
                                        S.op("dve", lambda e: e.scalar_tensor_tensor(
                                            out=qT[:, m, :], in0=C_["ac"][:, :], scalar=0.125, in1=C_["cb"][:, 0:TT],
                                            op0=ALU.mult, op1=ALU.mult), reads=ackeys(C_) + cbkeys(C_), writes=["qT"])
                                    else:
                                        S.op("dve", lambda e: e.tensor_tensor(
                                            out=kT[:, m, :], in0=C_["ac"][:, :], in1=C_["cb"][:, 0:TT], op=ALU.mult),
                                            reads=ackeys(C_) + cbkeys(C_), writes=["kT"])
                                pend.append([st2(), False])
                    flush()
                    S.barrier()
                    SL = Sbd[l]; SBl = Sb[l]; skey = "Sbd%d" % l; sbkey = "Sb%d" % l
                    def h3(ap):
                        return ap.rearrange("p (h x) -> p h x", h=8)
                    def tkb(n, i, parts=64):
                        return tk[n][0:parts, i * 8:(i + 1) * 8].unsqueeze(2).to_broadcast([parts, 8, 64])
                    def cb(ap):
                        return ap.unsqueeze(1).to_broadcast([64, 8, 64])
                    i64b = identb[0:64, 0:64]
                    pT = pb[7][:, :]

                    def prep(i):
                        pp = str(i % 2)
                        TTm = TTm2[i % 2]; qkTb = qkTb2[i % 2]; kd = kd2[i % 2]; vb = vb2[i % 2]; egm = egm2[i % 2]
                        tok = slice(i * 64, (i + 1) * 64)
                        kq5 = kqbd.rearrange("p (w m hh x) -> p w m hh x", w=2, m=4, hh=2)
                        for w_, src, skey_ in ((0, kT, "kT"), (1, qT, "qT")):
                            for hh in range(2):
                                S.op("pool", lambda e, w_=w_, src=src, hh=hh: e.tensor_copy(
                                    out=kq5[hh * 64:(hh + 1) * 64, w_, :, hh, :], in_=src[hh * 64:(hh + 1) * 64, :, tok]),
                                    reads=[skey_], writes=["kqbd"])
                        kq3 = kqbd.rearrange("p (w m n) -> p w m n", w=2, m=4)
                        for w_ in range(2):
                            for m in range(4):
                                S.op("pe", lambda e, w_=w_, m=m: e.matmul(
                                    pb[w_][0:64, m * 128:(m + 1) * 128], lhsT=kT[:, m, tok], rhs=kq3[:, w_, m, :],
                                    start=True, stop=True), reads=["kT", "kqbd"], writes=[pk[w_]], inc=(m == 3))
                        for w_, src, skey_ in ((0, kT, "kT"), (1, vT, "vT")):
                            for m in range(4):
                                S.op("pe", lambda e, w_=w_, m=m, src=src: e.transpose(
                                    pT[0:64, w_ * 512 + m * 128: w_ * 512 + (m + 1) * 128], src[:, m, tok], identb[:]),
                                    reads=[skey_, "identb"], writes=[pk[7]], inc=(w_ == 1 and m == 3))
                        yield
                        S.op("pool", lambda e: e.tensor_tensor(out=h3(R2L), in0=cb(trigt_f[:, :]), in1=tkb("g", i), op=ALU.mult),
                             reads=["cb16", "g"], writes=["R2L"])
                        S.op("pool", lambda e: e.tensor_tensor(out=h3(R2Q), in0=cb(tri), in1=tkb("g", i), op=ALU.mult),
                             reads=["cst", "g"], writes=["R2Q"])
                        S.op("dve", lambda e: e.tensor_tensor(out=h3(kd), in0=h3(pT[0:64, 0:512]), in1=tkb("ekd", i),
                                                              op=ALU.mult), reads=[pk[7], "ekd"], writes=["kd" + pp])
                        S.op("dve", lambda e: e.tensor_tensor(out=h3(vb), in0=h3(pT[0:64, 512:1024]), in1=tkb("bet", i),
                                                              op=ALU.mult), reads=[pk[7], "bet"], writes=["vb" + pp])
                        yield
                        S.op("pe", lambda e: e.matmul(pb[3][0:64, :], lhsT=tri_b, rhs=R2L, start=True, stop=False),
                             reads=["cb16", "R2L"], writes=[pk[3]], inc=False)
                        S.op("pe", lambda e: e.matmul(pb[3][0:64, :], lhsT=i64b, rhs=mLs_b, start=False, stop=True),
                             reads=["cb16", "identb"], writes=[pk[3]])
                        S.op("pe", lambda e: e.matmul(pb[2][0:64, :], lhsT=trigt_b, rhs=R2Q, start=True, stop=False),
                             reads=["cb16", "R2Q"], writes=[pk[2]], inc=False)
                        S.op("pe", lambda e: e.matmul(pb[2][0:64, :], lhsT=i64b, rhs=mUi_b, start=False, stop=True),
                             reads=["cb16", "identb"], writes=[pk[2]])
                        S.op("act", lambda e: e.activation(out=decL, in_=pb[3][0:64, :], func=AF.Exp), reads=[pk[3]], writes=["decL"])
                        S.op("act", lambda e: e.activation(out=decQ, in_=pb[2][0:64, :], func=AF.Exp), reads=[pk[2]], writes=["decQ"])
                        yield
                        S.op("pool", lambda e: e.tensor_tensor(out=h3(decL), in0=h3(decL), in1=tkb("nbet", i), op=ALU.mult),
                             reads=["decL", "nbet"], writes=["decL"])
                        S.op("dve", lambda e: e.tensor_tensor(out=qkTb, in0=pb[1][0:64, :], in1=decQ, op=ALU.mult),
                             reads=[pk[1], "decQ"], writes=["qkT" + pp])
                        S.op("dve", lambda e: e.tensor_tensor(out=N0, in0=pb[0][0:64, :], in1=decL, op=ALU.mult),
                             reads=[pk[0], "decL"], writes=["N0"])
                        yield
                        for h in range(8):
                            hb = slice(h * 64, (h + 1) * 64)
                            S.op("pe", lambda e, hb=hb: e.transpose(pT[0:64, hb], N0[:, hb], i64b),
                                 reads=["N0", "identb"], writes=[pk[7]], inc=(h == 7))
                        S.op("act", lambda e: e.copy(out=N0T, in_=pT[0:64, 0:512]), reads=[pk[7]], writes=["N0T"])
                        S.op("pool", lambda e: e.tensor_tensor(out=h3(TTm), in0=h3(N0T), in1=cb(i64), op=ALU.add),
                             reads=["N0T", "cst"], writes=["TT" + pp])
                        yield
                        Pc, PTc, pkc, ptkc = N0, N0T, "N0", "N0T"
                        for j in range(1, 6):
                            Pn = Pb[j % 2]; PTn = PTb[j % 2]; pkn = "P%d" % (j % 2); ptkn = "PT%d" % (j % 2)
                            for h in range(8):
                                hb = slice(h * 64, (h + 1) * 64)
                                S.op("pe", lambda e, hb=hb, Pc=Pc, PTc=PTc: e.matmul(pb[6][0:64, hb], lhsT=PTc[:, hb], rhs=Pc[:, hb],
                                                                                    start=True, stop=True),
                                     reads=[pkc, ptkc], writes=[pk[6]], inc=(h == 7))
                            S.op("act", lambda e, Pn=Pn: e.copy(out=Pn, in_=pb[6][0:64, :]), reads=[pk[6]], writes=[pkn])
                            if j < 5:
                                for h in range(8):
                                    hb = slice(h * 64, (h + 1) * 64)
                                    S.op("pe", lambda e, hb=hb, Pc=Pc, PTc=PTc: e.matmul(pb[2][0:64, hb], lhsT=Pc[:, hb], rhs=PTc[:, hb],
                                                                                        start=True, stop=True),
                                         reads=[pkc, ptkc], writes=[pk[2]], inc=(h == 7))
                                S.op("act", lambda e, PTn=PTn: e.copy(out=PTn, in_=pb[2][0:64, :]), reads=[pk[2]], writes=[ptkn])
                            yield
                            for h in range(8):
                                hb = slice(h * 64, (h + 1) * 64)
                                S.op("pe", lambda e, hb=hb, Pn=Pn: e.matmul(pb[6][0:64, hb], lhsT=Pn[:, hb], rhs=TTm[:, hb],
                                                                           start=True, stop=True),
                                     reads=[pkn, "TT" + pp], writes=[pk[6]], inc=(h == 7))
                            S.op("dve", lambda e: e.tensor_tensor(out=TTm, in0=pb[6][0:64, :], in1=TTm, op=ALU.add),
                                 reads=[pk[6], "TT" + pp], writes=["TT" + pp])
                            Pc, PTc, pkc, ptkc = Pn, PTn, pkn, ptkn
                            yield
                        S.op("pool", lambda e: e.tensor_tensor(
                            out=egm.rearrange("p (h x) -> p h x", h=8),
                            in0=maskrep.rearrange("p (h x) -> p h x", h=8),
                            in1=egl[:, i * 8:(i + 1) * 8].unsqueeze(2).to_broadcast([128, 8, 64]), op=ALU.mult),
                            reads=["cst", "egl"], writes=["egm" + pp])
                        yield

                    def rec(i):
                        pp = str(i % 2)
                        TTm = TTm2[i % 2]; qkTb = qkTb2[i % 2]; kd = kd2[i % 2]; vb = vb2[i % 2]; egm = egm2[i % 2]
                        tok = slice(i * 64, (i + 1) * 64)
                        for m in range(4):
                            mb = slice(m * 128, (m + 1) * 128)
                            S.op("pe", lambda e, m=m, mb=mb: e.matmul(pb[5][0:64, mb], lhsT=kT[:, m, tok], rhs=SBl[:, mb],
                                                                      start=True, stop=True),
                                 reads=["kT", sbkey], writes=[pk[5]], inc=(m == 3))
                        for m in range(4):
                            mb = slice(m * 128, (m + 1) * 128)
                            S.op("pe", lambda e, m=m, mb=mb: e.matmul(pb[4][0:64, mb], lhsT=qT[:, m, tok], rhs=SBl[:, mb],
                                                                      start=True, stop=True),
                                 reads=["qT", sbkey], writes=[pk[4]], inc=(m == 3))
                        yield
                        S.op("dve", lambda e: e.tensor_tensor(out=h3(t_x), in0=h3(pb[5][0:64, :]), in1=tkb("bg", i), op=ALU.mult),
                             reads=[pk[5], "bg"], writes=["t_x"])
                        S.op("dve", lambda e: e.tensor_tensor(out=r_b, in0=vb, in1=t_x, op=ALU.subtract),
                             reads=["vb" + pp, "t_x"], writes=["r_b"])
                        S.op("dve", lambda e: e.tensor_tensor(out=h3(o1), in0=h3(pb[4][0:64, :]), in1=tkb("gam", i), op=ALU.mult),
                             reads=[pk[4], "gam"], writes=["o1"])
                        yield
                        for h in range(8):
                            hb = slice(h * 64, (h + 1) * 64)
                            S.op("pe", lambda e, hb=hb: e.matmul(pb[5][0:64, hb], lhsT=TTm[:, hb], rhs=r_b[:, hb],
                                                                 start=True, stop=True),
                                 reads=["TT" + pp, "r_b"], writes=[pk[5]], inc=(h == 7))
                        S.op("act", lambda e: e.copy(out=vnew, in_=pb[5][0:64, :]), reads=[pk[5]], writes=["vnew"])
                        yield
                        for m in range(4):
                            mb = slice(m * 128, (m + 1) * 128)
                            S.op("pe", lambda e, mb=mb: e.matmul(pb[4][:, mb], lhsT=kd[:, mb], rhs=vnew[:, mb],
                                                                 start=True, stop=True),
                                 reads=["kd" + pp, "vnew"], writes=[pk[4]], inc=(m == 3))
                        for h in range(8):
                            hb = slice(h * 64, (h + 1) * 64)
                            S.op("pe", lambda e, hb=hb: e.matmul(pb[5][0:64, hb], lhsT=qkTb[:, hb], rhs=vnew[:, hb],
                                                                 start=True, stop=True),
                                 reads=["qkT" + pp, "vnew"], writes=[pk[5]], inc=(h == 7))
                        yield
                        S.op("dve", lambda e: e.tensor_tensor(out=t1, in0=pb[4][:, :], in1=maskrep, op=ALU.mult),
                             reads=[pk[4], "cst"], writes=["t1"])
                        S.op("pool", lambda e: e.tensor_tensor(out=SL[:, :], in0=SL[:, :], in1=egm, op=ALU.mult),
                             reads=[skey, "egm" + pp], writes=[skey])
                        S.op("dve", lambda e: e.tensor_tensor(out=SL[:, :], in0=SL[:, :], in1=t1, op=ALU.add),
                             reads=[skey, "t1"], writes=[skey])
                        S.op("act", lambda e: e.copy(out=SBl[:, :], in_=SL[:, :]), reads=[skey], writes=[sbkey])
                        yield
                        S.op("dve", lambda e: e.tensor_tensor(out=o_tok, in0=o1, in1=pb[5][0:64, :], op=ALU.add),
                             reads=["o1", pk[5]], writes=["o_tok"])
                        S.op("act", lambda e: e.activation(out=osq, in_=o_tok, func=AF.Square), reads=["o_tok"], writes=["osq"])
                        S.op("dve", lambda e: e.tensor_reduce(out=ssr, in_=h3(osq), axis=mybir.AxisListType.X, op=ALU.add),
                             reads=["osq"], writes=["ssr"])
                        S.op("act", lambda e: e.activation(out=rr, in_=ssr, func=AF.Ln, bias=EPS, scale=1.0 / 64),
                             reads=["ssr"], writes=["rr"])
                        S.op("act", lambda e: e.activation(out=rr, in_=rr, func=AF.Exp, scale=-0.5), reads=["rr"], writes=["rr"])
                        S.op("dve", lambda e: e.tensor_tensor(out=h3(on_b), in0=h3(o_tok),
                                                              in1=rr.unsqueeze(2).to_broadcast([64, 8, 64]), op=ALU.mult),
                             reads=["o_tok", "rr"], writes=["on_b"])
                        yield
                        for m in range(4):
                            S.op("pe", lambda e, m=m: e.transpose(pT[:, 512 + m * 64:512 + (m + 1) * 64], on_b[:, m * 128:(m + 1) * 128],
                                                                  i64b),
                                 reads=["on_b", "identb"], writes=[pk[7]], inc=(m == 3))
                        S.op("dve", lambda e: e.scalar_tensor_tensor(
                            out=oT[:, :, tok], in0=pT[:, 512:768].rearrange("p (m c) -> p m c", m=4), scalar=P(l, 146),
                            in1=szT[:, :, tok], op0=ALU.mult, op1=ALU.mult),
                            reads=[pk[7], "szT", "prm"], writes=["oT"])
                        yield

                    for _ in prep(0):
                        pass
                    for i in range(NCH):
                        ga = rec(i)
                        gb = prep(i + 1) if i + 1 < NCH else iter(())
                        da = db = False
                        while not (da and db):
                            if not da:
                                try:
                                    next(ga)
                                except StopIteration:
                                    da = True
                            for _ in range(2):
                                if not db:
                                    try:
                                        next(gb)
                                    except StopIteration:
                                        db = True
                    for dg in range(2):
                        wi, (wO,) = wload([(KD, 512)])
                        for dd in range(4):
                            d = dg * 4 + dd
                            banks = next_banks()
                            proj(banks, lambda k, dd=dd: wO[:, k, dd * 128:(dd + 1) * 128],
                                 lambda k, half: (oT[:, k, half * 512:(half + 1) * 512] if k < 4
                                                  else yT[:, k - 4, half * 512:(half + 1) * 512]),
                                 KD, "w%d" % wi, ["oT", "yT"])
                            for half in range(2):
                                S.op("dve", lambda e, d=d, half=half, b=banks[half]: e.tensor_tensor(
                                    out=xT[:, d, half * 512:(half + 1) * 512], in0=pb[b][:, :],
                                    in1=xT[:, d, half * 512:(half + 1) * 512], op=ALU.add),
                                    reads=[pk[banks[half]], "xT"], writes=["xT"])
                    S.barrier()
                    rmsnorm_to_hT(lambda k: P(l, 8 + k))
                    for g in range(NF // 2):
                        wi, (wV, wG) = wload([(KD, 256)] * 2)
                        for jj in range(2):
                            f = g * 2 + jj
                            C_ = next_set(); n = C_["n"]
                            bV = next_banks()
                            proj(bV, lambda k, jj=jj: wV[:, k, jj * 128:(jj + 1) * 128], hT_rhs, KD, "w%d" % wi, HTK)
                            hal = hal_ff[:, (l * NF + f) * 2:(l * NF + f) * 2 + 2]
                            hkey = "hff%d_%d" % (l, f)
                            halo_in(C_, hal, hkey, 3, first)
                            for half in range(2):
                                S.op("act", lambda e, half=half: e.copy(out=C_["cb"][:, 2 + half * 512:2 + (half + 1) * 512],
                                                                         in_=pb[bV[half]][:, :]),
                                     reads=[pk[bV[half]]], writes=["cb" + n + "h%d" % half])
                            conv(C_, 3, lambda j, f=f: P(l, 76 + f * 3 + j))
                            halo_out(C_, hal, hkey, 3)
                            S.op("act", lambda e: e.activation(out=C_["ac"][:, :], in_=C_["ac"][:, :], func=AF.Silu),
                                 reads=ackeys(C_), writes=ackeys(C_))
                            bG = next_banks()
                            proj(bG, lambda k, jj=jj: wG[:, k, jj * 128:(jj + 1) * 128], hT_rhs, KD, "w%d" % wi, HTK)
                            for half in range(2):
                                S.op("dve", lambda e, f=f, half=half: e.tensor_tensor(
                                    out=actT[:, f, half * 512:(half + 1) * 512], in0=pb[bG[half]][:, :],
                                    in1=C_["ac"][:, half * 512:(half + 1) * 512], op=ALU.mult),
                                    reads=[pk[bG[half]]] + ackeys(C_), writes=["actT"])
                    for d in range(KD):
                        wi, (wD,) = wload([(NF, 128)])
                        banks = next_banks()
                        proj(banks, lambda f: wD[:, f, :], lambda f, half: actT[:, f, half * 512:(half + 1) * 512],
                             NF, "w%d" % wi, ["actT"])
                        for half in range(2):
                            S.op("dve", lambda e, d=d, half=half, b=banks[half]: e.tensor_tensor(
                                out=xT[:, d, half * 512:(half + 1) * 512], in0=pb[b][:, :],
                                in1=xT[:, d, half * 512:(half + 1) * 512], op=ALU.add),
                                reads=[pk[banks[half]], "xT"], writes=["xT"])
                S.barrier()
                rmsnorm_to_hT(lambda k: prm[:, PL * DEPTH + k:PL * DEPTH + k + 1])
                for k in range(KD):
                    S.op("dve", lambda e, k=k: e.scalar_tensor_tensor(
                        out=xT[:, k, :], in0=xT[:, k, :], scalar=prm[:, PL * DEPTH + k:PL * DEPTH + k + 1], in1=rstd[:, :],
                        op0=ALU.mult, op1=ALU.mult), reads=["xT", "rstd0", "rstd1", "prm"] + HTK, writes=["xT"])
                for tb in range(8):
                    st = stage[tb % 2]
                    for dq in range(2):
                        b = 5 + dq
                        for dd in range(4):
                            d = dq * 4 + dd
                            S.op("pe", lambda e, d=d, dd=dd, b=b: e.transpose(
                                pb[b][:, dd * 128:(dd + 1) * 128], xT[:, d, tb * 128:(tb + 1) * 128], ident),
                                reads=["xT", "cst"], writes=[pk[b]], inc=(dd == 3))
                        S.op("act", lambda e, dq=dq, b=b, st=st: e.copy(out=st[:, dq * 512:(dq + 1) * 512], in_=pb[b][:, :]),
                             reads=[pk[b]], writes=["stage%d" % (tb % 2)])
                    S.dma("sp", out_d[row0 + tb * 128: row0 + (tb + 1) * 128, :], st, "so%d" % (tb % 2),
                          reads=["stage%d" % (tb % 2)])
        S.drain_all("sp")
        build.nins = S.nins
    return nc


def make_consts():
    c = np.zeros((128, 128 * 4 + 64 * 4 + 128 + 512), np.float32)
    c[:, 0:128] = np.eye(128)
    c[:, 128:256] = 1.0
    c[0:64, 256:320] = 1.0
    c[64:128, 320:384] = 1.0
    c[63, 384:512] = 1.0
    p = np.arange(64)[:, None]
    f = np.arange(64)[None, :]
    c0 = 512
    c[0:64, c0:c0 + 64] = (p <= f)
    c[0:64, c0 + 64:c0 + 128] = np.where(p > f, 0.0, NEG)
    c[0:64, c0 + 128:c0 + 192] = np.where(f > p, 0.0, NEG)
    c[0:64, c0 + 192:c0 + 256] = np.where(f >= p, 0.0, NEG)
    c1 = c0 + 256
    c[0:64, c1:c1 + 64] = 1.0
    c2 = c1 + 128
    for h in range(8):
        hh = h % 2
        c[hh * 64:(hh + 1) * 64, c2 + h * 64:c2 + (h + 1) * 64] = 1.0
    return c


def make_params(depth, attn_norm, conv_qkv, a_log, dt_bias, head_norm, conv_sc, sc_norm, ffn_norm, conv_ffn, final_norm):
    pr = np.zeros((128, PL * depth + 8), np.float32)
    for l in range(depth):
        o = l * PL
        pr[:, o + 0:o + 8] = attn_norm[l].reshape(8, 128).T
        pr[:, o + 8:o + 16] = ffn_norm[l].reshape(8, 128).T
        pr[:, o + 16:o + 64] = conv_qkv[l].reshape(4, 12, 128).transpose(2, 1, 0).reshape(128, 48)
        pr[:, o + 64:o + 76] = conv_sc[l].reshape(3, 4, 128).transpose(2, 1, 0).reshape(128, 12)
        pr[:, o + 76:o + 142] = conv_ffn[l].reshape(3, NF, 128).transpose(2, 1, 0).reshape(128, 66)
        pr[:, o + 142:o + 146] = sc_norm[l].reshape(4, 128).T
        pr[:, o + 146] = np.tile(head_norm[l], 2)
        pr[:, o + 147:o + 155] = a_log[l][None, :]
        pr[:, o + 155:o + 163] = dt_bias[l][None, :]
    pr[:, PL * depth:PL * depth + 8] = final_norm.reshape(8, 128).T
    return pr


def make_wstream(depth, W):
    tot = layer_stream_cols()
    out = np.empty((depth, 128, tot), np.float32)
    for l in range(depth):
        off = 0
        for job in layer_jobspec():
            for (name, c0, n, K_) in job:
                blk = W[name][l][:, c0:c0 + n].reshape(K_, 128, n).transpose(1, 0, 2).reshape(128, K_ * n)
                out[l, :, off:off + K_ * n] = blk
                off += K_ * n
        assert off == tot
    return out


_cache = {}


def run(x, attn_norm, w_in, conv_qkv, a_log, dt_bias, head_norm, conv_sc, sc_norm, w_out, ffn_norm, w_up,
        conv_ffn, w_down, final_norm, n_cores=8):
    x = np.asarray(x, np.float32)
    B = x.shape[0]
    depth = w_in.shape[0]
    nseq = B // n_cores
    key = (nseq, depth)
    if key not in _cache:
        _cache[key] = build(nseq, depth)
    nc = _cache[key]
    prm = make_params(depth, *(np.asarray(a, np.float32) for a in (attn_norm, conv_qkv, a_log, dt_bias, head_norm,
                                                                     conv_sc, sc_norm, ffn_norm, conv_ffn, final_norm)))
    cst = make_consts()
    shared = {"wst": make_wstream(depth, {"w_in": np.asarray(w_in, np.float32), "w_out": np.asarray(w_out, np.float32),
                                          "w_up": np.asarray(w_up, np.float32), "w_down": np.asarray(w_down, np.float32)}),
              "prm": prm, "cst": cst}
    in_maps = []
    for c in range(n_cores):
        m = dict(shared)
        m["x"] = np.ascontiguousarray(x[c * nseq:(c + 1) * nseq].reshape(nseq * T, D))
        in_maps.append(m)
    res = run_bass_kernel_spmd(nc, in_maps, core_ids=list(range(n_cores)))
    out = np.concatenate([np.asarray(r["out"]).reshape(nseq, T, D) for r in res.results], axis=0)
    if DBG:
        np.save("_dbg.npy", np.asarray(res.results[0]["dbg"]))
    return out.astype(np.float32)


def kernel(x, attn_norm, w_in, conv_qkv, a_log, dt_bias, head_norm, conv_sc, sc_norm, w_out, ffn_norm, w_up,
           conv_ffn, w_down, final_norm):
    return run(x, attn_norm, w_in, conv_qkv, a_log, dt_bias, head_norm, conv_sc, sc_norm, w_out, ffn_norm, w_up,
               conv_ffn, w_down, final_norm, n_cores=8)
```

```python
import numpy as np
from contextlib import ExitStack
import concourse.bass as bass
import concourse.mybir as mybir
from concourse.bass_utils import run_bass_kernel_spmd

F32 = mybir.dt.float32
BF16 = mybir.dt.bfloat16
ALU = mybir.AluOpType
AF = mybir.ActivationFunctionType

D = 1024
KD = 8
T = 2048
TT = 1024
NT = T // TT
NCH = TT // 64
DFF = 2816
NF = 22
EPS = 1e-6
NEG = -30000.0
CQ, CK, CV, CZ, CB, CA, CSB, CSC, CSX = 0, 512, 1024, 1536, 2048, 2056, 2064, 2576, 3088
PL = 163


def layer_jobspec():
    jl = []
    for m in range(4):
        jl.append([("w_in", CSC + m * 128, 128, KD), ("w_in", CSX + m * 128, 128, KD), ("w_in", CSB + m * 128, 128, KD)])
    jl.append([("w_in", CZ, 512, KD)])
    jl.append([("w_in", CB, 16, KD)])
    for c0_ in (CQ, CK, CV):
        jl.append([("w_in", c0_, 512, KD)])
    for dg in range(2):
        jl.append([("w_out", dg * 512, 512, KD)])
    for g in range(NF // 2):
        jl.append([("w_up", g * 256, 256, KD), ("w_up", DFF + g * 256, 256, KD)])
    for d in range(KD):
        jl.append([("w_down", d * 128, 128, NF)])
    return jl


def layer_stream_cols():
    return sum(K_ * n for job in layer_jobspec() for (_, _, n, K_) in job)


class Sched:
    def __init__(self, nc, es):
        self.nc = nc
        self.es = es
        self.eng = {"pe": nc.tensor, "act": nc.scalar, "dve": nc.vector, "pool": nc.gpsimd, "sp": nc.sync}
        self.sem = {n: es.enter_context(nc.semaphore("s_" + n)) for n in self.eng}
        self.cnt = {n: 0 for n in self.eng}
        self.known = {n: {} for n in self.eng}
        self.lastw = {}
        self.readers = {}
        self.dsem = {}
        self.dcnt = {}
        self.pending = {n: False for n in self.eng}
        self.nins = 0

    def _deps(self, reads, writes):
        toks = []
        for k in reads:
            t = self.lastw.get(k)
            if t is not None:
                toks.append(t)
        for k in writes:
            t = self.lastw.get(k)
            if t is not None:
                toks.append(t)
            toks.extend(self.readers.get(k, ()))
        return toks

    def _wait(self, en, toks):
        need = {}
        kn = self.known[en]
        for (s, v) in toks:
            if kn.get(s, 0) >= v:
                continue
            if need.get(s, 0) < v:
                need[s] = v
        for s, v in need.items():
            self.eng[en].wait_ge(s, v)
            kn[s] = v
            self.nins += 1

    def _record(self, tok, reads, writes):
        for k in writes:
            self.lastw[k] = tok
            self.readers[k] = []
        for k in reads:
            self.readers.setdefault(k, []).append(tok)

    def op(self, en, fn, reads=(), writes=(), inc=True):
        toks = self._deps(reads, writes)
        for k in reads:
            if k.startswith("pb"):
                toks.extend(t for t in self.readers.get(k, ()) if t[0] is not self.sem[en])
        self._wait(en, toks)
        ins = fn(self.eng[en])
        s = self.sem[en]
        self.nins += 1
        if inc:
            self.cnt[en] += 1
            ins.then_inc(s, 1)
            tok = (s, self.cnt[en])
            self.pending[en] = False
        else:
            tok = (s, self.cnt[en] + 1)
            self.pending[en] = True
        if en == "pe":
            self.known[en][s] = tok[1]
        self._record(tok, reads, writes)
        return ins

    def dma(self, en, out, in_, slot, reads=(), writes=()):
        if slot not in self.dsem:
            self.dsem[slot] = self.es.enter_context(self.nc.semaphore("d_" + slot))
            self.dcnt[slot] = 0
        self._wait(en, self._deps(reads, writes))
        self.dcnt[slot] += 16
        ins = self.eng[en].dma_start(out=out, in_=in_)
        ins.then_inc(self.dsem[slot], 16)
        self.nins += 1
        tok = (self.dsem[slot], self.dcnt[slot])
        self._record(tok, reads, writes)
        return ins

    def barrier(self):
        for en in self.eng:
            assert not self.pending[en], en
        toks = [(self.sem[o], self.cnt[o]) for o in self.eng if self.cnt[o] > 0]
        toks += [(self.dsem[k], self.dcnt[k]) for k in self.dsem]
        for en in ("pe", "act", "dve", "pool", "sp"):
            self._wait(en, toks)

    def drain_all(self, en):
        toks = [(self.sem[o], self.cnt[o]) for o in self.eng if self.cnt[o] > 0]
        toks += [(self.dsem[k], self.dcnt[k]) for k in self.dsem]
        self._wait(en, toks)


DBG = False


def build(NSEQ, DEPTH):
    nc = bass.Bass("TRN2", target_bir_lowering=False)
    x_d = nc.dram_tensor("x", [NSEQ * T, D], F32, kind="ExternalInput").ap()
    TOTL = layer_stream_cols()
    wst_d = nc.dram_tensor("wst", [DEPTH, 128, TOTL], F32, kind="ExternalInput").ap()
    NPRM = PL * DEPTH + 8
    prm_d = nc.dram_tensor("prm", [128, NPRM], F32, kind="ExternalInput").ap()
    NCST = 128 * 4 + 64 * 4 + 128 + 512
    cst_d = nc.dram_tensor("cst", [128, NCST], F32, kind="ExternalInput").ap()
    out_d = nc.dram_tensor("out", [NSEQ * T, D], F32, kind="ExternalOutput").ap()
    dbg_d = nc.dram_tensor("dbg", [16, 128, 512], F32, kind="ExternalOutput").ap() if DBG else None

    with ExitStack() as es:
        S = Sched(nc, es)

        def sb(name, shape, dt):
            return es.enter_context(nc.sbuf_tensor(name, shape, dt))

        def ps(name, shape, dt):
            return es.enter_context(nc.psum_tensor(name, shape, dt))

        xT = sb("xT", [128, KD, TT], F32)
        arena = sb("arena", [128, 28 * 1024], BF16)
        arenaB = sb("arenaB", [128, 12 * 1024], F32)
        rstd = sb("rstd", [128, TT], F32)
        kqbd_t = sb("kqbd_t", [128, 1024], BF16)
        ctmp = rstd
        HTK = ["hT%d" % k_ for k_ in range(KD)]
        NW = 4
        wbuf = [sb("wbuf%d" % i, [128, 4096], BF16) for i in range(NW)]
        prm = sb("prm_sb", [128, NPRM], F32)
        cst = sb("cst_sb", [128, NCST], F32)
        identb = sb("identb", [128, 128], BF16)
        onesb = sb("onesb", [128, 128], BF16)
        blkb = sb("blkb", [128, 128], BF16)
        Sbd = [sb("Sbd%d" % l, [128, 512], F32) for l in range(DEPTH)]
        Sb = [sb("Sb%d" % l, [128, 512], BF16) for l in range(DEPTH)]
        hal_qkv = sb("hal_qkv", [128, DEPTH * 12 * 3], F32)
        hal_sc = sb("hal_sc", [128, DEPTH * 4 * 2], F32)
        hal_ff = sb("hal_ff", [128, DEPTH * NF * 2], F32)
        tk = {n: sb("tk_" + n, [64, 128], F32) for n in
              ("bet", "nbet", "xg", "g", "gc", "gam", "bg", "ekd", "nega")}
        egl = sb("egl", [128, 128], F32)

        def av(off, n):
            return arena[:, off:off + n]
        qT = av(0, 4096).rearrange("p (m t) -> p m t", m=4)
        kT = av(4096, 4096).rearrange("p (m t) -> p m t", m=4)
        vT = av(8192, 4096).rearrange("p (m t) -> p m t", m=4)
        yT = av(12288, 4096).rearrange("p (m t) -> p m t", m=4)
        oT = av(16384, 4096).rearrange("p (m t) -> p m t", m=4)
        szT = av(20480, 8192).bitcast(F32).rearrange("p (m t) -> p m t", m=4)
        actT = av(0, NF * TT).rearrange("p (f t) -> p f t", f=NF)
        stage = [av(22528 + i * 2048, 2048).bitcast(F32) for i in range(2)]
        hT = arenaB[:, 0:4096].bitcast(BF16).rearrange("p (k t) -> p k t", k=KD)
        def bv(off, n, dt=F32, parts=64):
            a = arenaB[0:parts, off:off + n]
            return a.bitcast(dt) if dt is not F32 else a
        o_ = [0]
        def balloc(n_f32, dt=F32, parts=64):
            a = bv(o_[0], n_f32, dt, parts)
            o_[0] += n_f32
            return a
        decL = balloc(512); decQ = balloc(512)
        R2L = balloc(256, BF16); R2Q = balloc(256, BF16)
        N0 = balloc(256, BF16); N0T = balloc(256, BF16)
        Pb = [balloc(256, BF16) for _ in range(2)]
        PTb = [balloc(256, BF16) for _ in range(2)]
        TTm2 = [balloc(256, BF16) for _ in range(2)]
        qkTb2 = [balloc(256, BF16) for _ in range(2)]
        kd2 = [balloc(256, BF16) for _ in range(2)]
        vb2 = [balloc(512) for _ in range(2)]
        t_x = balloc(512)
        r_b = balloc(256, BF16)
        vnew = balloc(256, BF16)
        t1 = balloc(512, F32, 128)
        o1 = balloc(512); o_tok = balloc(512); osq = balloc(512)
        on_b = balloc(256, BF16)
        egm2 = [balloc(512, F32, 128) for _ in range(2)]
        ssr = balloc(8); rr = balloc(8)
        kqbd = kqbd_t[:, :]
        assert o_[0] <= 11 * 1024, o_[0]
        CHS = []
        off_ = 4096
        for s_ in range(3):
            cb_ = arenaB[:, off_:off_ + TT + 4]; off_ += TT + 4
            ac_ = arenaB[:, off_:off_ + TT]; off_ += TT
            sq_ = arenaB[:, off_:off_ + TT // 2].bitcast(BF16); off_ += TT // 2
            CHS.append(dict(cb=cb_, ac=ac_, sq=sq_, n="%d" % s_))
        assert off_ <= 12 * 1024
        chn = [0]
        def next_set():
            c_ = CHS[chn[0] % 3]
            chn[0] += 1
            return c_
        bkn = [0]
        def next_banks():
            b_ = ((0, 1), (2, 3), (4, 5))[bkn[0] % 3]
            bkn[0] += 1
            return b_
        pend = []
        def flushA(keep=0):
            for it in pend[:max(0, len(pend) - keep)]:
                if not it[1]:
                    next(it[0])
                    it[1] = True
        def flushB(keep=0):
            while len(pend) > keep:
                it = pend.pop(0)
                if not it[1]:
                    next(it[0])
                for _ in it[0]:
                    pass
        def flush(keep=0):
            flushA(keep)
            flushB(keep)

        pb = [ps("pb%d" % i, [128, 512], F32) for i in range(7)] + [ps("pb7", [128, 1024], BF16)]
        pk = ["pb%d" % i for i in range(8)]

        S.dma("sp", prm[:], prm_d, "prm", writes=["prm"])
        S.dma("sp", cst[:], cst_d, "cst", writes=["cst"])
        ident = cst[:, 0:128]
        ones_f = cst[:, 128:256]
        blk_f = cst[:, 256:384]
        sellast = cst[0:64, 384:512]
        c0 = 512
        tri = cst[0:64, c0:c0 + 64]
        maskLs = cst[0:64, c0 + 64:c0 + 128]
        maskUs = cst[0:64, c0 + 128:c0 + 192]
        maskUi = cst[0:64, c0 + 192:c0 + 256]
        c1 = c0 + 256
        ones64 = cst[0:64, c1:c1 + 64]
        i64 = cst[0:64, 0:64]
        c2 = c1 + 128
        maskrep = cst[:, c2:c2 + 512]
        S.op("act", lambda e: e.copy(out=identb[:], in_=ident), reads=["cst"], writes=["identb"])
        S.op("act", lambda e: e.copy(out=onesb[:], in_=ones_f), reads=["cst"], writes=["onesb"])
        S.op("act", lambda e: e.copy(out=blkb[:], in_=blk_f), reads=["cst"], writes=["blkb"])
        S.op("pool", lambda e: e.memset(kqbd, 0.0), writes=["kqbd"])
        cb16 = sb("cb16", [64, 128 + 1024], BF16)
        trigt_f = sb("trigt_f", [64, 64], F32)
        tri_b = cb16[:, 0:64]
        trigt_b = cb16[:, 64:128]
        mLs_b = cb16[:, 128:640]
        mUi_b = cb16[:, 640:1152]
        S.op("dve", lambda e: e.tensor_scalar(out=trigt_f[:, :], in0=maskLs, scalar1=1.0 / 30000.0, scalar2=1.0,
                                              op0=ALU.mult, op1=ALU.add), reads=["cst"], writes=["cb16"])
        S.op("act", lambda e: e.copy(out=tri_b, in_=tri), reads=["cst"], writes=["cb16"])
        S.op("act", lambda e: e.copy(out=trigt_b, in_=trigt_f[:, :]), reads=["cb16"], writes=["cb16"])
        S.op("act", lambda e: e.copy(out=mLs_b.rearrange("p (h x) -> p h x", h=8),
                                     in_=maskLs.unsqueeze(1).to_broadcast([64, 8, 64])), reads=["cst"], writes=["cb16"])
        S.op("act", lambda e: e.copy(out=mUi_b.rearrange("p (h x) -> p h x", h=8),
                                     in_=maskUi.unsqueeze(1).to_broadcast([64, 8, 64])), reads=["cst"], writes=["cb16"])

        def P(l, off, n=1):
            return prm[:, l * PL + off: l * PL + off + n]

        wjobs = []
        wstate = {"use": 0, "iss": 0}
        LOOK = 2
        def wviews(j):
            i = j % NW
            views = []
            off = 0
            for (K_, n_) in wjobs[j][2]:
                views.append(wbuf[i][:, off:off + K_ * n_].rearrange("p (k n) -> p k n", k=K_))
                off += K_ * n_
            assert off <= 4096
            return i, views, off
        def wissue(j):
            i, views, sz = wviews(j)
            l_, off_, _ = wjobs[j]
            S.dma("pool", wbuf[i][:, 0:sz], wst_d[l_][:, off_:off_ + sz], "w%d" % i, writes=["w%d" % i])
        def wload(shapes):
            j = wstate["use"]
            assert list(shapes) == list(wjobs[j][2]), (shapes, wjobs[j])
            while wstate["iss"] < len(wjobs) and wstate["iss"] <= j + LOOK:
                wissue(wstate["iss"])
                wstate["iss"] += 1
            wstate["use"] += 1
            i, views, _ = wviews(j)
            return i, views

        def proj(banks, lhs_fn, rhs, K_, wkey, rkeys):
            for half in range(2):
                b = banks[half]
                for k in range(K_):
                    S.op("pe", lambda e, k=k, half=half, b=b: e.matmul(
                        pb[b][:, :], lhsT=lhs_fn(k), rhs=rhs(k, half), start=(k == 0), stop=(k == K_ - 1)),
                        reads=[wkey] + rkeys, writes=[pk[b]], inc=(k == K_ - 1))

        def hT_rhs(k, half):
            return hT[:, k, half * 512:(half + 1) * 512]

        def rsqrt_from(psbank, half, out_ap, scale, key_out):
            S.op("act", lambda e: e.activation(out=out_ap, in_=pb[psbank][:, :], func=AF.Ln, bias=EPS, scale=scale),
                 reads=[pk[psbank]], writes=[key_out])
            S.op("act", lambda e: e.activation(out=out_ap, in_=out_ap, func=AF.Exp, scale=-0.5),
                 reads=[key_out], writes=[key_out])

        def rmsnorm_to_hT(gcol_fn):
            for k in range(KD):
                S.op("act", lambda e, k=k: e.activation(out=hT[:, k, :], in_=xT[:, k, :], func=AF.Square),
                     reads=["xT"], writes=["hT%d" % k])
            bk = next_banks()
            for half in range(2):
                for k in range(KD):
                    S.op("pe", lambda e, k=k, half=half: e.matmul(
                        pb[bk[half]][:, :], lhsT=onesb[:], rhs=hT[:, k, half * 512:(half + 1) * 512],
                        start=(k == 0), stop=(k == KD - 1)),
                        reads=["onesb", "hT%d" % k], writes=[pk[bk[half]]], inc=(k == KD - 1))
                rsqrt_from(bk[half], half, rstd[:, half * 512:(half + 1) * 512], 1.0 / D, "rstd%d" % half)
            for k in range(KD):
                S.op("dve", lambda e, k=k: e.scalar_tensor_tensor(
                    out=hT[:, k, :], in0=xT[:, k, :], scalar=gcol_fn(k), in1=rstd[:, :],
                    op0=ALU.mult, op1=ALU.mult), reads=["xT", "rstd0", "rstd1", "prm"], writes=["hT%d" % k])

        def conv(C_, K_, wcol_fn):
            n = C_["n"]
            sp = 896 if K_ == 4 else 768
            wp = TT - sp
            rk = ["cb" + n + "h0", "cb" + n + "h1", "cb" + n + "hal", "prm"]
            hk = ["ac" + n + "h0", "ac" + n + "h1"]
            S.op("dve", lambda e: e.tensor_scalar(out=C_["ac"][:, 0:sp], in0=C_["cb"][:, 0:sp],
                                                  scalar1=wcol_fn(0), scalar2=None, op0=ALU.mult),
                 reads=rk, writes=["ac" + n + "c0"] + hk)
            for j in range(1, K_):
                S.op("dve", lambda e, j=j: e.scalar_tensor_tensor(
                    out=C_["ac"][:, 0:sp], in0=C_["cb"][:, j:j + sp], scalar=wcol_fn(j),
                    in1=C_["ac"][:, 0:sp], op0=ALU.mult, op1=ALU.add), reads=rk, writes=["ac" + n + "c0"])
            tmp = C_["sq"].bitcast(F32)[:, 0:wp]
            S.op("pool", lambda e: e.tensor_tensor(out=C_["ac"][:, sp:TT], in0=C_["cb"][:, sp:TT],
                                                   in1=wcol_fn(0).to_broadcast([128, wp]), op=ALU.mult),
                 reads=rk, writes=["ac" + n + "c1"] + hk)
            for j in range(1, K_):
                S.op("pool", lambda e, j=j: e.tensor_tensor(out=tmp, in0=C_["cb"][:, sp + j:TT + j],
                                                            in1=wcol_fn(j).to_broadcast([128, wp]), op=ALU.mult),
                     reads=rk, writes=["sq" + n])
                S.op("pool", lambda e: e.tensor_tensor(out=C_["ac"][:, sp:TT], in0=C_["ac"][:, sp:TT], in1=tmp,
                                                       op=ALU.add), reads=["sq" + n], writes=["ac" + n + "c1"])

        def ackeys(C_):
            return ["ac" + C_["n"] + "h0", "ac" + C_["n"] + "h1", "ac" + C_["n"] + "c0", "ac" + C_["n"] + "c1"]

        def cbkeys(C_):
            return ["cb" + C_["n"] + "h0", "cb" + C_["n"] + "h1", "cb" + C_["n"] + "hal"]

        def halo_in(C_, hal_ap, hkey, K_, first):
            n = C_["n"]
            if first:
                S.op("pool", lambda e: e.memset(C_["cb"][:, 0:K_ - 1], 0.0), writes=["cb" + n + "hal"])
            else:
                S.op("pool", lambda e: e.tensor_copy(out=C_["cb"][:, 0:K_ - 1], in_=hal_ap), reads=[hkey],
                     writes=["cb" + n + "hal"])

        def halo_out(C_, hal_ap, hkey, K_):
            n = C_["n"]
            S.op("pool", lambda e: e.tensor_copy(out=hal_ap, in_=C_["cb"][:, TT:TT + K_ - 1]), reads=["cb" + n + "h1"],
                 writes=[hkey])

        for _s in range(NSEQ):
            for _t in range(NT):
                for l in range(DEPTH):
                    off_ = 0
                    for job in layer_jobspec():
                        shp = [(K_, n) for (_, _, n, K_) in job]
                        wjobs.append((l, off_, shp))
                        off_ += sum(K_ * n for (K_, n) in shp)

        for sq_i in range(NSEQ):
            for ti in range(NT):
                first = (ti == 0)
                row0 = sq_i * T + ti * TT
                S.barrier()
                for tb in range(8):
                    st = stage[tb % 2]
                    S.dma("sp", st, x_d[row0 + tb * 128: row0 + (tb + 1) * 128, :], "st%d" % (tb % 2),
                          writes=["stage%d" % (tb % 2)])
                    for dq in range(2):
                        b = 5 + dq
                        for dd in range(4):
                            d = dq * 4 + dd
                            S.op("pe", lambda e, d=d, dd=dd, b=b, st=st: e.transpose(
                                pb[b][:, dd * 128:(dd + 1) * 128], st[:, d * 128:(d + 1) * 128], ident),
                                reads=["stage%d" % (tb % 2), "cst"], writes=[pk[b]], inc=(dd == 3))
                        S.op("act", lambda e, dq=dq, b=b, tb=tb: e.copy(
                            out=xT[:, dq * 4:(dq + 1) * 4, tb * 128:(tb + 1) * 128],
                            in_=pb[b][:, :].rearrange("p (d t) -> p d t", d=4)),
                            reads=[pk[b]], writes=["xT"])
                if first:
                    for l in range(DEPTH):
                        S.op("pool", lambda e, l=l: e.memset(Sbd[l][:], 0.0), writes=["Sbd%d" % l])
                        S.op("pool", lambda e, l=l: e.memset(Sb[l][:], 0.0), writes=["Sb%d" % l])

                for l in range(DEPTH):
                    S.barrier()
                    rmsnorm_to_hT(lambda k: P(l, 0 + k))
                    for m in range(4):
                        wi, (wC, wX, wB) = wload([(KD, 128)] * 3)
                        wkey = "w%d" % wi
                        C_ = next_set(); n = C_["n"]
                        bC = next_banks()
                        proj(bC, lambda k: wC[:, k, :], hT_rhs, KD, wkey, HTK)
                        for half in range(2):
                            S.op("act", lambda e, half=half: e.copy(out=C_["ac"][:, half * 512:(half + 1) * 512],
                                                                     in_=pb[bC[half]][:, :]),
                                 reads=[pk[bC[half]]], writes=["ac" + n + "h%d" % half])
                        flushA(keep=1)
                        bX = next_banks()
                        proj(bX, lambda k: wX[:, k, :], hT_rhs, KD, wkey, HTK)
                        hal = hal_sc[:, (l * 4 + m) * 2:(l * 4 + m) * 2 + 2]
                        hkey = "hsc%d_%d" % (l, m)
                        halo_in(C_, hal, hkey, 3, first)
                        for half in range(2):
                            S.op("dve", lambda e, half=half: e.tensor_tensor(
                                out=C_["cb"][:, 2 + half * 512:2 + (half + 1) * 512], in0=pb[bX[half]][:, :],
                                in1=C_["ac"][:, half * 512:(half + 1) * 512], op=ALU.mult),
                                reads=[pk[bX[half]], "ac" + n + "h%d" % half], writes=["cb" + n + "h%d" % half])
                        conv(C_, 3, lambda j: P(l, 64 + m * 3 + j))
                        halo_out(C_, hal, hkey, 3)
                        bB = next_banks()
                        proj(bB, lambda k: wB[:, k, :], hT_rhs, KD, wkey, HTK)
                        for half in range(2):
                            S.op("dve", lambda e, half=half: e.tensor_tensor(
                                out=C_["cb"][:, half * 512:(half + 1) * 512], in0=pb[bB[half]][:, :],
                                in1=C_["ac"][:, half * 512:(half + 1) * 512], op=ALU.mult),
                                reads=[pk[bB[half]]] + ackeys(C_) + cbkeys(C_), writes=cbkeys(C_))
                        S.op("pool", lambda e: e.tensor_tensor(out=C_["sq"][:, :], in0=C_["cb"][:, 0:TT], in1=C_["cb"][:, 0:TT],
                                                               op=ALU.mult), reads=cbkeys(C_), writes=["sq" + n])
                        flush(keep=1)
                        def st2(C_=C_, n=n, m=m):
                            bS = next_banks()
                            for half in range(2):
                                S.op("pe", lambda e, half=half: e.matmul(pb[bS[half]][:, :], lhsT=blkb[:],
                                                                          rhs=C_["sq"][:, half * 512:(half + 1) * 512],
                                                                          start=True, stop=True),
                                     reads=["blkb", "sq" + n], writes=[pk[bS[half]]])
                                rsqrt_from(bS[half], half, C_["ac"][:, half * 512:(half + 1) * 512], 1.0 / 64, "ac" + n + "h%d" % half)
                            yield
                            S.op("dve", lambda e: e.scalar_tensor_tensor(
                                out=yT[:, m, :], in0=C_["cb"][:, 0:TT], scalar=P(l, 142 + m), in1=C_["ac"][:, :],
                                op0=ALU.mult, op1=ALU.mult), reads=cbkeys(C_) + ackeys(C_) + ["prm"], writes=["yT"])
                        pend.append([st2(), False])
                    wi, (wZ,) = wload([(KD, 512)])
                    for m in range(4):
                        bZ = next_banks()
                        proj(bZ, lambda k, m=m: wZ[:, k, m * 128:(m + 1) * 128], hT_rhs, KD, "w%d" % wi, HTK)
                        for half in range(2):
                            S.op("act", lambda e, half=half, m=m: e.activation(
                                out=szT[:, m, half * 512:(half + 1) * 512], in_=pb[bZ[half]][:, :], func=AF.Silu),
                                reads=[pk[bZ[half]]], writes=["szT"])
                        flush()
                    wi, (wBA,) = wload([(KD, 16)])
                    for i in range(NCH):
                        for k in range(KD):
                            S.op("pe", lambda e, i=i, k=k: e.matmul(
                                pb[4][0:64, i * 16:(i + 1) * 16], lhsT=hT[:, k, i * 64:(i + 1) * 64], rhs=wBA[:, k, :],
                                start=(k == 0), stop=(k == KD - 1)),
                                reads=["w%d" % wi] + HTK, writes=[pk[4]], inc=(k == KD - 1))
                    bav = pb[4][0:64, 0:256].rearrange("p (i c) -> p i c", c=16)
                    def tv(n):
                        return tk[n][:, :].rearrange("p (i h) -> p i h", h=8)
                    S.op("act", lambda e: e.activation(out=tv("bet"), in_=bav[:, :, 0:8], func=AF.Sigmoid),
                         reads=[pk[4]], writes=["bet"])
                    S.op("dve", lambda e: e.tensor_scalar(out=tk["nbet"][:, :], in0=tk["bet"][:, :], scalar1=-1.0, scalar2=None,
                                                          op0=ALU.mult), reads=["bet"], writes=["nbet"])
                    S.op("dve", lambda e: e.tensor_tensor(
                        out=tv("xg"), in0=bav[:, :, 8:16],
                        in1=prm[0:64, l * PL + 155:l * PL + 163].unsqueeze(1).to_broadcast([64, NCH, 8]), op=ALU.add),
                        reads=[pk[4], "prm"], writes=["xg"])
                    S.op("act", lambda e: e.activation(out=tk["xg"][:, :], in_=tk["xg"][:, :], func=AF.Exp),
                         reads=["xg"], writes=["xg"])
                    S.op("act", lambda e: e.activation(out=tk["xg"][:, :], in_=tk["xg"][:, :], func=AF.Ln, bias=1.0),
                         reads=["xg"], writes=["xg"])
                    S.op("act", lambda e: e.activation(out=tk["nega"][:, 0:8], in_=prm[0:64, l * PL + 147:l * PL + 155],
                                                       func=AF.Exp), reads=["prm"], writes=["nega"])
                    S.op("dve", lambda e: e.scalar_tensor_tensor(
                        out=tv("g"), in0=tv("xg"), scalar=-1.0,
                        in1=tk["nega"][:, 0:8].unsqueeze(1).to_broadcast([64, NCH, 8]), op0=ALU.mult, op1=ALU.mult),
                        reads=["xg", "nega"], writes=["g"])
                    S.op("pe", lambda e: e.matmul(pb[5][0:64, 0:128], lhsT=tri, rhs=tk["g"][:, :], start=True, stop=True),
                         reads=["cst", "g"], writes=[pk[5]])
                    S.op("act", lambda e: e.copy(out=tk["gc"][:, :], in_=pb[5][0:64, 0:128]), reads=[pk[5]], writes=["gc"])
                    S.op("pe", lambda e: e.matmul(pb[4][:, 0:128], lhsT=sellast, rhs=tk["gc"][:, :], start=True, stop=True),
                         reads=["cst", "gc"], writes=[pk[4]])
                    S.op("act", lambda e: e.activation(out=egl[:, :], in_=pb[4][:, 0:128], func=AF.Exp),
                         reads=[pk[4]], writes=["egl"])
                    S.op("dve", lambda e: e.tensor_tensor(out=tk["ekd"][:, :], in0=pb[4][0:64, 0:128], in1=tk["gc"][:, :],
                                                          op=ALU.subtract), reads=[pk[4], "gc"], writes=["ekd"])
                    S.op("act", lambda e: e.activation(out=tk["ekd"][:, :], in_=tk["ekd"][:, :], func=AF.Exp),
                         reads=["ekd"], writes=["ekd"])
                    S.op("act", lambda e: e.activation(out=tk["gam"][:, :], in_=tk["gc"][:, :], func=AF.Exp),
                         reads=["gc"], writes=["gam"])
                    S.op("dve", lambda e: e.tensor_tensor(out=tk["bg"][:, :], in0=tk["bet"][:, :], in1=tk["gam"][:, :],
                                                          op=ALU.mult), reads=["bet", "gam"], writes=["bg"])
                    flush()
                    s1b_prev = [None]
                    for ty, (c0_, dst) in enumerate(((CQ, qT), (CK, kT), (CV, vT))):
                        wi, (wQ,) = wload([(KD, 512)])
                        for m in range(4):
                            ch = ty * 4 + m
                            C_ = next_set(); n = C_["n"]
                            bQ = next_banks()
                            proj(bQ, lambda k, m=m: wQ[:, k, m * 128:(m + 1) * 128], hT_rhs, KD, "w%d" % wi, HTK)
                            hal = hal_qkv[:, (l * 12 + ch) * 3:(l * 12 + ch) * 3 + 3]
                            hkey = "hqkv%d_%d" % (l, ch)
                            halo_in(C_, hal, hkey, 4, first)
                            for half in range(2):
                                S.op("act", lambda e, half=half: e.copy(
                                    out=C_["cb"][:, 3 + half * 512:3 + (half + 1) * 512], in_=pb[bQ[half]][:, :]),
                                    reads=[pk[bQ[half]]], writes=["cb" + n + "h%d" % half])
                            flushA(0)
                            if s1b_prev[0] is not None:
                                s1b_prev[0]()
                            conv(C_, 4, lambda j, ch=ch: P(l, 16 + ch * 4 + j))
                            halo_out(C_, hal, hkey, 4)
                            flushB(keep=1 if ty < 2 else 0)

                            def s1b(C_=C_, n=n, m=m, ty=ty):
                                if ty == 2:
                                    S.op("act", lambda e: e.activation(out=vT[:, m, :], in_=C_["ac"][:, :], func=AF.Silu),
                                         reads=ackeys(C_), writes=["vT"])
                                    return
                                S.op("act", lambda e: e.activation(out=C_["ac"][:, :], in_=C_["ac"][:, :], func=AF.Silu),
                                     reads=ackeys(C_), writes=ackeys(C_))
                                S.op("pool", lambda e: e.tensor_tensor(out=C_["sq"][:, :], in0=C_["ac"][:, :], in1=C_["ac"][:, :],
                                                                       op=ALU.mult), reads=ackeys(C_), writes=["sq" + n])

                                def st2():
                                    bS = next_banks()
                                    for half in range(2):
                                        S.op("pe", lambda e, half=half: e.matmul(pb[bS[half]][:, :], lhsT=blkb[:],
                                                                                  rhs=C_["sq"][:, half * 512:(half + 1) * 512],
                                                                                  start=True, stop=True),
                                             reads=["blkb", "sq" + n], writes=[pk[bS[half]]])
                                        rsqrt_from(bS[half], half, C_["cb"][:, half * 512:(half + 1) * 512], 1.0,
                                                   "cb" + n + "h%d" % half)
                                    yield
                                    if ty == 0:
                                        S.op("dve", lambda e: e.scalar_tensor_tensor(
                                            out=qT[:, m, :], in0=C_["ac"][:, :], scalar=0.125, in1=C_["cb"][:, 0:TT],
                                            op0=ALU.mult, op1=ALU.mult), reads=ackeys(C_) + cbkeys(C_), writes=["qT"])
                                    else:
                                        S.op("dve", lambda e: e.tensor_tensor(
                                            out=kT[:, m, :], in0=C_["ac"][:, :], in1=C_["cb"][:, 0:TT], op=ALU.mult),
                                            reads=ackeys(C_) + cbkeys(C_), writes=["kT"])
                                pend.append([st2(), False])
                            s1b_prev[0] = s1b
                    if s1b_prev[0] is not None:
                        s1b_prev[0]()
                    flush()
                    S.barrier()
                    SL = Sbd[l]; SBl = Sb[l]; skey = "Sbd%d" % l; sbkey = "Sb%d" % l
                    def h3(ap):
                        return ap.rearrange("p (h x) -> p h x", h=8)
                    def tkb(n, i, parts=64):
                        return tk[n][0:parts, i * 8:(i + 1) * 8].unsqueeze(2).to_broadcast([parts, 8, 64])
                    def cb(ap):
                        return ap.unsqueeze(1).to_broadcast([64, 8, 64])
                    i64b = identb[0:64, 0:64]
                    pT = pb[7][:, :]

                    def prep(i):
                        pp = str(i % 2)
                        TTm = TTm2[i % 2]; qkTb = qkTb2[i % 2]; kd = kd2[i % 2]; vb = vb2[i % 2]; egm = egm2[i % 2]
                        tok = slice(i * 64, (i + 1) * 64)
                        kq5 = kqbd.rearrange("p (w m hh x) -> p w m hh x", w=2, m=4, hh=2)
                        for w_, src, skey_ in ((0, kT, "kT"), (1, qT, "qT")):
                            for hh in range(2):
                                S.op("pool", lambda e, w_=w_, src=src, hh=hh: e.tensor_copy(
                                    out=kq5[hh * 64:(hh + 1) * 64, w_, :, hh, :], in_=src[hh * 64:(hh + 1) * 64, :, tok]),
                                    reads=[skey_], writes=["kqbd"])
                        kq3 = kqbd.rearrange("p (w m n) -> p w m n", w=2, m=4)
                        for w_ in range(2):
                            for m in range(4):
                                S.op("pe", lambda e, w_=w_, m=m: e.matmul(
                                    pb[w_][0:64, m * 128:(m + 1) * 128], lhsT=kT[:, m, tok], rhs=kq3[:, w_, m, :],
                                    start=True, stop=True), reads=["kT", "kqbd"], writes=[pk[w_]], inc=(m == 3))
                        for w_, src, skey_ in ((0, kT, "kT"), (1, vT, "vT")):
                            for m in range(4):
                                S.op("pe", lambda e, w_=w_, m=m, src=src: e.transpose(
                                    pT[0:64, w_ * 512 + m * 128: w_ * 512 + (m + 1) * 128], src[:, m, tok], identb[:]),
                                    reads=[skey_, "identb"], writes=[pk[7]], inc=(w_ == 1 and m == 3))
                        yield
                        S.op("pool", lambda e: e.tensor_tensor(out=h3(R2L), in0=cb(trigt_f[:, :]), in1=tkb("g", i), op=ALU.mult),
                             reads=["cb16", "g"], writes=["R2L"])
                        S.op("pool", lambda e: e.tensor_tensor(out=h3(R2Q), in0=cb(tri), in1=tkb("g", i), op=ALU.mult),
                             reads=["cst", "g"], writes=["R2Q"])
                        S.op("dve", lambda e: e.tensor_tensor(out=h3(kd), in0=h3(pT[0:64, 0:512]), in1=tkb("ekd", i),
                                                              op=ALU.mult), reads=[pk[7], "ekd"], writes=["kd" + pp])
                        S.op("dve", lambda e: e.tensor_tensor(out=h3(vb), in0=h3(pT[0:64, 512:1024]), in1=tkb("bet", i),
                                                              op=ALU.mult), reads=[pk[7], "bet"], writes=["vb" + pp])
                        yield
                        S.op("pe", lambda e: e.matmul(pb[3][0:64, :], lhsT=tri_b, rhs=R2L, start=True, stop=False),
                             reads=["cb16", "R2L"], writes=[pk[3]], inc=False)
                        S.op("pe", lambda e: e.matmul(pb[3][0:64, :], lhsT=i64b, rhs=mLs_b, start=False, stop=True),
                             reads=["cb16", "identb"], writes=[pk[3]])
                        S.op("pe", lambda e: e.matmul(pb[2][0:64, :], lhsT=trigt_b, rhs=R2Q, start=True, stop=False),
                             reads=["cb16", "R2Q"], writes=[pk[2]], inc=False)
                        S.op("pe", lambda e: e.matmul(pb[2][0:64, :], lhsT=i64b, rhs=mUi_b, start=False, stop=True),
                             reads=["cb16", "identb"], writes=[pk[2]])
                        S.op("act", lambda e: e.activation(out=decL, in_=pb[3][0:64, :], func=AF.Exp), reads=[pk[3]], writes=["decL"])
                        S.op("act", lambda e: e.activation(out=decQ, in_=pb[2][0:64, :], func=AF.Exp), reads=[pk[2]], writes=["decQ"])
                        yield
                        S.op("pool", lambda e: e.tensor_tensor(out=h3(decL), in0=h3(decL), in1=tkb("nbet", i), op=ALU.mult),
                             reads=["decL", "nbet"], writes=["decL"])
                        S.op("dve", lambda e: e.tensor_tensor(out=qkTb, in0=pb[1][0:64, :], in1=decQ, op=ALU.mult),
                             reads=[pk[1], "decQ"], writes=["qkT" + pp])
                        S.op("dve", lambda e: e.tensor_tensor(out=N0, in0=pb[0][0:64, :], in1=decL, op=ALU.mult),
                             reads=[pk[0], "decL"], writes=["N0"])
                        yield
                        for h in range(8):
                            hb = slice(h * 64, (h + 1) * 64)
                            S.op("pe", lambda e, hb=hb: e.transpose(pT[0:64, hb], N0[:, hb], i64b),
                                 reads=["N0", "identb"], writes=[pk[7]], inc=(h == 7))
                        S.op("act", lambda e: e.copy(out=N0T, in_=pT[0:64, 0:512]), reads=[pk[7]], writes=["N0T"])
                        S.op("pool", lambda e: e.tensor_tensor(out=h3(TTm), in0=h3(N0T), in1=cb(i64), op=ALU.add),
                             reads=["N0T", "cst"], writes=["TT" + pp])
                        yield
                        Pc, PTc, pkc, ptkc = N0, N0T, "N0", "N0T"
                        for j in range(1, 6):
                            Pn = Pb[j % 2]; PTn = PTb[j % 2]; pkn = "P%d" % (j % 2); ptkn = "PT%d" % (j % 2)
                            for h in range(8):
                                hb = slice(h * 64, (h + 1) * 64)
                                S.op("pe", lambda e, hb=hb, Pc=Pc, PTc=PTc: e.matmul(pb[6][0:64, hb], lhsT=PTc[:, hb], rhs=Pc[:, hb],
                                                                                    start=True, stop=True),
                                     reads=[pkc, ptkc], writes=[pk[6]], inc=(h == 7))
                            S.op("act", lambda e, Pn=Pn: e.copy(out=Pn, in_=pb[6][0:64, :]), reads=[pk[6]], writes=[pkn])
                            if j < 5:
                                for h in range(8):
                                    hb = slice(h * 64, (h + 1) * 64)
                                    S.op("pe", lambda e, hb=hb, Pc=Pc, PTc=PTc: e.matmul(pb[2][0:64, hb], lhsT=Pc[:, hb], rhs=PTc[:, hb],
                                                                                        start=True, stop=True),
                                         reads=[pkc, ptkc], writes=[pk[2]], inc=(h == 7))
                                S.op("act", lambda e, PTn=PTn: e.copy(out=PTn, in_=pb[2][0:64, :]), reads=[pk[2]], writes=[ptkn])
                            yield
                            for h in range(8):
                                hb = slice(h * 64, (h + 1) * 64)
                                S.op("pe", lambda e, hb=hb, Pn=Pn: e.matmul(pb[6][0:64, hb], lhsT=Pn[:, hb], rhs=TTm[:, hb],
                                                                           start=True, stop=True),
                                     reads=[pkn, "TT" + pp], writes=[pk[6]], inc=(h == 7))
                            S.op("dve", lambda e: e.tensor_tensor(out=TTm, in0=pb[6][0:64, :], in1=TTm, op=ALU.add),
                                 reads=[pk[6], "TT" + pp], writes=["TT" + pp])
                            Pc, PTc, pkc, ptkc = Pn, PTn, pkn, ptkn
                            yield
                        S.op("pool", lambda e: e.tensor_tensor(
                            out=egm.rearrange("p (h x) -> p h x", h=8),
                            in0=maskrep.rearrange("p (h x) -> p h x", h=8),
                            in1=egl[:, i * 8:(i + 1) * 8].unsqueeze(2).to_broadcast([128, 8, 64]), op=ALU.mult),
                            reads=["cst", "egl"], writes=["egm" + pp])
                        yield

                    def rec(i):
                        pp = str(i % 2)
                        TTm = TTm2[i % 2]; qkTb = qkTb2[i % 2]; kd = kd2[i % 2]; vb = vb2[i % 2]; egm = egm2[i % 2]
                        tok = slice(i * 64, (i + 1) * 64)
                        for m in range(4):
                            mb = slice(m * 128, (m + 1) * 128)
                            S.op("pe", lambda e, m=m, mb=mb: e.matmul(pb[5][0:64, mb], lhsT=kT[:, m, tok], rhs=SBl[:, mb],
                                                                      start=True, stop=True),
                                 reads=["kT", sbkey], writes=[pk[5]], inc=(m == 3))
                        for m in range(4):
                            mb = slice(m * 128, (m + 1) * 128)
                            S.op("pe", lambda e, m=m, mb=mb: e.matmul(pb[4][0:64, mb], lhsT=qT[:, m, tok], rhs=SBl[:, mb],
                                                                      start=True, stop=True),
                                 reads=["qT", sbkey], writes=[pk[4]], inc=(m == 3))
                        yield
                        S.op("dve", lambda e: e.tensor_tensor(out=h3(t_x), in0=h3(pb[5][0:64, :]), in1=tkb("bg", i), op=ALU.mult),
                             reads=[pk[5], "bg"], writes=["t_x"])
                        S.op("dve", lambda e: e.tensor_tensor(out=r_b, in0=vb, in1=t_x, op=ALU.subtract),
                             reads=["vb" + pp, "t_x"], writes=["r_b"])
                        S.op("dve", lambda e: e.tensor_tensor(out=h3(o1), in0=h3(pb[4][0:64, :]), in1=tkb("gam", i), op=ALU.mult),
                             reads=[pk[4], "gam"], writes=["o1"])
                        yield
                        for h in range(8):
                            hb = slice(h * 64, (h + 1) * 64)
                            S.op("pe", lambda e, hb=hb: e.matmul(pb[5][0:64, hb], lhsT=TTm[:, hb], rhs=r_b[:, hb],
                                                                 start=True, stop=True),
                                 reads=["TT" + pp, "r_b"], writes=[pk[5]], inc=(h == 7))
                        S.op("act", lambda e: e.copy(out=vnew, in_=pb[5][0:64, :]), reads=[pk[5]], writes=["vnew"])
                        yield
                        for m in range(4):
                            mb = slice(m * 128, (m + 1) * 128)
                            S.op("pe", lambda e, mb=mb: e.matmul(pb[4][:, mb], lhsT=kd[:, mb], rhs=vnew[:, mb],
                                                                 start=True, stop=True),
                                 reads=["kd" + pp, "vnew"], writes=[pk[4]], inc=(m == 3))
                        for h in range(8):
                            hb = slice(h * 64, (h + 1) * 64)
                            S.op("pe", lambda e, hb=hb: e.matmul(pb[5][0:64, hb], lhsT=qkTb[:, hb], rhs=vnew[:, hb],
                                                                 start=True, stop=True),
                                 reads=["qkT" + pp, "vnew"], writes=[pk[5]], inc=(h == 7))
                        yield
                        S.op("dve", lambda e: e.tensor_tensor(out=t1, in0=pb[4][:, :], in1=maskrep, op=ALU.mult),
                             reads=[pk[4], "cst"], writes=["t1"])
                        S.op("pool", lambda e: e.tensor_tensor(out=SL[:, :], in0=SL[:, :], in1=egm, op=ALU.mult),
                             reads=[skey, "egm" + pp], writes=[skey])
                        S.op("dve", lambda e: e.tensor_tensor(out=SL[:, :], in0=SL[:, :], in1=t1, op=ALU.add),
                             reads=[skey, "t1"], writes=[skey])
                        S.op("act", lambda e: e.copy(out=SBl[:, :], in_=SL[:, :]), reads=[skey], writes=[sbkey])
                        yield
                        S.op("dve", lambda e: e.tensor_tensor(out=o_tok, in0=o1, in1=pb[5][0:64, :], op=ALU.add),
                             reads=["o1", pk[5]], writes=["o_tok"])
                        S.op("act", lambda e: e.activation(out=osq, in_=o_tok, func=AF.Square), reads=["o_tok"], writes=["osq"])
                        S.op("dve", lambda e: e.tensor_reduce(out=ssr, in_=h3(osq), axis=mybir.AxisListType.X, op=ALU.add),
                             reads=["osq"], writes=["ssr"])
                        S.op("act", lambda e: e.activation(out=rr, in_=ssr, func=AF.Ln, bias=EPS, scale=1.0 / 64),
                             reads=["ssr"], writes=["rr"])
                        S.op("act", lambda e: e.activation(out=rr, in_=rr, func=AF.Exp, scale=-0.5), reads=["rr"], writes=["rr"])
                        S.op("dve", lambda e: e.tensor_tensor(out=h3(on_b), in0=h3(o_tok),
                                                              in1=rr.unsqueeze(2).to_broadcast([64, 8, 64]), op=ALU.mult),
                             reads=["o_tok", "rr"], writes=["on_b"])
                        yield
                        for m in range(4):
                            S.op("pe", lambda e, m=m: e.transpose(pT[:, 512 + m * 64:512 + (m + 1) * 64], on_b[:, m * 128:(m + 1) * 128],
                                                                  i64b),
                                 reads=["on_b", "identb"], writes=[pk[7]], inc=(m == 3))
                        S.op("dve", lambda e: e.scalar_tensor_tensor(
                            out=oT[:, :, tok], in0=pT[:, 512:768].rearrange("p (m c) -> p m c", m=4), scalar=P(l, 146),
                            in1=szT[:, :, tok], op0=ALU.mult, op1=ALU.mult),
                            reads=[pk[7], "szT", "prm"], writes=["oT"])
                        yield

                    for _ in prep(0):
                        pass
                    for i in range(NCH):
                        ga = rec(i)
                        gb = prep(i + 1) if i + 1 < NCH else iter(())
                        da = db = False
                        while not (da and db):
                            if not da:
                                try:
                                    next(ga)
                                except StopIteration:
                                    da = True
                            for _ in range(2):
                                if not db:
                                    try:
                                        next(gb)
                                    except StopIteration:
                                        db = True
                    for dg in range(2):
                        wi, (wO,) = wload([(KD, 512)])
                        for dd in range(4):
                            d = dg * 4 + dd
                            banks = next_banks()
                            proj(banks, lambda k, dd=dd: wO[:, k, dd * 128:(dd + 1) * 128],
                                 lambda k, half: (oT[:, k, half * 512:(half + 1) * 512] if k < 4
                                                  else yT[:, k - 4, half * 512:(half + 1) * 512]),
                                 KD, "w%d" % wi, ["oT", "yT"])
                            for half in range(2):
                                S.op("dve", lambda e, d=d, half=half, b=banks[half]: e.tensor_tensor(
                                    out=xT[:, d, half * 512:(half + 1) * 512], in0=pb[b][:, :],
                                    in1=xT[:, d, half * 512:(half + 1) * 512], op=ALU.add),
                                    reads=[pk[banks[half]], "xT"], writes=["xT"])
                    S.barrier()
                    rmsnorm_to_hT(lambda k: P(l, 8 + k))
                    for g in range(NF // 2):
                        wi, (wV, wG) = wload([(KD, 256)] * 2)
                        for jj in range(2):
                            f = g * 2 + jj
                            C_ = next_set(); n = C_["n"]
                            bV = next_banks()
                            proj(bV, lambda k, jj=jj: wV[:, k, jj * 128:(jj + 1) * 128], hT_rhs, KD, "w%d" % wi, HTK)
                            hal = hal_ff[:, (l * NF + f) * 2:(l * NF + f) * 2 + 2]
                            hkey = "hff%d_%d" % (l, f)
                            halo_in(C_, hal, hkey, 3, first)
                            for half in range(2):
                                S.op("act", lambda e, half=half: e.copy(out=C_["cb"][:, 2 + half * 512:2 + (half + 1) * 512],
                                                                         in_=pb[bV[half]][:, :]),
                                     reads=[pk[bV[half]]], writes=["cb" + n + "h%d" % half])
                            conv(C_, 3, lambda j, f=f: P(l, 76 + f * 3 + j))
                            halo_out(C_, hal, hkey, 3)
                            S.op("act", lambda e: e.activation(out=C_["ac"][:, :], in_=C_["ac"][:, :], func=AF.Silu),
                                 reads=ackeys(C_), writes=ackeys(C_))
                            bG = next_banks()
                            proj(bG, lambda k, jj=jj: wG[:, k, jj * 128:(jj + 1) * 128], hT_rhs, KD, "w%d" % wi, HTK)
                            for half in range(2):
                                S.op("dve", lambda e, f=f, half=half: e.tensor_tensor(
                                    out=actT[:, f, half * 512:(half + 1) * 512], in0=pb[bG[half]][:, :],
                                    in1=C_["ac"][:, half * 512:(half + 1) * 512], op=ALU.mult),
                                    reads=[pk[bG[half]]] + ackeys(C_), writes=["actT"])
                    for d in range(KD):
                        wi, (wD,) = wload([(NF, 128)])
                        banks = next_banks()
                        proj(banks, lambda f: wD[:, f, :], lambda f, half: actT[:, f, half * 512:(half + 1) * 512],
                             NF, "w%d" % wi, ["actT"])
                        for half in range(2):
                            S.op("dve", lambda e, d=d, half=half, b=banks[half]: e.tensor_tensor(
                                out=xT[:, d, half * 512:(half + 1) * 512], in0=pb[b][:, :],
                                in1=xT[:, d, half * 512:(half + 1) * 512], op=ALU.add),
                                reads=[pk[banks[half]], "xT"], writes=["xT"])
                S.barrier()
                rmsnorm_to_hT(lambda k: prm[:, PL * DEPTH + k:PL * DEPTH + k + 1])
                for k in range(KD):
                    S.op("dve", lambda e, k=k: e.scalar_tensor_tensor(
                        out=xT[:, k, :], in0=xT[:, k, :], scalar=prm[:, PL * DEPTH + k:PL * DEPTH + k + 1], in1=rstd[:, :],
                        op0=ALU.mult, op1=ALU.mult), reads=["xT", "rstd0", "rstd1", "prm"] + HTK, writes=["xT"])
                for tb in range(8):
                    st = stage[tb % 2]
                    for dq in range(2):
                        b = 5 + dq
                        for dd in range(4):
                            d = dq * 4 + dd
                            S.op("pe", lambda e, d=d, dd=dd, b=b: e.transpose(
                                pb[b][:, dd * 128:(dd + 1) * 128], xT[:, d, tb * 128:(tb + 1) * 128], ident),
                                reads=["xT", "cst"], writes=[pk[b]], inc=(dd == 3))
                        S.op("act", lambda e, dq=dq, b=b, st=st: e.copy(out=st[:, dq * 512:(dq + 1) * 512], in_=pb[b][:, :]),
                             reads=[pk[b]], writes=["stage%d" % (tb % 2)])
                    S.dma("sp", out_d[row0 + tb * 128: row0 + (tb + 1) * 128, :], st, "so%d" % (tb % 2),
                          reads=["stage%d" % (tb % 2)])
        S.drain_all("sp")
        build.nins = S.nins
    return nc


def make_consts():
    c = np.zeros((128, 128 * 4 + 64 * 4 + 128 + 512), np.float32)
    c[:, 0:128] = np.eye(128)
    c[:, 128:256] = 1.0
    c[0:64, 256:320] = 1.0
    c[64:128, 320:384] = 1.0
    c[63, 384:512] = 1.0
    p = np.arange(64)[:, None]
    f = np.arange(64)[None, :]
    c0 = 512
    c[0:64, c0:c0 + 64] = (p <= f)
    c[0:64, c0 + 64:c0 + 128] = np.where(p > f, 0.0, NEG)
    c[0:64, c0 + 128:c0 + 192] = np.where(f > p, 0.0, NEG)
    c[0:64, c0 + 192:c0 + 256] = np.where(f >= p, 0.0, NEG)
    c1 = c0 + 256
    c[0:64, c1:c1 + 64] = 1.0
    c2 = c1 + 128
    for h in range(8):
        hh = h % 2
        c[hh * 64:(hh + 1) * 64, c2 + h * 64:c2 + (h + 1) * 64] = 1.0
    return c


def make_params(depth, attn_norm, conv_qkv, a_log, dt_bias, head_norm, conv_sc, sc_norm, ffn_norm, conv_ffn, final_norm):
    pr = np.zeros((128, PL * depth + 8), np.float32)
    for l in range(depth):
        o = l * PL
        pr[:, o + 0:o + 8] = attn_norm[l].reshape(8, 128).T
        pr[:, o + 8:o + 16] = ffn_norm[l].reshape(8, 128).T
        pr[:, o + 16:o + 64] = conv_qkv[l].reshape(4, 12, 128).transpose(2, 1, 0).reshape(128, 48)
        pr[:, o + 64:o + 76] = conv_sc[l].reshape(3, 4, 128).transpose(2, 1, 0).reshape(128, 12)
        pr[:, o + 76:o + 142] = conv_ffn[l].reshape(3, NF, 128).transpose(2, 1, 0).reshape(128, 66)
        pr[:, o + 142:o + 146] = sc_norm[l].reshape(4, 128).T
        pr[:, o + 146] = np.tile(head_norm[l], 2)
        pr[:, o + 147:o + 155] = a_log[l][None, :]
        pr[:, o + 155:o + 163] = dt_bias[l][None, :]
    pr[:, PL * depth:PL * depth + 8] = final_norm.reshape(8, 128).T
    return pr


def make_wstream(depth, W):
    tot = layer_stream_cols()
    out = np.empty((depth, 128, tot), np.float32)
    for l in range(depth):
        off = 0
        for job in layer_jobspec():
            for (name, c0, n, K_) in job:
                blk = W[name][l][:, c0:c0 + n].reshape(K_, 128, n).transpose(1, 0, 2).reshape(128, K_ * n)
                out[l, :, off:off + K_ * n] = blk
                off += K_ * n
        assert off == tot
    return out


_cache = {}


def run(x, attn_norm, w_in, conv_qkv, a_log, dt_bias, head_norm, conv_sc, sc_norm, w_out, ffn_norm, w_up,
        conv_ffn, w_down, final_norm, n_cores=8):
    x = np.asarray(x, np.float32)
    B = x.shape[0]
    depth = w_in.shape[0]
    nseq = B // n_cores
    key = (nseq, depth)
    if key not in _cache:
        _cache[key] = build(nseq, depth)
    nc = _cache[key]
    prm = make_params(depth, *(np.asarray(a, np.float32) for a in (attn_norm, conv_qkv, a_log, dt_bias, head_norm,
                                                                     conv_sc, sc_norm, ffn_norm, conv_ffn, final_norm)))
    cst = make_consts()
    shared = {"wst": make_wstream(depth, {"w_in": np.asarray(w_in, np.float32), "w_out": np.asarray(w_out, np.float32),
                                          "w_up": np.asarray(w_up, np.float32), "w_down": np.asarray(w_down, np.float32)}),
              "prm": prm, "cst": cst}
    in_maps = []
    for c in range(n_cores):
        m = dict(shared)
        m["x"] = np.ascontiguousarray(x[c * nseq:(c + 1) * nseq].reshape(nseq * T, D))
        in_maps.append(m)
    res = run_bass_kernel_spmd(nc, in_maps, core_ids=list(range(n_cores)))
    out = np.concatenate([np.asarray(r["out"]).reshape(nseq, T, D) for r in res.results], axis=0)
    if DBG:
        np.save("_dbg.npy", np.asarray(res.results[0]["dbg"]))
    return out.astype(np.float32)


def kernel(x, attn_norm, w_in, conv_qkv, a_log, dt_bias, head_norm, conv_sc, sc_norm, w_out, ffn_norm, w_up,
           conv_ffn, w_down, final_norm):
    return run(x, attn_norm, w_in, conv_qkv, a_log, dt_bias, head_norm, conv_sc, sc_norm, w_out, ffn_norm, w_up,
               conv_ffn, w_down, final_norm, n_cores=8)
```
